# Optimizing a Trainium2 kernel written in Bass

```python
import jax, jax.numpy as jnp
from jax import lax
import numpy as np

D_MODEL = 1024
BATCH = 16
SEQ = 2048
DEPTH = 2

GRID_W = 64
CTX_LEN = 256
MIX_WIDTH = D_MODEL
NA_HEAD_DIM = 64
NA_WIDTH = MIX_WIDTH // 2
NA_HEADS = NA_WIDTH // NA_HEAD_DIM
NB_ROWS = 8
NB_COLS = 16
QB_COLS = 16
KB_COLS = QB_COLS + NB_COLS
CONV_CH = MIX_WIDTH // 4
CONV_WIDTH = 31
RET_WIDTH = MIX_WIDTH // 4
RET_HEADS = 4
RET_V_DIM = RET_WIDTH // RET_HEADS
RET_QK_DIM = RET_V_DIM // 2
RET_QK_WIDTH = RET_HEADS * RET_QK_DIM
RET_DECAY_BASE = 5.0
CHUNK = 128
IN_WIDTH = 3 * NA_WIDTH + 2 * CONV_CH + 2 * RET_QK_WIDTH + 3 * RET_WIDTH
D_FF = 2816
N_EXPERTS = 8
TOP_K = 2
D_FF_EXPERT = 3584
ROPE_BASE = 10000.0
EPS = 1e-6
NEG_INF = -1e30

kernel_name = "hybrid_na_conv_retention_moe_dit"


def _in_layout():
    names = ("na_q", "na_k", "na_v", "conv_glu", "ret_q", "ret_k", "ret_v", "ret_gf", "ret_gb")
    sizes = (NA_WIDTH, NA_WIDTH, NA_WIDTH, 2 * CONV_CH, RET_QK_WIDTH, RET_QK_WIDTH,
             RET_WIDTH, RET_WIDTH, RET_WIDTH)
    out, start = {}, 0
    for n, s in zip(names, sizes):
        out[n] = (start, s)
        start += s
    return out


def _cols(a, span):
    s, n = span
    return a[..., s:s + n]


def _heads(a, n_heads):
    b, l, _ = a.shape
    return a.reshape(b, l, n_heads, -1).transpose(0, 2, 1, 3)


def _merge(a):
    b, h, l, d = a.shape
    return a.transpose(0, 2, 1, 3).reshape(b, l, h * d)


def _flip(a):
    return a[:, :, ::-1]


def _rmsnorm(x, g):
    xf = x.astype(jnp.float32)
    y = xf * lax.rsqrt(jnp.mean(xf * xf, axis=-1, keepdims=True) + EPS)
    return (y * g.astype(jnp.float32)).astype(x.dtype)


def _layernorm(x, g, b):
    xf = x.astype(jnp.float32)
    mu = jnp.mean(xf, axis=-1, keepdims=True)
    var = jnp.mean(jnp.square(xf - mu), axis=-1, keepdims=True)
    y = (xf - mu) * lax.rsqrt(var + EPS) * g.astype(jnp.float32) + b.astype(jnp.float32)
    return y.astype(x.dtype)


def _adaln(cvec, w_mod, b_mod, n):
    m = jax.nn.silu(cvec) @ w_mod[:, :n * D_MODEL] + b_mod[:n * D_MODEL]
    return jnp.split(m[..., None, :], n, axis=-1)


def _modulate(h, g, shift, scale):
    return _rmsnorm(h, g) * (1.0 + scale) + shift


def _axial_rope(t_len, dim):
    t = jnp.arange(t_len)
    row = (t // GRID_W).astype(jnp.float32)
    col = (t % GRID_W).astype(jnp.float32)
    axis_dim = dim // 2
    inv = ROPE_BASE ** (-jnp.arange(0, axis_dim, 2, dtype=jnp.float32) / axis_dim)
    ang = jnp.concatenate([row[:, None] * inv, col[:, None] * inv], axis=-1)
    return jnp.cos(ang), jnp.sin(ang)


def _rope(x, cos, sin):
    x1, x2 = jnp.split(x, 2, axis=-1)
    return jnp.concatenate([x1 * cos - x2 * sin, x2 * cos + x1 * sin], axis=-1)


def _neighbourhood_attention(q, k, v, k_ctx, v_ctx, rpb):
    b, h, t, dh = q.shape
    rows = t // GRID_W
    wr = min(NB_ROWS, rows)
    ncb = GRID_W // QB_COLS
    r = np.arange(rows)
    key_rows = np.clip(r - wr // 2, 0, rows - wr)[:, None] + np.arange(wr)[None, :]
    qcol = np.arange(GRID_W).reshape(ncb, QB_COLS)
    kc0 = np.clip(qcol[:, 0] - NB_COLS // 2, 0, GRID_W - KB_COLS)
    key_cols = kc0[:, None] + np.arange(KB_COLS)[None, :]
    win_c0 = np.clip(qcol - NB_COLS // 2, 0, GRID_W - NB_COLS)
    kcol = key_cols[:, None, :]
    col_ok = (kcol >= win_c0[..., None]) & (kcol < win_c0[..., None] + NB_COLS)
    dr = key_rows - r[:, None] + NB_ROWS - 1
    dc = np.clip(kcol - qcol[..., None] + NB_COLS - 1, 0, 2 * NB_COLS - 2)
    bias = rpb.astype(jnp.float32)[:, dr[:, None, None, :, None], dc[None, :, :, None, :]]
    bias = jnp.where(col_ok[None, None, :, :, None, :], bias, NEG_INF)

    ridx = key_rows[:, None, :, None]
    cidx = key_cols[None, :, None, :]
    kg = k.reshape(b, h, rows, GRID_W, dh)[:, :, ridx, cidx]
    vg = v.reshape(b, h, rows, GRID_W, dh)[:, :, ridx, cidx]
    qb = q.reshape(b, h, rows, ncb, QB_COLS, dh) * (dh ** -0.5)
    s_loc = jnp.einsum("bhrnqd,bhrnwkd->bhrnqwk", qb, kg).astype(jnp.float32) + bias
    n_loc = wr * KB_COLS
    s_loc = s_loc.reshape(b, h, rows, ncb, QB_COLS, n_loc)
    s_ctx = jnp.einsum("bhrnqd,bhcd->bhrnqc", qb, k_ctx).astype(jnp.float32)
    p = jax.nn.softmax(jnp.concatenate([s_loc, s_ctx], axis=-1), axis=-1).astype(v.dtype)
    p_loc = p[..., :n_loc].reshape(b, h, rows, ncb, QB_COLS, wr, KB_COLS)
    o = (jnp.einsum("bhrnqwk,bhrnwkd->bhrnqd", p_loc, vg)
         + jnp.einsum("bhrnqc,bhcd->bhrnqd", p[..., n_loc:], v_ctx))
    return o.reshape(b, h, t, dh)


def _context_attention(q, k, v):
    s = jnp.einsum("bhid,bhjd->bhij", q * (q.shape[-1] ** -0.5), k).astype(jnp.float32)
    p = jax.nn.softmax(s, axis=-1).astype(v.dtype)
    return jnp.einsum("bhij,bhjd->bhid", p, v)


def _conv_module(u, conv_w, conv_b, ln_w, ln_b):
    a, g = jnp.split(u, 2, axis=-1)
    y = a * jax.nn.sigmoid(g)
    y = lax.conv_general_dilated(
        y, conv_w[:, None, :].astype(y.dtype), window_strides=(1,),
        padding=[(CONV_WIDTH // 2, CONV_WIDTH // 2)],
        dimension_numbers=("NWC", "WIO", "NWC"), feature_group_count=CONV_CH) + conv_b
    return jax.nn.silu(_layernorm(y, ln_w, ln_b))


def _retention_chunked(q, k, v, gamma, s0):
    b, h, t, _ = q.shape
    dv = v.shape[-1]
    n_chunks = t // CHUNK
    lg = jnp.log(gamma)[:, None]
    pos = jnp.arange(CHUNK, dtype=jnp.float32)
    diff = pos[:, None] - pos[None, :]
    intra = jnp.where(diff >= 0, jnp.exp(lg[:, :, None] * jnp.maximum(diff, 0.0)), 0.0)
    q_dec = jnp.exp(lg * (pos + 1.0))[..., None]
    k_dec = jnp.exp(lg * (CHUNK - 1.0 - pos))[..., None]
    c_dec = jnp.exp(lg[:, 0] * CHUNK)[:, None, None]

    def to_chunks(a):
        return jnp.moveaxis(a.reshape(b, h, n_chunks, CHUNK, a.shape[-1]), 2, 0)

    def step(s, xs):
        qc, kc, vc = xs
        inner = jnp.einsum("bhid,bhjd->bhij", qc, kc) * intra
        o = (jnp.einsum("bhij,bhje->bhie", inner, vc)
             + jnp.einsum("bhid,bhde->bhie", qc * q_dec, s))
        s = c_dec * s + jnp.einsum("bhjd,bhje->bhde", kc * k_dec, vc)
        return s, o

    s_fin, o = lax.scan(step, s0, (to_chunks(q), to_chunks(k), to_chunks(v)))
    return jnp.moveaxis(o, 0, 2).reshape(b, h, t, dv), s_fin


def _retention_state(k, v, gamma):
    t = k.shape[2]
    w = jnp.exp(jnp.log(gamma)[:, None] * (t - 1.0 - jnp.arange(t, dtype=jnp.float32)))
    return jnp.einsum("bhtd,bhte->bhde", k * w[..., None], v)


def _head_norm(o, g):
    mu = jnp.mean(o, axis=-1, keepdims=True)
    var = jnp.mean(jnp.square(o - mu), axis=-1, keepdims=True)
    return _merge((o - mu) * lax.rsqrt(var + EPS)) * g.astype(jnp.float32)


def _bidir_retention(q, k, v, g_f, g_b, s0, gamma, gn_w):
    o_f, s_f = _retention_chunked(q, k, v, gamma[0], s0[0])
    o_b, s_b = _retention_chunked(_flip(q), _flip(k), _flip(v), gamma[1], s0[1])
    y = (jax.nn.silu(g_f.astype(jnp.float32)) * _head_norm(o_f, gn_w)
         + jax.nn.silu(g_b.astype(jnp.float32)) * _head_norm(_flip(o_b), gn_w))
    return y, (s_f, s_b)


def _mixer(ux, uc, w_in, w_out, rpb, conv_w, conv_b, conv_ln_w, conv_ln_b, ret_decay, ret_gn_w,
           cos, sin, need_ctx_out):
    f32 = jnp.float32
    lay = _in_layout()
    px = ux @ w_in
    col_x = lambda name: _cols(px, lay[name])
    if need_ctx_out:
        pc = uc @ w_in
        col_c = lambda name: _cols(pc, lay[name])
    else:
        col_c = lambda name: uc @ _cols(w_in, lay[name])

    k_na_c = _heads(col_c("na_k"), NA_HEADS)
    v_na_c = _heads(col_c("na_v"), NA_HEADS)
    y_na = _merge(_neighbourhood_attention(
        _heads(col_x("na_q"), NA_HEADS), _heads(col_x("na_k"), NA_HEADS),
        _heads(col_x("na_v"), NA_HEADS), k_na_c, v_na_c, rpb))

    y_conv = _conv_module(col_x("conv_glu"), conv_w, conv_b, conv_ln_w, conv_ln_b)

    gamma = 1.0 - jnp.exp2(-ret_decay.astype(f32))
    k_scale = RET_QK_DIM ** -0.5
    kc = _heads(col_c("ret_k"), RET_HEADS).astype(f32) * k_scale
    vc = _heads(col_c("ret_v"), RET_HEADS).astype(f32)
    if need_ctx_out:
        qc = _heads(col_c("ret_q"), RET_HEADS).astype(f32)
        zero = jnp.zeros(kc.shape[:2] + (RET_QK_DIM, RET_V_DIM), f32)
        y_ret_c, s_ctx = _bidir_retention(qc, kc, vc, col_c("ret_gf"), col_c("ret_gb"),
                                          (zero, zero), gamma, ret_gn_w)
    else:
        s_ctx = (_retention_state(kc, vc, gamma[0]),
                 _retention_state(_flip(kc), _flip(vc), gamma[1]))
    q = _rope(_heads(col_x("ret_q"), RET_HEADS).astype(f32), cos, sin)
    k = _rope(_heads(col_x("ret_k"), RET_HEADS).astype(f32), cos, sin) * k_scale
    v = _heads(col_x("ret_v"), RET_HEADS).astype(f32)
    y_ret, _ = _bidir_retention(q, k, v, col_x("ret_gf"), col_x("ret_gb"), s_ctx, gamma, ret_gn_w)

    y_lat = jnp.concatenate([y_na, y_conv, y_ret.astype(ux.dtype)], axis=-1) @ w_out
    if not need_ctx_out:
        return y_lat, None
    y_na_c = _merge(_context_attention(_heads(col_c("na_q"), NA_HEADS), k_na_c, v_na_c))
    y_conv_c = _conv_module(col_c("conv_glu"), conv_w, conv_b, conv_ln_w, conv_ln_b)
    y_ctx = jnp.concatenate([y_na_c, y_conv_c, y_ret_c.astype(uc.dtype)], axis=-1) @ w_out
    return y_lat, y_ctx


def _swiglu(h, w_gate, w_up, w_down):
    return (jax.nn.silu(h @ w_gate) * (h @ w_up)) @ w_down


def _moe(h, router_w, router_b, w_gate, w_up, w_down):
    logits = (h @ router_w).astype(jnp.float32) + router_b.astype(jnp.float32)
    top_v, top_i = lax.top_k(logits, TOP_K)
    top_p = jax.nn.softmax(top_v, axis=-1)
    combine = jnp.sum(jax.nn.one_hot(top_i, N_EXPERTS, dtype=jnp.float32) * top_p[..., None],
                      axis=-2).astype(h.dtype)
    y = jnp.zeros_like(h)
    for e in range(N_EXPERTS):
        y = y + combine[..., e:e + 1] * _swiglu(h, w_gate[e], w_up[e], w_down[e])
    return y


def setup_inputs(seed: int = 0) -> dict:
    key = jax.random.key(seed)
    ks = iter(jax.random.split(key, 32))
    f32 = jnp.float32
    nrm = lambda shape, s: jax.random.normal(next(ks), shape, f32) * s
    n_dense = (DEPTH + 1) // 2
    n_moe = DEPTH // 2
    return {
        "x": nrm((BATCH, SEQ, D_MODEL), 1.0),
        "c": nrm((BATCH, D_MODEL), 1.0),
        "ctx": nrm((BATCH, CTX_LEN, D_MODEL), 1.0),
        "c_ctx": nrm((D_MODEL,), 1.0),
        "w_mod": nrm((DEPTH, D_MODEL, 6 * D_MODEL), 0.5 * D_MODEL ** -0.5),
        "b_mod": nrm((DEPTH, 6 * D_MODEL), 0.02),
        "norm1_w": 1.0 + nrm((DEPTH, D_MODEL), 0.05),
        "norm2_w": 1.0 + nrm((DEPTH, D_MODEL), 0.05),
        "w_in": nrm((DEPTH, D_MODEL, IN_WIDTH), D_MODEL ** -0.5),
        "w_out": nrm((DEPTH, MIX_WIDTH, D_MODEL), MIX_WIDTH ** -0.5),
        "na_rpb": nrm((DEPTH, NA_HEADS, 2 * NB_ROWS - 1, 2 * NB_COLS - 1), 0.2),
        "conv_w": nrm((DEPTH, CONV_WIDTH, CONV_CH), CONV_WIDTH ** -0.5),
        "conv_b": nrm((DEPTH, CONV_CH), 0.02),
        "conv_ln_w": 1.0 + nrm((DEPTH, CONV_CH), 0.05),
        "conv_ln_b": nrm((DEPTH, CONV_CH), 0.02),
        "ret_decay": RET_DECAY_BASE + jnp.arange(RET_HEADS, dtype=f32) + nrm((DEPTH, 2, RET_HEADS), 0.1),
        "ret_gn_w": 1.0 + nrm((DEPTH, RET_WIDTH), 0.05),
        "ffn_w_gate": nrm((n_dense, D_MODEL, D_FF), D_MODEL ** -0.5),
        "ffn_w_up": nrm((n_dense, D_MODEL, D_FF), D_MODEL ** -0.5),
        "ffn_w_down": nrm((n_dense, D_FF, D_MODEL), D_FF ** -0.5),
        "moe_router": nrm((n_moe, D_MODEL, N_EXPERTS), D_MODEL ** -0.5),
        "moe_router_b": nrm((n_moe, N_EXPERTS), 0.01),
        "moe_w_gate": nrm((n_moe, N_EXPERTS, D_MODEL, D_FF_EXPERT), D_MODEL ** -0.5),
        "moe_w_up": nrm((n_moe, N_EXPERTS, D_MODEL, D_FF_EXPERT), D_MODEL ** -0.5),
        "moe_w_down": nrm((n_moe, N_EXPERTS, D_FF_EXPERT, D_MODEL), D_FF_EXPERT ** -0.5),
        "final_norm_w": 1.0 + nrm((D_MODEL,), 0.05),
    }


def reference(x, c, ctx, c_ctx, w_mod, b_mod, norm1_w, norm2_w, w_in, w_out, na_rpb, conv_w, conv_b,
              conv_ln_w, conv_ln_b, ret_decay, ret_gn_w, ffn_w_gate, ffn_w_up, ffn_w_down,
              moe_router, moe_router_b, moe_w_gate, moe_w_up, moe_w_down, final_norm_w):
    n_ctx = ctx.shape[1]
    cos, sin = _axial_rope(x.shape[1], RET_QK_DIM)
    h_lat, h_ctx = x, ctx
    for l in range(DEPTH):
        last = l == DEPTH - 1
        sh1, sc1, g1, sh2, sc2, g2 = _adaln(c, w_mod[l], b_mod[l], 6)
        if last:
            csh1, csc1 = _adaln(c_ctx, w_mod[l], b_mod[l], 2)
        else:
            csh1, csc1, cg1, csh2, csc2, cg2 = _adaln(c_ctx, w_mod[l], b_mod[l], 6)
        ux = _modulate(h_lat, norm1_w[l], sh1, sc1)
        uc = _modulate(h_ctx, norm1_w[l], csh1, csc1)
        y_lat, y_ctx = _mixer(ux, uc, w_in[l], w_out[l], na_rpb[l], conv_w[l], conv_b[l],
                              conv_ln_w[l], conv_ln_b[l], ret_decay[l], ret_gn_w[l],
                              cos, sin, not last)
        h_lat = h_lat + g1 * y_lat
        if last:
            u = _modulate(h_lat, norm2_w[l], sh2, sc2)
        else:
            h_ctx = h_ctx + cg1 * y_ctx
            u = jnp.concatenate([_modulate(h_ctx, norm2_w[l], csh2, csc2),
                                 _modulate(h_lat, norm2_w[l], sh2, sc2)], axis=1)
        if l % 2 == 0:
            f = _swiglu(u, ffn_w_gate[l // 2], ffn_w_up[l // 2], ffn_w_down[l // 2])
        else:
            f = _moe(u, moe_router[l // 2], moe_router_b[l // 2], moe_w_gate[l // 2],
                     moe_w_up[l // 2], moe_w_down[l // 2])
        if last:
            h_lat = h_lat + g2 * f
        else:
            h_ctx = h_ctx + cg2 * f[:, :n_ctx]
            h_lat = h_lat + g2 * f[:, n_ctx:]
    return _rmsnorm(h_lat, final_norm_w)
```

```python
import contextlib
import numpy as np
CUT = 0
import concourse.bass as bass
import concourse.mybir as mybir
from concourse.bass_utils import run_bass_kernel_spmd

F32 = mybir.dt.float32
BF16 = mybir.dt.bfloat16
ALU = mybir.AluOpType
AF = mybir.ActivationFunctionType
AX = mybir.AxisListType

ENGS = ("tensor", "vector", "scalar", "gpsimd", "sync")
NDMA = 12


class Op:
    __slots__ = ("eng", "fn", "dma", "deps", "signal", "sem", "val", "idx")

    def __init__(self, eng, fn, dma):
        self.eng, self.fn, self.dma = eng, fn, dma
        self.deps = []
        self.signal = dma
        self.sem = None
        self.val = None


class Sched:
    def __init__(self, nc, stack):
        self.nc = nc
        self.csem = {e: stack.enter_context(nc.semaphore("cs_" + e)) for e in ENGS}
        self.ccnt = {e: 0 for e in ENGS}
        self.dsem = {e: [stack.enter_context(nc.semaphore("ds_%s%d" % (e, i))) for i in range(NDMA)]
                     for e in ("sync", "gpsimd")}
        self.dcnt = {e: [0] * NDMA for e in self.dsem}
        self.drr = {e: 0 for e in self.dsem}
        self.known = {e: {} for e in ENGS}
        self.same_engine_sync = True
        self.reset()

    def reset(self):
        self.ops = {e: [] for e in ENGS}
        self.lastw = {}
        self.readers = {}
        self.allops = []

    def op(self, eng, fn, R=(), W=(), dma=False):
        o = Op(eng, fn, dma)
        deps = []
        for r in R:
            w = self.lastw.get(r)
            if w is not None:
                deps.append(w)
        for w_ in W:
            w = self.lastw.get(w_)
            if w is not None:
                deps.append(w)
            deps.extend(self.readers.get(w_, ()))
        seen = set()
        for d in deps:
            if id(d) in seen or d is o:
                continue
            seen.add(id(d))
            if d.eng == eng and not d.dma and not dma:
                if eng == "tensor" or not self.same_engine_sync:
                    continue
            o.deps.append(d)
        for r in R:
            lst = self.readers.setdefault(r, [])
            if not dma:
                lst[:] = [x for x in lst if not (x.eng == eng and not x.dma)]
            lst.append(o)
        for w_ in W:
            self.lastw[w_] = o
            self.readers[w_] = []
        self.ops[eng].append(o)
        self.allops.append(o)
        return o

    def mm(self, out, lhsT, rhs, start=True, stop=True, R=(), W=()):
        return self.op("tensor", lambda e: e.matmul(out, lhsT=lhsT, rhs=rhs, start=start, stop=stop), R, W)

    def tr(self, out, in_, ident, R=(), W=()):
        return self.op("tensor", lambda e: e.transpose(out=out, in_=in_, identity=ident), R, W)

    def actf(self, out, in_, func, R=(), W=(), **kw):
        return self.op("scalar", lambda e: e.activation(out=out, in_=in_, func=func, **kw), R, W)

    def tt(self, out, in0, in1, op, R=(), W=(), eng="vector"):
        return self.op(eng, lambda e: e.tensor_tensor(out=out, in0=in0, in1=in1, op=op), R, W)

    def ts(self, out, in0, s1, s2, op0, op1=None, R=(), W=(), eng="vector", **kw):
        if op1 is None:
            return self.op(eng, lambda e: e.tensor_scalar(out=out, in0=in0, scalar1=s1, scalar2=None, op0=op0, **kw), R, W)
        return self.op(eng, lambda e: e.tensor_scalar(out=out, in0=in0, scalar1=s1, scalar2=s2, op0=op0, op1=op1, **kw), R, W)

    def stt(self, out, in0, scalar, in1, op0, op1, R=(), W=(), **kw):
        return self.op("vector", lambda e: e.scalar_tensor_tensor(out=out, in0=in0, scalar=scalar, in1=in1,
                                                                  op0=op0, op1=op1, **kw), R, W)

    def copy(self, out, in_, R=(), W=(), eng="vector"):
        if eng == "scalar":
            return self.op(eng, lambda e: e.activation(out=out, in_=in_, func=AF.Copy), R, W)
        return self.op(eng, lambda e: e.tensor_copy(out=out, in_=in_), R, W)

    def recip(self, out, in_, R=(), W=()):
        return self.op("vector", lambda e: e.reciprocal(out=out, in_=in_), R, W)

    def rsum(self, out, in_, R=(), W=()):
        return self.op("vector", lambda e: e.reduce_sum(out=out, in_=in_, axis=AX.X), R, W)

    def memset(self, ap, val, W=(), eng="gpsimd"):
        return self.op(eng, lambda e: e.memset(ap, val), (), W)

    def dma(self, out, in_, R=(), W=(), q="sync", **kw):
        return self.op(q, lambda e: e.dma_start(out=out, in_=in_, **kw), R, W, dma=True)

    def emit(self, block):
        for o in self.allops:
            for d in o.deps:
                d.signal = True
        for e in ENGS:
            for o in self.ops[e]:
                if o.dma:
                    i = self.drr[e]
                    self.drr[e] = (i + 1) % NDMA
                    prev = self.dcnt[e][i]
                    self.dcnt[e][i] = prev + 16
                    o.sem = self.dsem[e][i]
                    o.val = prev + 16
                    o.idx = (e, i, prev)
                elif o.signal:
                    self.ccnt[e] += 1
                    o.sem = self.csem[e]
                    o.val = self.ccnt[e]
        sched = self

        def make(e):
            def body(eng):
                known = sched.known[e]
                for o in sched.ops[e]:
                    waits = {}
                    for d in o.deps:
                        k = id(d.sem)
                        if k not in waits or waits[k][1] < d.val:
                            waits[k] = (d.sem, d.val)
                    if o.dma and o.idx[2] > 0:
                        s = sched.dsem[o.idx[0]][o.idx[1]]
                        k = id(s)
                        if k not in waits or waits[k][1] < o.idx[2]:
                            waits[k] = (s, o.idx[2])
                    for k, (s, v) in waits.items():
                        if known.get(k, 0) >= v:
                            continue
                        eng.wait_ge(s, v)
                        known[k] = v
                    inst = o.fn(eng)
                    if o.signal:
                        inst.then_inc(o.sem, 16 if o.dma else 1)
                if e in sched.dsem:
                    for i, s in enumerate(sched.dsem[e]):
                        v = sched.dcnt[e][i]
                        if v > 0 and known.get(id(s), 0) < v:
                            eng.wait_ge(s, v)
                            known[id(s)] = v
            return body

        for e in ENGS:
            if self.ops[e]:
                getattr(block, e)(make(e))
        self.reset()


D = 1024
T = 2048
LC = 256
NT = T + LC
NTILE = NT // 128
GRID_W = 64
EPS = 1e-6
NH = 8
D_FF = 2816
D_FFE = 3584
NE = 8
C_NAQ, C_NAK, C_NAV, C_CVA, C_CVG, C_RQ, C_RK, C_RV, C_GF, C_GB = 0, 512, 1024, 1536, 1792, 2048, 2176, 2304, 2560, 2816
R_NAQ, R_NAK, R_CV, R_RQ, R_RK, PXT_ROWS = 0, 512, 1024, 1280, 1408, 1536
PXV_COLS = 1280


def na_patterns():
    pats, index, table = [], {}, {}
    for a in range(16):
        kts, pids = [], []
        for kt in range(16):
            quad = []
            anyv = False
            for kr in range(2):
                for qr in range(2):
                    krow, qrow = 2 * kt + kr, 2 * a + qr
                    st = min(max(qrow - 4, 0), 24)
                    if st <= krow <= st + 7:
                        quad.append(krow - qrow + 7)
                        anyv = True
                    else:
                        quad.append(-1)
            if not anyv:
                continue
            atype = a if a in (0, 1, 14, 15) else -1
            key = (atype, kt - a) + tuple(quad)
            if key not in index:
                index[key] = len(pats)
                pats.append(tuple(quad))
            kts.append(kt)
            pids.append(index[key])
        table[a] = (kts, pids)
    return pats, table


def build_program(debug=False, upto=99, part=0, bs=(0, 1)):
    nc = bass.Bass("TRN2", target_bir_lowering=False)
    dk = "ExternalOutput" if debug else "Internal"

    def din(name, shape, dt=F32):
        return nc.dram_tensor(name, list(shape), dt, kind="ExternalInput").ap()

    def dscr(name, shape, dt, handoff=False):
        kind = dk
        if handoff and part == 1:
            kind = "ExternalOutput"
        if handoff and part == 2:
            kind = "ExternalInput"
        return nc.dram_tensor(name, list(shape), dt, kind=kind).ap()

    P1 = part in (0, 1)
    P2 = part in (0, 2)
    _din = din

    def din1(name, shape, dt=F32):
        return _din(name, shape, dt) if P1 else None

    def din2(name, shape, dt=F32):
        return _din(name, shape, dt) if P2 else None

    x2 = din1("x2", [2, T, D]); ctx2 = din1("ctx2", [2, LC, D]); cvecT = din1("cvecT", [128, 8, 3])
    w_mod = din1("w_mod", [2, D, 6 * D]); b_mod = din1("b_mod", [2, 6 * D])
    norm1_w = din1("norm1_w", [2, D]); norm2_w = din1("norm2_w", [2, D])
    w_in = din1("w_in", [2, D, 3072]); w_out = din1("w_out", [2, D, D])
    convwT = din1("convwT", [2, 128, 2, 31]); cpard = din1("cpard", [2, 128, 3, 2])
    ret_decay = din1("ret_decay", [2, 8]); ret_gn_w = din1("ret_gn_w", [2, 256])
    ffn_wg = din1("ffn_w_gate", [1, D, D_FF]); ffn_wu = din1("ffn_w_up", [1, D, D_FF]); ffn_wd = din1("ffn_w_down", [1, D_FF, D])
    moe_routerT = din1("moe_routerT", [NE, D]); moe_rb = din1("moe_router_b", [1, NE])
    moe_wg = din2("moe_w_gate", [NE, D, D_FFE]); moe_wu = din2("moe_w_up", [NE, D, D_FFE]); moe_wd = din2("moe_w_down", [NE, D_FFE, D])
    final_w = din2("final_norm_w", [1, D])
    identd = din1("ident", [128, 128]); rope = din1("rope", [4, 128, NT]); retc = din1("retc", [128, 772])
    napb = din1("napb", [2, 64, NH * 15 * 64])
    out = nc.dram_tensor("out", [2, T, D], F32, kind="ExternalOutput").ap() if P2 else None

    MODS = dscr("MODS", [2, 3, 6 * D], F32, True)
    PXT = dscr("PXT", [2, PXT_ROWS, NT], BF16)
    PXV = dscr("PXV", [2, NT, PXV_COLS], BF16)
    YC = dscr("YC", [2, NT, 768], BF16)
    YT = dscr("YT", [2, 256, NT], BF16)
    HA = dscr("HA", [2, NT, D], F32, True)
    HB = dscr("HB", [2, NT, D], F32)
    UT = dscr("UT", [2, D, NT], BF16, True)
    COMB = dscr("COMB", [2, NT, NE], F32, True)

    pats, ptable = na_patterns()
    NPAT = len(pats)

    with contextlib.ExitStack() as glob:
        S = Sched(nc, glob)
        idb = glob.enter_context(nc.sbuf_tensor("idb", [128, 128], BF16))
        id32 = glob.enter_context(nc.sbuf_tensor("id32", [128, 128], F32))

        def emit_block():
            if not S.allops:
                return
            with nc.Block() as blk:
                S.emit(blk)

        def stage(fn):
            with contextlib.ExitStack() as stg:
                r = fn(stg)
                if r is not None:
                    for _ in r:
                        emit_block()
                emit_block()

        uid = [0]

        def un(name):
            uid[0] += 1
            return "%s_%d" % (name, uid[0])

        def sb(stg, name, shape, dt):
            return stg.enter_context(nc.sbuf_tensor(un(name), list(shape), dt))

        def psf(stg, name, n=512):
            return stg.enter_context(nc.psum_tensor(un(name), [128, n], F32))

        def psb(stg, name, n=1024):
            return stg.enter_context(nc.psum_tensor(un(name), [128, n], BF16))

        def hsrc(l, b, t):
            if l == 0:
                return ctx2[b, t * 128:(t + 1) * 128, :] if t < 2 else x2[b, (t - 2) * 128:(t - 1) * 128, :]
            return HB[b, t * 128:(t + 1) * 128, :]

        def rms_rstd(stg_bufs, h_ap, hkey, junk, ss, rstd, tag):
            S.actf(junk, h_ap, AF.Square, R=[hkey], W=["junk" + tag, "ss" + tag], accum_out=ss)
            S.actf(rstd, ss, AF.Sqrt, R=["ss" + tag], W=["rstd" + tag], scale=1.0 / D, bias=epsb[:, 0:1])
            S.recip(rstd, rstd, R=["rstd" + tag], W=["rstd" + tag])

        epsb = glob.enter_context(nc.sbuf_tensor("epsb", [128, 1], F32))

        def stage_a(stg):
            S.dma(idb[:], identd, W=["idb"], q="gpsimd")
            S.dma(id32[:], identd, W=["id32"])
            S.memset(epsb[:], EPS, W=["epsb"])
            cT = sb(stg, "cT", [128, 8, 3], F32)
            sT = sb(stg, "sT", [128, 8, 3], F32)
            S.dma(cT[:], cvecT, W=["cT"])
            S.actf(sT[:], cT[:], AF.Silu, R=["cT"], W=["sT"])
            wb = [sb(stg, "wmb%d" % i, [128, 8, 512], F32) for i in range(2)]
            modrow = sb(stg, "modrow", [3, 6 * D], F32)
            bm3 = sb(stg, "bm3", [3, 6 * D], F32)
            nw3 = sb(stg, "nw3", [3, 2, D], F32)
            pm = [psf(stg, "pm%d" % i) for i in range(2)]
            it = 0
            for l in range(2):
                S.dma(bm3[:], b_mod[l:l + 1, :].partition_broadcast(3), R=["bm3"], W=["bm3"])
                S.dma(nw3[:, 0, :], norm1_w[l:l + 1, :].partition_broadcast(3), W=["nw3"])
                S.dma(nw3[:, 1, :], norm2_w[l:l + 1, :].partition_broadcast(3), W=["nw3"])
                for nb in range(12):
                    i = it % 2
                    it += 1
                    S.dma(wb[i][:], w_mod[l, :, nb * 512:(nb + 1) * 512].rearrange("(k p) n -> p k n", p=128),
                          W=["wmb%d" % i])
                    for k in range(8):
                        S.mm(pm[i][0:3, :], sT[:, k, :], wb[i][:, k, :], start=(k == 0), stop=(k == 7),
                             R=["sT", "wmb%d" % i], W=["pm%d" % i])
                    S.copy(modrow[:, nb * 512:(nb + 1) * 512], pm[i][0:3, :], R=["pm%d" % i], W=["modrow"], eng="scalar")
                S.tt(modrow[:], modrow[:], bm3[:], ALU.add, R=["modrow", "bm3"], W=["modrow"])
                for s_, wsel in ((1, 0), (4, 1)):
                    seg = modrow[:, s_ * D:(s_ + 1) * D]
                    S.stt(seg, seg, 1.0, nw3[:, wsel, :], ALU.add, ALU.mult, R=["modrow", "nw3"], W=["modrow"])
                S.dma(MODS[l], modrow[:], R=["modrow"], W=["MODS"])

        def stage_a2(stg):
            S.memset(epsb[:], EPS, W=["epsb"])

        stage(stage_a if P1 else stage_a2)

        def modbc(dst, l, j, s):
            return MODS[l, j:j + 1, s * D:(s + 1) * D].partition_broadcast(128)

        def stage_b(l, bsel):
            def f(stg):
                w = sb(stg, "win", [128, 8, 3328], BF16)
                for k in range(8):
                    S.dma(w[:, k, 0:3072], w_in[l, k * 128:(k + 1) * 128, :], W=["win"], q="gpsimd")
                for (src, dst) in ((C_RQ, 3072), (C_RK, 3200)):
                    sv = w[:, :, src:src + 128].rearrange("p k (h two i) -> p k h two i", two=2, i=16)
                    dv = w[:, :, dst:dst + 128].rearrange("p k (h two i) -> p k h two i", two=2, i=16)
                    S.copy(dv[:, :, :, 0, :], sv[:, :, :, 1, :], R=["win"], W=["win"])
                    S.copy(dv[:, :, :, 1, :], sv[:, :, :, 0, :], R=["win"], W=["win"], eng="gpsimd")
                if CUT == 1:
                    return
                mb = sb(stg, "mb", [128, 4, D], F32)
                ropet = [sb(stg, "ropet%d" % i, [128, 4, 512], F32) for i in range(2)]
                ht = [sb(stg, "ht%d" % i, [128, D], F32) for i in range(3)]
                junk = sb(stg, "junk", [128, D], BF16)
                xn = [sb(stg, "xn%d" % i, [128, D], F32) for i in range(2)]
                ux = [sb(stg, "ux%d" % i, [128, D], BF16) for i in range(2)]
                uxT = [sb(stg, "uxT%d" % i, [128, 8, 512], BF16) for i in range(2)]
                ss = sb(stg, "ss", [128, 4], F32)
                rstd = sb(stg, "rstd", [128, 4], F32)
                fo = [sb(stg, "fo%d" % i, [128, 512], BF16) for i in range(3)]
                sg = [sb(stg, "sg%d" % i, [128, 512], F32) for i in range(2)]
                r1 = [sb(stg, "r1%d" % i, [128, 512], F32) for i in range(2)]
                r2 = [sb(stg, "r2%d" % i, [128, 512], F32) for i in range(2)]
                to = [sb(stg, "to%d" % i, [128, PXV_COLS], BF16) for i in range(2)]
                pT = [psb(stg, "pT%d" % i) for i in range(2)]
                pA = [psf(stg, "pA%d" % i) for i in range(6)]
                tcnt = 0
                gcnt = 0
                focnt = 0
                pacnt = 0
                for b in bsel:
                    for s_, (j, slot) in enumerate(((2, 1), (2, 0), (b, 1), (b, 0))):
                        S.dma(mb[:, s_, :], modbc(None, l, j, slot), R=["MODS"], W=["mb"])
                    for g in range(5):
                        tiles = list(range(4 * g, min(4 * g + 4, NTILE)))
                        N = 128 * len(tiles)
                        gi = gcnt % 2
                        gcnt += 1
                        c0 = 4 * g * 128
                        S.dma(ropet[gi][:, :, 0:N], rope[:, :, c0:c0 + N].rearrange("f p n -> p f n"), W=["ropet%d" % gi])
                        for si, t in enumerate(tiles):
                            hi = tcnt % 3
                            xi = tcnt % 2
                            tcnt += 1
                            mo = 0 if t < 2 else 2
                            S.dma(ht[hi][:], hsrc(l, b, t), R=["HB"], W=["ht%d" % hi])
                            S.actf(junk[:], ht[hi][:], AF.Square, R=["ht%d" % hi], W=["junk", "ss%d" % xi], accum_out=ss[:, xi:xi + 1])
                            S.actf(rstd[:, xi:xi + 1], ss[:, xi:xi + 1], AF.Sqrt, R=["ss%d" % xi, "epsb"], W=["rstd%d" % xi],
                                   scale=1.0 / D, bias=epsb[:, 0:1])
                            S.recip(rstd[:, xi:xi + 1], rstd[:, xi:xi + 1], R=["rstd%d" % xi], W=["rstd%d" % xi])
                            S.stt(xn[xi][:], ht[hi][:], rstd[:, xi:xi + 1], mb[:, mo, :], ALU.mult, ALU.mult,
                                  R=["ht%d" % hi, "rstd%d" % xi, "mb"], W=["xn%d" % xi])
                            S.tt(ux[xi][:], xn[xi][:], mb[:, mo + 1, :], ALU.add, R=["xn%d" % xi, "mb"], W=["ux%d" % xi])
                            for k in range(8):
                                S.tr(pT[xi][:, k * 128:(k + 1) * 128], ux[xi][:, k * 128:(k + 1) * 128], idb[:],
                                     R=["ux%d" % xi, "idb"], W=["pT%d" % xi])
                            S.copy(uxT[gi][:, :, si * 128:(si + 1) * 128], pT[xi][:].rearrange("p (k n) -> p k n", k=8),
                                   R=["pT%d" % xi], W=["uxT%d" % gi], eng="scalar")
                        uk = "uxT%d" % gi
                        if CUT == 2:
                            return

                        def fm(col):
                            nonlocal pacnt
                            pi = pacnt % 6
                            pacnt += 1
                            for k in range(8):
                                S.mm(pA[pi][:, 0:N], w[:, k, col:col + 128], uxT[gi][:, k, 0:N], start=(k == 0), stop=(k == 7),
                                     R=["win", uk], W=["pA%d" % pi])
                            return pi

                        def fo_store(fi, row):
                            S.dma(PXT[b, row:row + 128, c0:c0 + N], fo[fi][:, 0:N], R=["fo%d" % fi], W=["PXT"], q="gpsimd")

                        for (col, row) in ((C_NAQ, R_NAQ), (C_NAK, R_NAK)):
                            for m in range(4):
                                pi = fm(col + m * 128)
                                fi = focnt % 3
                                focnt += 1
                                if m % 2 == 0:
                                    S.copy(fo[fi][:, 0:N], pA[pi][:, 0:N], R=["pA%d" % pi], W=["fo%d" % fi], eng="scalar")
                                else:
                                    S.copy(fo[fi][:, 0:N], pA[pi][:, 0:N], R=["pA%d" % pi], W=["fo%d" % fi])
                                fo_store(fi, row + m * 128)
                        if CUT == 3:
                            return
                        for m in range(2):
                            pa = fm(C_CVA + m * 128)
                            pg = fm(C_CVG + m * 128)
                            si_ = m
                            S.actf(sg[si_][:, 0:N], pA[pg][:, 0:N], AF.Sigmoid, R=["pA%d" % pg], W=["sg%d" % si_])
                            fi = focnt % 3
                            focnt += 1
                            S.tt(fo[fi][:, 0:N], pA[pa][:, 0:N], sg[si_][:, 0:N], ALU.mult, R=["pA%d" % pa, "sg%d" % si_], W=["fo%d" % fi])
                            fo_store(fi, R_CV + m * 128)
                        if CUT == 4:
                            return
                        for qi, (col, scol, row) in enumerate(((C_RQ, 3072, R_RQ), (C_RK, 3200, R_RK))):
                            p0 = fm(col)
                            p1 = fm(scol)
                            S.tt(r1[qi][:, 0:N], pA[p0][:, 0:N], ropet[gi][:, 2 * qi, 0:N], ALU.mult,
                                 R=["pA%d" % p0, "ropet%d" % gi], W=["r1%d" % qi])
                            S.tt(r2[qi][:, 0:N], pA[p1][:, 0:N], ropet[gi][:, 2 * qi + 1, 0:N], ALU.mult,
                                 R=["pA%d" % p1, "ropet%d" % gi], W=["r2%d" % qi])
                            fi = focnt % 3
                            focnt += 1
                            S.tt(fo[fi][:, 0:N], r1[qi][:, 0:N], r2[qi][:, 0:N], ALU.add, R=["r1%d" % qi, "r2%d" % qi],
                                 W=["fo%d" % fi], eng="gpsimd")
                            fo_store(fi, row)
                        if CUT == 5:
                            return
                        for si, t in enumerate(tiles):
                            ti = (tcnt + si) % 2
                            for (col, ncol, ocol, kind) in ((C_NAV, 512, 0, 0), (C_RV, 512, 512, 1), (C_GB, 256, 1024, 2)):
                                pi = pacnt % 6
                                pacnt += 1
                                for k in range(8):
                                    S.mm(pA[pi][:, 0:ncol], uxT[gi][:, k, si * 128:(si + 1) * 128], w[:, k, col:col + ncol],
                                         start=(k == 0), stop=(k == 7), R=["win", uk], W=["pA%d" % pi])
                                if kind == 0:
                                    S.copy(to[ti][:, 0:512], pA[pi][:, 0:512], R=["pA%d" % pi], W=["to%d" % ti])
                                elif kind == 1:
                                    S.copy(to[ti][:, 512:768], pA[pi][:, 0:256], R=["pA%d" % pi], W=["to%d" % ti], eng="scalar")
                                    S.actf(to[ti][:, 768:1024], pA[pi][:, 256:512], AF.Silu, R=["pA%d" % pi], W=["to%d" % ti])
                                else:
                                    S.actf(to[ti][:, 1024:1280], pA[pi][:, 0:256], AF.Silu, R=["pA%d" % pi], W=["to%d" % ti])
                            S.dma(PXV[b, t * 128:(t + 1) * 128, :], to[ti][:], R=["to%d" % ti], W=["PXV"], q="gpsimd")
                        if CUT == 6:
                            return
                        if (CUT == 7 and g == 3) or (CUT == 8 and g == 4):
                            return
            return f

        def stage_c(l, b):
            def f(stg):
                qT = sb(stg, "qT", [128, 4, NT], BF16)
                kT = sb(stg, "kT", [128, 4, NT], BF16)
                V = sb(stg, "V", [128, NTILE, NH, 65], BF16)
                ET = sb(stg, "ET", [128, NH * 15 * 64], F32)
                ETb = sb(stg, "ETb", [128, NH, 15, 64], BF16)
                PT = sb(stg, "PT", [128, NH, NPAT, 128], BF16)
                Pt = [sb(stg, "Pt%d" % i, [128, 7, 128], BF16) for i in range(3)]
                yc = [sb(stg, "yc%d" % i, [128, 512], BF16) for i in range(2)]
                rec = [sb(stg, "rec%d" % i, [128, 4], F32) for i in range(2)]
                pS = [psf(stg, "pS%d" % i) for i in range(4)]
                pO = [psf(stg, "pO%d" % i) for i in range(2)]
                S.dma(qT[:], PXT[b, R_NAQ:R_NAQ + 512, :].rearrange("(c p) t -> p c t", p=128), R=["PXT"], W=["qT"])
                S.dma(kT[:], PXT[b, R_NAK:R_NAK + 512, :].rearrange("(c p) t -> p c t", p=128), R=["PXT"], W=["kT"])
                S.memset(V[:], 1.0, W=["V"])
                for t0 in range(NTILE):
                    S.dma(V[:, t0, :, 0:64],
                          PXV[b, t0 * 128:(t0 + 1) * 128, 0:512].rearrange("p (h d) -> p h d", d=64),
                          R=["PXV"], W=["V"])
                S.dma(ET[0:64, :], napb[l], W=["ET"])
                S.dma(ET[64:128, :], napb[l], W=["ET"])
                S.actf(ETb[:].rearrange("p h r c -> p (h r c)"), ET[:], AF.Exp, R=["ET"], W=["ETb"])
                S.memset(PT[:], 0.0, W=["PT"])
                ci = 0
                for pid, quad in enumerate(pats):
                    for kr in range(2):
                        for qr in range(2):
                            dr = quad[kr * 2 + qr]
                            if dr < 0:
                                continue
                            eng = ("vector", "gpsimd")[ci % 2]
                            ci += 1
                            S.copy(PT[kr * 64:(kr + 1) * 64, :, pid, qr * 64:(qr + 1) * 64], ETb[kr * 64:(kr + 1) * 64, :, dr, :],
                                   R=["ETb"], W=["PT"], eng=eng)
                qtiles = list(range(2, NTILE)) + ([0, 1] if l == 0 else [])
                iters = []
                for qi_, tq in enumerate(qtiles):
                    if tq >= 2:
                        kts, pids = ptable[tq - 2]
                        ktoks = [kt + 2 for kt in kts] + [0, 1]
                        nl = len(kts)
                        assert pids == list(range(pids[0], pids[0] + nl))
                    else:
                        ktoks, nl, pids = [0, 1], 0, [0]
                    for h in range(NH):
                        iters.append((qi_, tq, h, ktoks, nl, pids[0]))

                def rec_scores(n):
                    qi_, tq, h, ktoks, nl, p0 = iters[n]
                    hp, hc = h % 2, h // 2
                    sl = slice(hp * 64, (hp + 1) * 64)
                    i2 = n % 2
                    for s_, kt in enumerate(ktoks):
                        bank = pS[2 * i2 + s_ // 4]
                        S.mm(bank[:, (s_ % 4) * 128:(s_ % 4 + 1) * 128], kT[sl, hc, kt * 128:(kt + 1) * 128],
                             qT[sl, hc, tq * 128:(tq + 1) * 128], R=["qT", "kT"], W=["pS%d" % (2 * i2 + s_ // 4)])

                def rec_softmax(n):
                    qi_, tq, h, ktoks, nl, p0 = iters[n]
                    i2, i3 = n % 2, n % 3
                    ns = len(ktoks)
                    n0 = min(ns, 4)
                    S.actf(Pt[i3][:, 0:n0, :].rearrange("p s q -> p (s q)"), pS[2 * i2][:, 0:n0 * 128], AF.Exp,
                           R=["pS%d" % (2 * i2)], W=["Pt%d" % i3], scale=0.125)
                    if ns > 4:
                        S.actf(Pt[i3][:, 4:ns, :].rearrange("p s q -> p (s q)"), pS[2 * i2 + 1][:, 0:(ns - 4) * 128], AF.Exp,
                               R=["pS%d" % (2 * i2 + 1)], W=["Pt%d" % i3], scale=0.125)
                    if nl:
                        S.tt(Pt[i3][:, 0:nl, :], Pt[i3][:, 0:nl, :], PT[:, h, p0:p0 + nl, :], ALU.mult,
                             R=["Pt%d" % i3, "PT"], W=["Pt%d" % i3], eng=("vector", "gpsimd")[n % 2])

                def rec_pv(n):
                    qi_, tq, h, ktoks, nl, p0 = iters[n]
                    i3 = n % 3
                    ns = len(ktoks)
                    yi = qi_ % 2
                    og = h // 4
                    hq = h % 4
                    for s_, kt in enumerate(ktoks):
                        S.mm(pO[og][:, hq * 65:(hq + 1) * 65], Pt[i3][:, s_, :], V[:, kt, h, :], start=(s_ == 0), stop=(s_ == ns - 1),
                             R=["Pt%d" % i3, "V"], W=["pO%d" % og])
                    if hq == 3:
                        pv = pO[og][:, 0:260].rearrange("p (h d) -> p h d", d=65)
                        S.recip(rec[og][:].unsqueeze(2), pv[:, :, 64:65], R=["pO%d" % og], W=["rec%d" % og])
                        S.tt(yc[yi][:, (h - 3) * 64:(h + 1) * 64].rearrange("p (h d) -> p h d", d=64), pv[:, :, 0:64],
                             rec[og][:].unsqueeze(2).to_broadcast([128, 4, 64]), ALU.mult,
                             R=["pO%d" % og, "rec%d" % og], W=["yc%d" % yi])
                    if h == NH - 1:
                        S.dma(YC[b, tq * 128:(tq + 1) * 128, 0:512], yc[yi][:], R=["yc%d" % yi], W=["YC"], q="gpsimd")

                rec_scores(0)
                for n in range(len(iters)):
                    rec_softmax(n)
                    if n + 1 < len(iters):
                        rec_scores(n + 1)
                    rec_pv(n)
            return f

        def stage_d(l, b):
            def f(stg):
                ylat = sb(stg, "ylat", [128, 2, T + 32], BF16)
                yctx = sb(stg, "yctx", [128, 2, LC + 32], BF16)
                cw = sb(stg, "cw", [128, 2, 31], F32)
                cpar = sb(stg, "cpar", [128, 3, 2], F32)
                diag = sb(stg, "diag", [128, 2, 31, 128], BF16)
                ones = sb(stg, "ones", [128, 128], F32)
                zc = [sb(stg, "zc%d" % i, [128, 512], F32) for i in range(2)]
                sq = [sb(stg, "sq%d" % i, [128, 512], F32) for i in range(2)]
                mean = sb(stg, "mean", [128, 512], F32)
                var = sb(stg, "var", [128, 512], F32)
                dd = [sb(stg, "dd%d" % i, [128, 512], F32) for i in range(2)]
                yo = [sb(stg, "yo%d" % i, [128, 512], BF16) for i in range(2)]
                pc = [psf(stg, "pc%d" % i) for i in range(4)]
                pm = [psf(stg, "pmn%d" % i) for i in range(2)]
                S.memset(ylat[:], 0.0, W=["ylat"])
                S.memset(yctx[:], 0.0, W=["yctx"])
                S.memset(ones[:], 1.0 / 256, W=["ones"])
                S.dma(ylat[:, :, 15:15 + T], PXT[b, R_CV:R_CV + 256, LC:NT].rearrange("(c p) t -> p c t", p=128), R=["PXT"], W=["ylat"])
                if l == 0:
                    S.dma(yctx[:, :, 15:15 + LC], PXT[b, R_CV:R_CV + 256, 0:LC].rearrange("(c p) t -> p c t", p=128), R=["PXT"], W=["yctx"])
                S.dma(cw[:], convwT[l], W=["cw"])
                S.dma(cpar[:], cpard[l], W=["cpar"])
                for c in range(2):
                    for j in range(31):
                        S.ts(diag[:, c, j, :], id32[:], cw[:, c, j:j + 1], None, ALU.mult, R=["cw", "id32"], W=["diag"],
                             eng=("vector", "gpsimd")[j % 2])
                blocks = [("lat", ylat, tb * 512, 512, LC + tb * 512) for tb in range(4)]
                if l == 0:
                    blocks.append(("ctx", yctx, 0, 256, 0))
                for bi, (_, ybuf, off, N, tok0) in enumerate(blocks):
                    ykey = "ylat" if ybuf is ylat else "yctx"
                    for c in range(2):
                        p = pc[(2 * bi + c) % 4]
                        pk = "pc%d" % ((2 * bi + c) % 4)
                        for j in range(31):
                            S.mm(p[:, 0:N], diag[:, c, j, :], ybuf[:, c, off + j:off + j + N], start=(j == 0), stop=(j == 30),
                                 R=["diag", ykey], W=[pk])
                        S.actf(zc[c][:, 0:N], p[:, 0:N], AF.Identity, R=[pk, "cpar"], W=["zc%d" % c], bias=cpar[:, 0, c:c + 1])
                        S.actf(sq[c][:, 0:N], p[:, 0:N], AF.Square, R=[pk, "cpar"], W=["sq%d" % c], bias=cpar[:, 0, c:c + 1])
                    for c in range(2):
                        S.mm(pm[0][:, 0:N], ones[:], zc[c][:, 0:N], start=(c == 0), stop=(c == 1), R=["ones", "zc%d" % c], W=["pmn0"])
                    for c in range(2):
                        S.mm(pm[1][:, 0:N], ones[:], sq[c][:, 0:N], start=(c == 0), stop=(c == 1), R=["ones", "sq%d" % c], W=["pmn1"])
                    S.copy(mean[:, 0:N], pm[0][:, 0:N], R=["pmn0"], W=["mean"], eng="scalar")
                    S.tt(var[:, 0:N], mean[:, 0:N], mean[:, 0:N], ALU.mult, R=["mean"], W=["var"])
                    S.tt(var[:, 0:N], pm[1][:, 0:N], var[:, 0:N], ALU.subtract, R=["pmn1", "var"], W=["var"])
                    S.actf(var[:, 0:N], var[:, 0:N], AF.Sqrt, R=["var", "epsb"], W=["var"], bias=epsb[:, 0:1])
                    S.recip(var[:, 0:N], var[:, 0:N], R=["var"], W=["var"])
                    for c in range(2):
                        S.tt(dd[c][:, 0:N], zc[c][:, 0:N], mean[:, 0:N], ALU.subtract, R=["zc%d" % c, "mean"], W=["dd%d" % c])
                        S.tt(dd[c][:, 0:N], dd[c][:, 0:N], var[:, 0:N], ALU.mult, R=["dd%d" % c, "var"], W=["dd%d" % c], eng="gpsimd")
                        S.actf(yo[c][:, 0:N], dd[c][:, 0:N], AF.Silu, R=["dd%d" % c, "cpar"], W=["yo%d" % c],
                               scale=cpar[:, 1, c:c + 1], bias=cpar[:, 2, c:c + 1])
                        S.dma(YT[b, c * 128:(c + 1) * 128, tok0:tok0 + N], yo[c][:, 0:N], R=["yo%d" % c], W=["YT"], q="gpsimd")
            return f

        def stage_e(l, b):
            def f(stg):
                qT = sb(stg, "rqT", [32, 4, NT], BF16)
                kT = sb(stg, "rkT", [32, 4, NT], BF16)
                ktm = sb(stg, "ktm", [128, NTILE, 128], BF16)
                V = sb(stg, "rV", [128, NTILE, 768], BF16)
                rc = sb(stg, "rc", [128, 772], F32)
                dec = sb(stg, "dec", [128, 8], F32)
                lg = sb(stg, "lg", [128, 8], F32)
                intra = sb(stg, "intra", [128, 2, 4, 128], F32)
                QD = sb(stg, "QD", [32, 2, 4, 128], F32)
                KD = sb(stg, "KD", [128, 2, 4], F32)
                CD = sb(stg, "CD", [32, 2, 4], F32)
                gnw = sb(stg, "gnw", [128, 256], F32)
                S32 = [sb(stg, "S32_%d" % i, [32, 4, 64], F32) for i in range(2)]
                Sbf = [sb(stg, "Sbf_%d" % i, [32, 4, 64], BF16) for i in range(2)]
                Pm = [sb(stg, "Pm%d" % i, [128, 4, 128], BF16) for i in range(2)]
                qd = [sb(stg, "qd%d" % i, [32, 4, 128], BF16) for i in range(2)]
                kd = [sb(stg, "kd%d" % i, [128, 4, 32], BF16) for i in range(2)]
                Oall = [sb(stg, "Oall%d" % i, [128, NTILE, 256], F32) for i in range(2)]
                sqa = sb(stg, "sqa", [128, NTILE, 256], F32)
                stt_ = [sb(stg, "stt%d" % i, [128, 3, 4 * NTILE], F32) for i in range(2)]
                Yo = sb(stg, "Yo", [128, NTILE, 256], BF16)
                pST = [psf(stg, "pST%d" % i) for i in range(2)]
                pOo = [psf(stg, "pOo%d" % i) for i in range(2)]
                pSs = [psf(stg, "pSs%d" % i) for i in range(2)]
                pK = psb(stg, "pK")
                S.dma(qT[:], PXT[b, R_RQ:R_RQ + 128, :].rearrange("(h d) t -> d h t", d=32), R=["PXT"], W=["rqT"])
                S.dma(kT[:], PXT[b, R_RK:R_RK + 128, :].rearrange("(h d) t -> d h t", d=32), R=["PXT"], W=["rkT"])
                for t0 in range(0, NTILE, 6):
                    S.dma(V[:, t0:t0 + 6, :], PXV[b, t0 * 128:(t0 + 6) * 128, 512:1280].rearrange("(t p) c -> p t c", p=128),
                          R=["PXV"], W=["rV"])
                S.dma(rc[:], retc, W=["rc"])
                S.dma(dec[:], ret_decay[l:l + 1, :].partition_broadcast(128), W=["dec"])
                S.dma(gnw[:], ret_gn_w[l:l + 1, :].partition_broadcast(128), W=["gnw"])
                S.actf(lg[:], dec[:], AF.Exp, R=["dec"], W=["lg"], scale=-float(np.log(2.0)))
                S.ts(lg[:], lg[:], -1.0, 1.0, ALU.mult, ALU.add, R=["lg"], W=["lg"])
                S.actf(lg[:], lg[:], AF.Ln, R=["lg"], W=["lg"])
                Dm = {0: rc[:, 0:128], 1: rc[:, 256:384]}
                Mm = {0: rc[:, 128:256], 1: rc[:, 384:512]}
                R12 = {0: rc[0:32, 512:640], 1: rc[0:32, 640:768]}
                C12 = {0: rc[:, 768:769], 1: rc[:, 769:770]}
                for d_ in range(2):
                    for h in range(4):
                        S.actf(intra[:, d_, h, :], Dm[d_], AF.Exp, R=["rc", "lg"], W=["intra"], scale=lg[:, d_ * 4 + h:d_ * 4 + h + 1])
                        S.tt(intra[:, d_, h, :], intra[:, d_, h, :], Mm[d_], ALU.mult, R=["intra", "rc"], W=["intra"])
                        S.actf(QD[:, d_, h, :], R12[d_], AF.Exp, R=["rc", "lg"], W=["QD"], scale=lg[0:32, d_ * 4 + h:d_ * 4 + h + 1])
                    S.actf(KD[:, d_, :], lg[:, d_ * 4:d_ * 4 + 4], AF.Exp, R=["rc", "lg"], W=["KD"], scale=C12[d_])
                    S.actf(CD[:, d_, :], lg[0:32, d_ * 4:d_ * 4 + 4], AF.Exp, R=["lg"], W=["CD"], scale=128.0)
                for t in range(NTILE):
                    for h in range(4):
                        S.tr(pK[:, h * 32:(h + 1) * 32], kT[:, h, t * 128:(t + 1) * 128], idb[0:32, 0:32], R=["rkT", "idb"], W=["pK"])
                    S.copy(ktm[:, t, :], pK[:, 0:128], R=["pK"], W=["ktm"], eng=("vector", "scalar")[t % 2])
                for d_ in range(2):
                    S.memset(S32[d_][:], 0.0, W=["S32_%d" % d_])
                    S.memset(Sbf[d_][:], 0.0, W=["Sbf_%d" % d_])
                order = {0: list(range(NTILE)), 1: [1, 0] + list(range(NTILE - 1, 1, -1))}
                for step in range(NTILE):
                    for d_ in range(2):
                        c = order[d_][step]
                        cs = slice(c * 128, (c + 1) * 128)
                        D_ = "%d" % d_
                        for h in range(4):
                            S.mm(pST[d_][:, h * 128:(h + 1) * 128], kT[:, h, cs], qT[:, h, cs], R=["rkT", "rqT"], W=["pST" + D_])
                        S.tt(Pm[d_][:], pST[d_][:].rearrange("p (h i) -> p h i", h=4), intra[:, d_, :, :], ALU.mult,
                             R=["pST" + D_, "intra"], W=["Pm" + D_])
                        S.tt(qd[d_][:], qT[:, :, cs], QD[:, d_, :, :], ALU.mult, R=["rqT", "QD"], W=["qd" + D_], eng="gpsimd")
                        S.tt(kd[d_][:], ktm[:, c, :].rearrange("p (h d) -> p h d", h=4),
                             KD[:, d_, :].unsqueeze(2).to_broadcast([128, 4, 32]), ALU.mult, R=["ktm", "KD"], W=["kd" + D_], eng="gpsimd")
                        for h in range(4):
                            S.mm(pOo[d_][:, h * 64:(h + 1) * 64], Pm[d_][:, h, :], V[:, c, h * 64:(h + 1) * 64], start=True, stop=False,
                                 R=["Pm" + D_, "rV"], W=["pOo" + D_])
                            S.mm(pOo[d_][:, h * 64:(h + 1) * 64], qd[d_][:, h, :], Sbf[d_][:, h, :], start=False, stop=True,
                                 R=["qd" + D_, "Sbf_" + D_], W=["pOo" + D_])
                        for h in range(4):
                            S.mm(pSs[d_][0:32, h * 64:(h + 1) * 64], kd[d_][:, h, :], V[:, c, h * 64:(h + 1) * 64],
                                 R=["kd" + D_, "rV"], W=["pSs" + D_])
                        S.tt(S32[d_][:], S32[d_][:], CD[:, d_, :].unsqueeze(2).to_broadcast([32, 4, 64]), ALU.mult,
                             R=["S32_" + D_, "CD"], W=["S32_" + D_])
                        S.tt(S32[d_][:], S32[d_][:], pSs[d_][0:32, 0:256].rearrange("p (h e) -> p h e", h=4), ALU.add,
                             R=["S32_" + D_, "pSs" + D_], W=["S32_" + D_])
                        S.copy(Sbf[d_][:], S32[d_][:], R=["S32_" + D_], W=["Sbf_" + D_])
                        S.copy(Oall[d_][:, c, :], pOo[d_][:, 0:256], R=["pOo" + D_], W=["Oall" + D_], eng="scalar")
                for d_ in range(2):
                    D_ = "%d" % d_
                    ok = "Oall" + D_
                    Of = Oall[d_][:].rearrange("p c f -> p (c f)")
                    O3 = Oall[d_][:].rearrange("p c (h e) -> p (c h) e", e=64)
                    s1, s3 = stt_[d_][:, 0, :], stt_[d_][:, 1, :]
                    sk = "stt" + D_
                    S.actf(sqa[:].rearrange("p c f -> p (c f)"), Of, AF.Square, R=[ok], W=["sqa"])
                    S.rsum(s1, O3, R=[ok], W=[sk])
                    S.rsum(s3, sqa[:].rearrange("p c (h e) -> p (c h) e", e=64), R=["sqa"], W=[sk])
                    S.ts(s1, s1, 1.0 / 64, None, ALU.mult, R=[sk], W=[sk])
                    S.ts(s3, s3, 1.0 / 64, None, ALU.mult, R=[sk], W=[sk])
                    S.tt(stt_[d_][:, 2, :], s1, s1, ALU.mult, R=[sk], W=[sk])
                    S.tt(s3, s3, stt_[d_][:, 2, :], ALU.subtract, R=[sk], W=[sk])
                    S.actf(s3, s3, AF.Sqrt, R=[sk, "epsb"], W=[sk], bias=epsb[:, 0:1])
                    S.recip(s3, s3, R=[sk], W=[sk])
                    S.tt(O3, O3, s1.unsqueeze(2).to_broadcast([128, 4 * NTILE, 64]), ALU.subtract, R=[ok, sk], W=[ok])
                    S.tt(O3, O3, s3.unsqueeze(2).to_broadcast([128, 4 * NTILE, 64]), ALU.mult, R=[ok, sk], W=[ok],
                         eng=("gpsimd", "vector")[d_])
                    S.tt(Oall[d_][:], Oall[d_][:], gnw[:].unsqueeze(1).to_broadcast([128, NTILE, 256]), ALU.mult, R=[ok, "gnw"], W=[ok],
                         eng=("vector", "gpsimd")[d_])
                    S.tt(Oall[d_][:], Oall[d_][:], V[:, :, 256 + 256 * d_:512 + 256 * d_], ALU.mult, R=[ok, "rV"], W=[ok],
                         eng=("gpsimd", "vector")[d_])
                S.tt(Yo[:], Oall[0][:], Oall[1][:], ALU.add, R=["Oall0", "Oall1"], W=["Yo"])
                S.dma(YC[b, :, 512:768].rearrange("(t p) c -> p t c", p=128), Yo[:], R=["Yo"], W=["YC"], q="gpsimd")
            return f

        def stage_f(l, b):
            last = l == 1

            def f(stg):
                wo = sb(stg, "wo", [128, 8, D], BF16)
                mb = sb(stg, "mbF", [128, 6, D], F32)
                ycs = [sb(stg, "ycs%d" % i, [128, 768], BF16) for i in range(2)]
                ycv = [sb(stg, "ycv%d" % i, [128, 2, 128], BF16) for i in range(2)]
                ycT = [sb(stg, "ycT%d" % i, [128, 6, 128], BF16) for i in range(2)]
                ht = [sb(stg, "htF%d" % i, [128, D], F32) for i in range(2)]
                hn = [sb(stg, "hn%d" % i, [128, D], F32) for i in range(2)]
                tmp = sb(stg, "tmpF", [128, D], F32)
                junk = sb(stg, "junkF", [128, D], BF16)
                u32 = [sb(stg, "u32_%d" % i, [128, D], F32) for i in range(2)]
                ubf = [sb(stg, "ubf%d" % i, [128, D], BF16) for i in range(2)]
                uTs = [sb(stg, "uTs%d" % i, [128, 8, 128], BF16) for i in range(2)]
                ss = sb(stg, "ssF", [128, 2], F32)
                rstd = sb(stg, "rstdF", [128, 2], F32)
                pT = [psb(stg, "pTF%d" % i) for i in range(2)]
                pY = [psf(stg, "pY%d" % i) for i in range(4)]
                for k in range(8):
                    S.dma(wo[:, k, :], w_out[l, k * 128:(k + 1) * 128, :], W=["wo"], q="gpsimd")
                for s_, (j, slot) in enumerate(((2, 2), (2, 4), (2, 3), (b, 2), (b, 4), (b, 3))):
                    if last and s_ < 3:
                        continue
                    S.dma(mb[:, s_, :], modbc(None, l, j, slot), R=["MODS"], W=["mbF"])
                if last:
                    wr = sb(stg, "wr", [128, NE, D], F32)
                    rb = sb(stg, "rb", [128, NE], F32)
                    lgt = [sb(stg, "lgt%d" % i, [128, 32], F32) for i in range(2)]
                    for e_ in range(NE):
                        S.dma(wr[:, e_, :], moe_routerT[e_:e_ + 1, :].partition_broadcast(128), W=["wr"])
                    S.dma(rb[:], moe_rb.partition_broadcast(128), W=["rb"])
                tiles = list(range(2, NTILE)) if last else list(range(NTILE))
                junkR = sb(stg, "junkR", [128, D], BF16)

                def front(ti, t):
                    i = ti % 2
                    I_ = "%d" % i
                    mo = 0 if t < 2 else 3
                    tsl = slice(t * 128, (t + 1) * 128)
                    S.dma(ycs[i][:], YC[b, tsl, :], R=["YC"], W=["ycs" + I_])
                    S.dma(ycv[i][:], YT[b, :, tsl].rearrange("(c p) t -> p c t", p=128), R=["YT"], W=["ycv" + I_])
                    S.dma(ht[i][:], hsrc(l, b, t), R=["HB"], W=["htF" + I_])
                    for k in range(6):
                        S.tr(pT[i][:, k * 128:(k + 1) * 128], ycs[i][:, k * 128:(k + 1) * 128], idb[:], R=["ycs" + I_, "idb"], W=["pTF" + I_])
                    S.copy(ycT[i][:].rearrange("p k n -> p (k n)"), pT[i][:, 0:768], R=["pTF" + I_], W=["ycT" + I_], eng="scalar")
                    lhs = [ycT[i][:, 0, :], ycT[i][:, 1, :], ycT[i][:, 2, :], ycT[i][:, 3, :], ycv[i][:, 0, :], ycv[i][:, 1, :],
                           ycT[i][:, 4, :], ycT[i][:, 5, :]]
                    for nb in range(2):
                        pk = "pY%d" % (2 * i + nb)
                        for k in range(8):
                            S.mm(pY[2 * i + nb][:], lhs[k], wo[:, k, nb * 512:(nb + 1) * 512], start=(k == 0), stop=(k == 7),
                                 R=["ycT" + I_, "ycv" + I_, "wo"], W=[pk])
                        hs = slice(nb * 512, (nb + 1) * 512)
                        S.tt(tmp[:, hs], pY[2 * i + nb][:], mb[:, mo, hs], ALU.mult, R=[pk, "mbF"], W=["tmpF%d" % nb])
                        S.tt(hn[i][:, hs], ht[i][:, hs], tmp[:, hs], ALU.add, R=["htF" + I_, "tmpF%d" % nb], W=["hn" + I_], eng="gpsimd")
                    S.dma(HA[b, tsl, :], hn[i][:], R=["hn" + I_], W=["HA"], q="gpsimd")
                    S.actf(junk[:], hn[i][:], AF.Square, R=["hn" + I_], W=["junkF", "ssF" + I_], accum_out=ss[:, i:i + 1])
                    S.actf(rstd[:, i:i + 1], ss[:, i:i + 1], AF.Sqrt, R=["ssF" + I_, "epsb"], W=["rstdF" + I_], scale=1.0 / D, bias=epsb[:, 0:1])
                    S.recip(rstd[:, i:i + 1], rstd[:, i:i + 1], R=["rstdF" + I_], W=["rstdF" + I_])
                    S.stt(u32[i][:], hn[i][:], rstd[:, i:i + 1], mb[:, mo + 1, :], ALU.mult, ALU.mult,
                          R=["hn" + I_, "rstdF" + I_, "mbF"], W=["u32_" + I_])
                    S.tt(u32[i][:], u32[i][:], mb[:, mo + 2, :], ALU.add, R=["u32_" + I_, "mbF"], W=["u32_" + I_])
                    S.copy(ubf[i][:], u32[i][:], R=["u32_" + I_], W=["ubf" + I_], eng="scalar")

                def back(ti, t):
                    i = ti % 2
                    I_ = "%d" % i
                    mo = 0 if t < 2 else 3
                    tsl = slice(t * 128, (t + 1) * 128)
                    for k in range(8):
                        S.tr(pT[i][:, k * 128:(k + 1) * 128], ubf[i][:, k * 128:(k + 1) * 128], idb[:], R=["ubf" + I_, "idb"], W=["pTF" + I_])
                    S.copy(uTs[i][:].rearrange("p k n -> p (k n)"), pT[i][:], R=["pTF" + I_], W=["uTs" + I_])
                    S.dma(UT[b, :, tsl].rearrange("(k p) t -> p k t", p=128), uTs[i][:], R=["uTs" + I_], W=["UT"], q="gpsimd")
                    if last:
                        lg_ = lgt[i]
                        lk = "lgt" + I_
                        for e_ in range(NE):
                            S.stt(junkR[:], u32[i][:], 1.0, wr[:, e_, :], ALU.mult, ALU.mult, R=["u32_" + I_, "wr"],
                                  W=["junkR", lk], accum_out=lg_[:, e_:e_ + 1])
                        S.tt(lg_[:, 0:8], lg_[:, 0:8], rb[:], ALU.add, R=[lk, "rb"], W=[lk])
                        S.op("vector", lambda e, a=lg_: e.max(out=a[:, 8:16], in_=a[:, 0:8]), R=[lk], W=[lk])
                        S.ts(lg_[:, 16:24], lg_[:, 0:8], lg_[:, 8:9], None, ALU.is_equal, R=[lk], W=[lk])
                        S.ts(lg_[:, 24:32], lg_[:, 0:8], lg_[:, 9:10], None, ALU.is_equal, R=[lk], W=[lk])
                        S.tt(lg_[:, 10:11], lg_[:, 8:9], lg_[:, 9:10], ALU.subtract, R=[lk], W=[lk])
                        S.actf(lg_[:, 11:12], lg_[:, 10:11], AF.Sigmoid, R=[lk], W=[lk])
                        S.actf(lg_[:, 12:13], lg_[:, 10:11], AF.Sigmoid, R=[lk], W=[lk], scale=-1.0)
                        S.ts(lg_[:, 16:24], lg_[:, 16:24], lg_[:, 11:12], None, ALU.mult, R=[lk], W=[lk])
                        S.stt(lg_[:, 0:8], lg_[:, 24:32], lg_[:, 12:13], lg_[:, 16:24], ALU.mult, ALU.add, R=[lk], W=[lk])
                        S.dma(COMB[b, tsl, :], lg_[:, 0:8], R=[lk], W=["COMB"], q="gpsimd")

                segs = [list(enumerate(tiles))[j:j + 6] for j in range(0, len(tiles), 6)]
                for si_, seg in enumerate(segs):
                    if si_ > 0:
                        yield
                    front(*seg[0])
                    for j in range(len(seg)):
                        if j + 1 < len(seg):
                            front(*seg[j + 1])
                        back(*seg[j])
            return f

        def stage_g(l, b):
            last = l == 1

            def f(stg):
                t0 = 2 if last else 0
                ntile = NTILE - t0
                ntok = ntile * 128
                tok0 = t0 * 128
                nexp = NE if last else 1
                dff = D_FFE if last else D_FF
                uT = sb(stg, "uT", [128, 8, ntok], BF16)
                yacc = sb(stg, "yacc", [128, ntile, D], F32)
                wg = [sb(stg, "wg%d" % i, [128, 8, 512], BF16) for i in range(2)]
                wu = [sb(stg, "wu%d" % i, [128, 8, 512], BF16) for i in range(2)]
                wd = [sb(stg, "wd%d" % i, [128, 4, D], BF16) for i in range(2)]
                act = [sb(stg, "act%d" % i, [128, 4, 512], BF16) for i in range(2)]
                sg = [sb(stg, "sgG%d" % i, [128, 512], F32) for i in range(2)]
                comb = sb(stg, "comb", [128, ntile, NE], F32)
                pG = [psf(stg, "pG%d" % i) for i in range(2)]
                pU = [psf(stg, "pU%d" % i) for i in range(2)]
                pD = [psf(stg, "pD%d" % i) for i in range(4)]
                for k in range(8):
                    S.dma(uT[:, k, :], UT[b, k * 128:(k + 1) * 128, tok0:NT], R=["UT"], W=["uT"])
                if last:
                    S.dma(comb[:], COMB[b, tok0:NT, :].rearrange("(t p) e -> p t e", p=128), R=["COMB"], W=["comb"])
                S.memset(yacc[:], 0.0, W=["yacc"])
                groups = []
                for e_ in range(nexp):
                    for f0 in range(0, dff, 512):
                        groups.append((e_, f0, min(512, dff - f0)))
                tgs = [(s0, min(512, ntok - s0)) for s0 in range(0, ntok, 512)]
                cnt = {"gu": 0, "pd": 0, "sg": 0, "act": 0}

                def rec_gu(u):
                    gi, e_, f0, fw, s0, N, ai = u
                    wi = gi % 2
                    W_ = "%d" % wi
                    A_ = "%d" % ai
                    for c in range(fw // 128):
                        pi = cnt["gu"] % 2
                        cnt["gu"] += 1
                        for k in range(8):
                            S.mm(pG[pi][:, 0:N], wg[wi][:, k, c * 128:(c + 1) * 128], uT[:, k, s0:s0 + N], start=(k == 0), stop=(k == 7),
                                 R=["wg" + W_, "uT"], W=["pG%d" % pi])
                        for k in range(8):
                            S.mm(pU[pi][:, 0:N], wu[wi][:, k, c * 128:(c + 1) * 128], uT[:, k, s0:s0 + N], start=(k == 0), stop=(k == 7),
                                 R=["wu" + W_, "uT"], W=["pU%d" % pi])
                        si = cnt["sg"] % 2
                        cnt["sg"] += 1
                        S.actf(sg[si][:, 0:N], pG[pi][:, 0:N], AF.Silu, R=["pG%d" % pi], W=["sgG%d" % si])
                        S.tt(act[ai][:, c, 0:N], pU[pi][:, 0:N], sg[si][:, 0:N], ALU.mult, R=["pU%d" % pi, "sgG%d" % si], W=["act" + A_])

                def rec_down(u):
                    gi, e_, f0, fw, s0, N, ai = u
                    wi = gi % 2
                    W_ = "%d" % wi
                    A_ = "%d" % ai
                    nch = fw // 128
                    for s_ in range(N // 128):
                        tl = s0 // 128 + s_
                        for nb in range(2):
                            di = cnt["pd"] % 4
                            cnt["pd"] += 1
                            for c in range(nch):
                                S.mm(pD[di][:], act[ai][:, c, s_ * 128:(s_ + 1) * 128], wd[wi][:, c, nb * 512:(nb + 1) * 512],
                                     start=(c == 0), stop=(c == nch - 1), R=["act" + A_, "wd" + W_], W=["pD%d" % di])
                            ya = yacc[:, tl, nb * 512:(nb + 1) * 512]
                            yk = "yacc%d_%d" % (tl, nb)
                            sc = comb[:, tl, e_:e_ + 1] if last else 1.0
                            S.stt(ya, pD[di][:], sc, ya, ALU.mult, ALU.add, R=["pD%d" % di, "comb", "yacc", yk], W=[yk])

                def rec_wload(gi, e_, f0, fw):
                    wi = gi % 2
                    W_ = "%d" % wi
                    nch = fw // 128
                    gsrc = (moe_wg[e_] if last else ffn_wg[0])
                    usrc = (moe_wu[e_] if last else ffn_wu[0])
                    dsrc = (moe_wd[e_] if last else ffn_wd[0])
                    S.dma(wg[wi][:, :, 0:fw], gsrc[:, f0:f0 + fw].rearrange("(k p) n -> p k n", p=128), W=["wg" + W_], q="gpsimd")
                    S.dma(wu[wi][:, :, 0:fw], usrc[:, f0:f0 + fw].rearrange("(k p) n -> p k n", p=128), W=["wu" + W_], q="gpsimd")
                    S.dma(wd[wi][:, 0:nch, :], dsrc[f0:f0 + fw, :].rearrange("(c p) n -> p c n", p=128), W=["wd" + W_], q="gpsimd")

                for e_ in range(nexp):
                    if e_ > 0:
                        yield
                    units = []
                    egroups = [(gi, g) for gi, g in enumerate(groups) if g[0] == e_]
                    for gi, (_, f0, fw) in egroups:
                        for (s0, N) in tgs:
                            units.append((gi, e_, f0, fw, s0, N, cnt["act"] % 2))
                            cnt["act"] += 1
                    loaded = set()

                    def ensure(u):
                        if u[0] not in loaded:
                            loaded.add(u[0])
                            rec_wload(u[0], u[1], u[2], u[3])

                    ensure(units[0])
                    rec_gu(units[0])
                    for n, u in enumerate(units):
                        if n + 1 < len(units):
                            ensure(units[n + 1])
                            rec_gu(units[n + 1])
                        rec_down(u)
                yield
                g2 = sb(stg, "g2", [128, 2, D], F32)
                S.dma(g2[:, 1, :], modbc(None, l, b, 5), R=["MODS"], W=["g2"])
                if not last:
                    S.dma(g2[:, 0, :], modbc(None, l, 2, 5), R=["MODS"], W=["g2"])
                else:
                    S.dma(g2[:, 0, :], final_w.partition_broadcast(128), W=["g2"])
                hh = [sb(stg, "hh%d" % i, [128, D], F32) for i in range(2)]
                ssb = sb(stg, "ssG", [128, 2], F32)
                rsb = sb(stg, "rsG", [128, 2], F32)
                junk = sb(stg, "junkG", [128, D], BF16)
                for tl in range(ntile):
                    t = t0 + tl
                    i = tl % 2
                    I_ = "%d" % i
                    tsl = slice(t * 128, (t + 1) * 128)
                    ykeys = ["yacc", "yacc%d_0" % tl, "yacc%d_1" % tl]
                    S.dma(hh[i][:], HA[b, tsl, :], R=["HA"], W=["hh" + I_])
                    gsel = 1 if (t >= 2) else 0
                    S.tt(yacc[:, tl, :], yacc[:, tl, :], g2[:, gsel if not last else 1, :], ALU.mult, R=ykeys + ["g2"], W=ykeys[1:])
                    S.tt(hh[i][:], hh[i][:], yacc[:, tl, :], ALU.add, R=ykeys + ["hh" + I_], W=["hh" + I_], eng="gpsimd")
                    if not last:
                        S.dma(HB[b, tsl, :], hh[i][:], R=["hh" + I_], W=["HB"], q="gpsimd")
                    else:
                        S.actf(junk[:], hh[i][:], AF.Square, R=["hh" + I_], W=["junkG", "ssG" + I_], accum_out=ssb[:, i:i + 1])
                        S.actf(rsb[:, i:i + 1], ssb[:, i:i + 1], AF.Sqrt, R=["ssG" + I_, "epsb"], W=["rsG" + I_], scale=1.0 / D, bias=epsb[:, 0:1])
                        S.recip(rsb[:, i:i + 1], rsb[:, i:i + 1], R=["rsG" + I_], W=["rsG" + I_])
                        S.stt(hh[i][:], hh[i][:], rsb[:, i:i + 1], g2[:, 0, :], ALU.mult, ALU.mult, R=["hh" + I_, "rsG" + I_, "g2"], W=["hh" + I_])
                        S.dma(out[b, (t - 2) * 128:(t - 1) * 128, :], hh[i][:], R=["hh" + I_], W=["out"], q="gpsimd")
            return f

        prog = []
        for l in range(2):
            if P1:
                prog.append(stage_b(l, (0, 1)))
                for b in range(2):
                    prog.append(stage_c(l, b))
                    prog.append(stage_d(l, b))
                    prog.append(stage_e(l, b))
                for b in range(2):
                    prog.append(stage_f(l, b))
            for b in range(2):
                if (l == 0 and P1) or (l == 1 and P2 and b in bs):
                    prog.append(stage_g(l, b))
        for i, fn in enumerate(prog):
            if i >= upto:
                break
            stage(fn)
    return nc


def host_consts(na_rpb):
    ident = np.eye(128, dtype=np.float32)
    t = np.arange(T)
    row = (t // GRID_W).astype(np.float32)
    col = (t % GRID_W).astype(np.float32)
    inv = (np.float32(10000.0) ** (-np.arange(0, 16, 2, dtype=np.float32) / np.float32(16))).astype(np.float32)
    ang = np.concatenate([row[:, None] * inv, col[:, None] * inv], axis=-1).astype(np.float32)
    cos, sin = np.cos(ang).astype(np.float32), np.sin(ang).astype(np.float32)
    cos2 = np.ones((32, NT), np.float32)
    sinS = np.zeros((32, NT), np.float32)
    cos2[0:16, LC:] = cos.T
    cos2[16:32, LC:] = cos.T
    sinS[0:16, LC:] = -sin.T
    sinS[16:32, LC:] = sin.T
    ks = np.float32(32 ** -0.5)
    rope = np.stack([np.tile(cos2, (4, 1)), np.tile(sinS, (4, 1)), np.tile(cos2, (4, 1)) * ks, np.tile(sinS, (4, 1)) * ks]).astype(np.float32)
    j = np.arange(128)[:, None].astype(np.float32)
    i = np.arange(128)[None, :].astype(np.float32)
    retc = np.zeros((128, 772), np.float32)
    retc[:, 0:128] = np.maximum(i - j, 0)
    retc[:, 128:256] = (i >= j)
    retc[:, 256:384] = np.maximum(j - i, 0)
    retc[:, 384:512] = (j >= i)
    retc[:, 512:640] = i + 1.0
    retc[:, 640:768] = 128.0 - i
    retc[:, 768] = 127.0 - j[:, 0]
    retc[:, 769] = j[:, 0]
    kc = np.arange(64)[:, None]
    qc = np.arange(64)[None, :]
    w0 = np.clip(qc - 8, 0, 48)
    ok = (kc >= w0) & (kc < w0 + 16)
    dc = np.clip(kc - qc + 15, 0, 30)
    g = na_rpb[:, :, :, dc]
    g = np.where(ok[None, None, None], g, np.float32(-30000.0)).astype(np.float32)
    napb = np.ascontiguousarray(g.transpose(0, 3, 1, 2, 4)).reshape(2, 64, NH * 15 * 64)
    return ident, rope, retc, napb


_CACHE = {}
LAUNCH2 = ((0,), (1,))
SINGLE = True


def kernel(x, c, ctx, c_ctx, w_mod, b_mod, norm1_w, norm2_w, w_in, w_out, na_rpb, conv_w, conv_b,
           conv_ln_w, conv_ln_b, ret_decay, ret_gn_w, ffn_w_gate, ffn_w_up, ffn_w_down,
           moe_router, moe_router_b, moe_w_gate, moe_w_up, moe_w_down, final_norm_w):
    f = lambda a: np.ascontiguousarray(np.asarray(a, dtype=np.float32))
    ident, rope, retc, napb = host_consts(f(na_rpb))
    shared = {
        "w_mod": f(w_mod), "b_mod": f(b_mod), "norm1_w": f(norm1_w), "norm2_w": f(norm2_w), "w_in": f(w_in), "w_out": f(w_out),
        "convwT": np.ascontiguousarray(f(conv_w).reshape(2, 31, 2, 128).transpose(0, 3, 2, 1)),
        "cpard": np.ascontiguousarray(np.stack([f(conv_b), f(conv_ln_w), f(conv_ln_b)], axis=1).reshape(2, 3, 2, 128).transpose(0, 3, 1, 2)),
        "ret_decay": f(ret_decay).reshape(2, 8), "ret_gn_w": f(ret_gn_w),
        "ffn_w_gate": f(ffn_w_gate), "ffn_w_up": f(ffn_w_up), "ffn_w_down": f(ffn_w_down),
        "moe_routerT": np.ascontiguousarray(f(moe_router)[0].T), "moe_router_b": f(moe_router_b).reshape(1, NE),
        "moe_w_gate": f(moe_w_gate)[0], "moe_w_up": f(moe_w_up)[0], "moe_w_down": f(moe_w_down)[0],
        "final_norm_w": f(final_norm_w).reshape(1, D),
        "ident": ident, "rope": rope, "retc": retc, "napb": napb,
    }
    x = f(x); c = f(c); ctx = f(ctx); c_ctx = f(c_ctx)
    p2names = ("moe_w_gate", "moe_w_up", "moe_w_down", "final_norm_w")
    in1 = []
    for i in range(8):
        m = {k: v for k, v in shared.items() if k not in p2names}
        m["x2"] = x[2 * i:2 * i + 2]
        m["ctx2"] = ctx[2 * i:2 * i + 2]
        cv = np.concatenate([c[2 * i:2 * i + 2], c_ctx[None, :]], axis=0)
        m["cvecT"] = np.ascontiguousarray(cv.reshape(3, 8, 128).transpose(2, 1, 0))
        in1.append(m)
    if SINGLE:
        for i in range(8):
            for k in p2names:
                in1[i][k] = shared[k]
        if "nc" not in _CACHE:
            _CACHE["nc"] = build_program(part=0)
        r0 = run_bass_kernel_spmd(_CACHE["nc"], in1, core_ids=list(range(8))).results
        return np.concatenate([r["out"] for r in r0], axis=0)
    if "nc1" not in _CACHE:
        _CACHE["nc1"] = build_program(part=1)
        _CACHE["nc2"] = [build_program(part=2, bs=bs_) for bs_ in LAUNCH2]
    r1 = run_bass_kernel_spmd(_CACHE["nc1"], in1, core_ids=list(range(8))).results
    outs = [np.zeros((2, T, D), np.float32) for _ in range(8)]
    for bs_, nc2 in zip(LAUNCH2, _CACHE["nc2"]):
        in2 = []
        for i in range(8):
            m = {k: shared[k] for k in p2names}
            for k in ("MODS", "HA", "UT", "COMB"):
                m[k] = r1[i][k]
            in2.append(m)
        r2 = run_bass_kernel_spmd(nc2, in2, core_ids=list(range(8))).results
        for i in range(8):
            for b in bs_:
                outs[i][b] = r2[i]["out"][b]
    return np.concatenate(outs, axis=0)
```

```python
import contextlib
import numpy as np
CUT = 0
import concourse.bass as bass
import concourse.mybir as mybir
from concourse.bass_utils import run_bass_kernel_spmd

F32 = mybir.dt.float32
BF16 = mybir.dt.bfloat16
ALU = mybir.AluOpType
AF = mybir.ActivationFunctionType
AX = mybir.AxisListType

ENGS = ("tensor", "vector", "scalar", "gpsimd", "sync")
NDMA = 12


class Op:
    __slots__ = ("eng", "fn", "dma", "deps", "signal", "sem", "val", "idx")

    def __init__(self, eng, fn, dma):
        self.eng, self.fn, self.dma = eng, fn, dma
        self.deps = []
        self.signal = dma
        self.sem = None
        self.val = None


class Sched:
    def __init__(self, nc, stack):
        self.nc = nc
        self.csem = {e: stack.enter_context(nc.semaphore("cs_" + e)) for e in ENGS}
        self.ccnt = {e: 0 for e in ENGS}
        self.dsem = {e: [stack.enter_context(nc.semaphore("ds_%s%d" % (e, i))) for i in range(NDMA)]
                     for e in ("sync", "gpsimd")}
        self.dcnt = {e: [0] * NDMA for e in self.dsem}
        self.drr = {e: 0 for e in self.dsem}
        self.known = {e: {} for e in ENGS}
        self.same_engine_sync = True
        self.reset()

    def reset(self):
        self.ops = {e: [] for e in ENGS}
        self.lastw = {}
        self.readers = {}
        self.allops = []

    def op(self, eng, fn, R=(), W=(), dma=False):
        o = Op(eng, fn, dma)
        deps = []
        for r in R:
            w = self.lastw.get(r)
            if w is not None:
                deps.append(w)
        for w_ in W:
            w = self.lastw.get(w_)
            if w is not None:
                deps.append(w)
            deps.extend(self.readers.get(w_, ()))
        seen = set()
        for d in deps:
            if id(d) in seen or d is o:
                continue
            seen.add(id(d))
            if d.eng == eng and not d.dma and not dma:
                if eng == "tensor" or not self.same_engine_sync:
                    continue
            o.deps.append(d)
        for r in R:
            lst = self.readers.setdefault(r, [])
            if not dma:
                lst[:] = [x for x in lst if not (x.eng == eng and not x.dma)]
            lst.append(o)
        for w_ in W:
            self.lastw[w_] = o
            self.readers[w_] = []
        self.ops[eng].append(o)
        self.allops.append(o)
        return o

    def mm(self, out, lhsT, rhs, start=True, stop=True, R=(), W=()):
        return self.op("tensor", lambda e: e.matmul(out, lhsT=lhsT, rhs=rhs, start=start, stop=stop), R, W)

    def tr(self, out, in_, ident, R=(), W=()):
        return self.op("tensor", lambda e: e.transpose(out=out, in_=in_, identity=ident), R, W)

    def actf(self, out, in_, func, R=(), W=(), **kw):
        return self.op("scalar", lambda e: e.activation(out=out, in_=in_, func=func, **kw), R, W)

    def tt(self, out, in0, in1, op, R=(), W=(), eng="vector"):
        return self.op(eng, lambda e: e.tensor_tensor(out=out, in0=in0, in1=in1, op=op), R, W)

    def ts(self, out, in0, s1, s2, op0, op1=None, R=(), W=(), eng="vector", **kw):
        if op1 is None:
            return self.op(eng, lambda e: e.tensor_scalar(out=out, in0=in0, scalar1=s1, scalar2=None, op0=op0, **kw), R, W)
        return self.op(eng, lambda e: e.tensor_scalar(out=out, in0=in0, scalar1=s1, scalar2=s2, op0=op0, op1=op1, **kw), R, W)

    def stt(self, out, in0, scalar, in1, op0, op1, R=(), W=(), **kw):
        return self.op("vector", lambda e: e.scalar_tensor_tensor(out=out, in0=in0, scalar=scalar, in1=in1,
                                                                  op0=op0, op1=op1, **kw), R, W)

    def copy(self, out, in_, R=(), W=(), eng="vector"):
        if eng == "scalar":
            return self.op(eng, lambda e: e.activation(out=out, in_=in_, func=AF.Copy), R, W)
        return self.op(eng, lambda e: e.tensor_copy(out=out, in_=in_), R, W)

    def recip(self, out, in_, R=(), W=()):
        return self.op("vector", lambda e: e.reciprocal(out=out, in_=in_), R, W)

    def rsum(self, out, in_, R=(), W=()):
        return self.op("vector", lambda e: e.reduce_sum(out=out, in_=in_, axis=AX.X), R, W)

    def memset(self, ap, val, W=(), eng="gpsimd"):
        return self.op(eng, lambda e: e.memset(ap, val), (), W)

    def dma(self, out, in_, R=(), W=(), q="sync", **kw):
        return self.op(q, lambda e: e.dma_start(out=out, in_=in_, **kw), R, W, dma=True)

    def emit(self, block):
        for o in self.allops:
            for d in o.deps:
                d.signal = True
        for e in ENGS:
            for o in self.ops[e]:
                if o.dma:
                    i = self.drr[e]
                    self.drr[e] = (i + 1) % NDMA
                    prev = self.dcnt[e][i]
                    self.dcnt[e][i] = prev + 16
                    o.sem = self.dsem[e][i]
                    o.val = prev + 16
                    o.idx = (e, i, prev)
                elif o.signal:
                    self.ccnt[e] += 1
                    o.sem = self.csem[e]
                    o.val = self.ccnt[e]
        sched = self

        def make(e):
            def body(eng):
                known = sched.known[e]
                for o in sched.ops[e]:
                    waits = {}
                    for d in o.deps:
                        k = id(d.sem)
                        if k not in waits or waits[k][1] < d.val:
                            waits[k] = (d.sem, d.val)
                    if o.dma and o.idx[2] > 0:
                        s = sched.dsem[o.idx[0]][o.idx[1]]
                        k = id(s)
                        if k not in waits or waits[k][1] < o.idx[2]:
                            waits[k] = (s, o.idx[2])
                    for k, (s, v) in waits.items():
                        if known.get(k, 0) >= v:
                            continue
                        eng.wait_ge(s, v)
                        known[k] = v
                    inst = o.fn(eng)
                    if o.signal:
                        inst.then_inc(o.sem, 16 if o.dma else 1)
                if e in sched.dsem:
                    for i, s in enumerate(sched.dsem[e]):
                        v = sched.dcnt[e][i]
                        if v > 0 and known.get(id(s), 0) < v:
                            eng.wait_ge(s, v)
                            known[id(s)] = v
            return body

        for e in ENGS:
            if self.ops[e]:
                getattr(block, e)(make(e))
        self.reset()


D = 1024
T = 2048
LC = 256
NT = T + LC
NTILE = NT // 128
GRID_W = 64
EPS = 1e-6
NH = 8
D_FF = 2816
D_FFE = 3584
NE = 8
C_NAQ, C_NAK, C_NAV, C_CVA, C_CVG, C_RQ, C_RK, C_RV, C_GF, C_GB = 0, 512, 1024, 1536, 1792, 2048, 2176, 2304, 2560, 2816
R_NAQ, R_NAK, R_CV, R_RQ, R_RK, PXT_ROWS = 0, 512, 1024, 1280, 1408, 1536
PXV_COLS = 1280


def na_patterns():
    pats, index, table = [], {}, {}
    for a in range(16):
        kts, pids = [], []
        for kt in range(16):
            quad = []
            anyv = False
            for kr in range(2):
                for qr in range(2):
                    krow, qrow = 2 * kt + kr, 2 * a + qr
                    st = min(max(qrow - 4, 0), 24)
                    if st <= krow <= st + 7:
                        quad.append(krow - qrow + 7)
                        anyv = True
                    else:
                        quad.append(-1)
            if not anyv:
                continue
            atype = a if a in (0, 1, 14, 15) else -1
            key = (atype, kt - a) + tuple(quad)
            if key not in index:
                index[key] = len(pats)
                pats.append(tuple(quad))
            kts.append(kt)
            pids.append(index[key])
        table[a] = (kts, pids)
    return pats, table


def build_program(debug=False, upto=99, part=0, bs=(0, 1)):
    nc = bass.Bass("TRN2", target_bir_lowering=False)
    dk = "ExternalOutput" if debug else "Internal"

    def din(name, shape, dt=F32):
        return nc.dram_tensor(name, list(shape), dt, kind="ExternalInput").ap()

    def dscr(name, shape, dt, handoff=False):
        kind = dk
        if handoff and part == 1:
            kind = "ExternalOutput"
        if handoff and part == 2:
            kind = "ExternalInput"
        return nc.dram_tensor(name, list(shape), dt, kind=kind).ap()

    P1 = part in (0, 1)
    P2 = part in (0, 2)
    _din = din

    def din1(name, shape, dt=F32):
        return _din(name, shape, dt) if P1 else None

    def din2(name, shape, dt=F32):
        return _din(name, shape, dt) if P2 else None

    x2 = din1("x2", [2, T, D]); ctx2 = din1("ctx2", [2, LC, D]); cvecT = din1("cvecT", [128, 8, 3])
    w_mod = din1("w_mod", [2, D, 6 * D]); b_mod = din1("b_mod", [2, 6 * D])
    norm1_w = din1("norm1_w", [2, D]); norm2_w = din1("norm2_w", [2, D])
    w_in = din1("w_in", [2, D, 3072]); w_out = din1("w_out", [2, D, D])
    convwT = din1("convwT", [2, 128, 2, 31]); cpard = din1("cpard", [2, 128, 3, 2])
    ret_decay = din1("ret_decay", [2, 8]); ret_gn_w = din1("ret_gn_w", [2, 256])
    ffn_wg = din1("ffn_w_gate", [1, D, D_FF]); ffn_wu = din1("ffn_w_up", [1, D, D_FF]); ffn_wd = din1("ffn_w_down", [1, D_FF, D])
    moe_routerT = din1("moe_routerT", [NE, D]); moe_rb = din1("moe_router_b", [1, NE])
    moe_wg = din2("moe_w_gate", [NE, D, D_FFE]); moe_wu = din2("moe_w_up", [NE, D, D_FFE]); moe_wd = din2("moe_w_down", [NE, D_FFE, D])
    final_w = din2("final_norm_w", [1, D])
    identd = din1("ident", [128, 128]); rope = din1("rope", [4, 128, NT]); retc = din1("retc", [128, 772])
    napb = din1("napb", [2, 64, NH * 15 * 64])
    out = nc.dram_tensor("out", [2, T, D], F32, kind="ExternalOutput").ap() if P2 else None

    MODS = dscr("MODS", [2, 3, 6 * D], F32, True)
    PXT = dscr("PXT", [2, PXT_ROWS, NT], BF16)
    PXV = dscr("PXV", [2, NT, PXV_COLS], BF16)
    YC = dscr("YC", [2, NT, 768], BF16)
    YT = dscr("YT", [2, 256, NT], BF16)
    HA = dscr("HA", [2, NT, D], F32, True)
    HB = dscr("HB", [2, NT, D], F32)
    UT = dscr("UT", [2, D, NT], BF16, True)
    COMB = dscr("COMB", [2, NT, NE], F32, True)

    pats, ptable = na_patterns()
    NPAT = len(pats)

    with contextlib.ExitStack() as glob:
        S = Sched(nc, glob)
        idb = glob.enter_context(nc.sbuf_tensor("idb", [128, 128], BF16))
        id32 = glob.enter_context(nc.sbuf_tensor("id32", [128, 128], F32))

        def emit_block():
            if not S.allops:
                return
            with nc.Block() as blk:
                S.emit(blk)

        def stage(fn):
            with contextlib.ExitStack() as stg:
                r = fn(stg)
                if r is not None:
                    for _ in r:
                        emit_block()
                emit_block()

        uid = [0]

        def un(name):
            uid[0] += 1
            return "%s_%d" % (name, uid[0])

        def sb(stg, name, shape, dt):
            return stg.enter_context(nc.sbuf_tensor(un(name), list(shape), dt))

        def psf(stg, name, n=512):
            return stg.enter_context(nc.psum_tensor(un(name), [128, n], F32))

        def psb(stg, name, n=1024):
            return stg.enter_context(nc.psum_tensor(un(name), [128, n], BF16))

        def hsrc(l, b, t):
            if l == 0:
                return ctx2[b, t * 128:(t + 1) * 128, :] if t < 2 else x2[b, (t - 2) * 128:(t - 1) * 128, :]
            return HB[b, t * 128:(t + 1) * 128, :]

        def rms_rstd(stg_bufs, h_ap, hkey, junk, ss, rstd, tag):
            S.actf(junk, h_ap, AF.Square, R=[hkey], W=["junk" + tag, "ss" + tag], accum_out=ss)
            S.actf(rstd, ss, AF.Sqrt, R=["ss" + tag], W=["rstd" + tag], scale=1.0 / D, bias=epsb[:, 0:1])
            S.recip(rstd, rstd, R=["rstd" + tag], W=["rstd" + tag])

        epsb = glob.enter_context(nc.sbuf_tensor("epsb", [128, 1], F32))

        def stage_a(stg):
            S.dma(idb[:], identd, W=["idb"], q="gpsimd")
            S.dma(id32[:], identd, W=["id32"])
            S.memset(epsb[:], EPS, W=["epsb"])
            cT = sb(stg, "cT", [128, 8, 3], F32)
            sT = sb(stg, "sT", [128, 8, 3], F32)
            S.dma(cT[:], cvecT, W=["cT"])
            S.actf(sT[:], cT[:], AF.Silu, R=["cT"], W=["sT"])
            wb = [sb(stg, "wmb%d" % i, [128, 8, 512], F32) for i in range(2)]
            modrow = sb(stg, "modrow", [3, 6 * D], F32)
            bm3 = sb(stg, "bm3", [3, 6 * D], F32)
            nw3 = sb(stg, "nw3", [3, 2, D], F32)
            pm = [psf(stg, "pm%d" % i) for i in range(2)]
            it = 0
            for l in range(2):
                S.dma(bm3[:], b_mod[l:l + 1, :].partition_broadcast(3), R=["bm3"], W=["bm3"])
                S.dma(nw3[:, 0, :], norm1_w[l:l + 1, :].partition_broadcast(3), W=["nw3"])
                S.dma(nw3[:, 1, :], norm2_w[l:l + 1, :].partition_broadcast(3), W=["nw3"])
                for nb in range(12):
                    i = it % 2
                    it += 1
                    S.dma(wb[i][:], w_mod[l, :, nb * 512:(nb + 1) * 512].rearrange("(k p) n -> p k n", p=128),
                          W=["wmb%d" % i])
                    for k in range(8):
                        S.mm(pm[i][0:3, :], sT[:, k, :], wb[i][:, k, :], start=(k == 0), stop=(k == 7),
                             R=["sT", "wmb%d" % i], W=["pm%d" % i])
                    S.copy(modrow[:, nb * 512:(nb + 1) * 512], pm[i][0:3, :], R=["pm%d" % i], W=["modrow"], eng="scalar")
                S.tt(modrow[:], modrow[:], bm3[:], ALU.add, R=["modrow", "bm3"], W=["modrow"])
                for s_, wsel in ((1, 0), (4, 1)):
                    seg = modrow[:, s_ * D:(s_ + 1) * D]
                    S.stt(seg, seg, 1.0, nw3[:, wsel, :], ALU.add, ALU.mult, R=["modrow", "nw3"], W=["modrow"])
                S.dma(MODS[l], modrow[:], R=["modrow"], W=["MODS"])

        def stage_a2(stg):
            S.memset(epsb[:], EPS, W=["epsb"])

        stage(stage_a if P1 else stage_a2)

        def modbc(dst, l, j, s):
            return MODS[l, j:j + 1, s * D:(s + 1) * D].partition_broadcast(128)

        def stage_b(l, bsel):
            def f(stg):
                w = sb(stg, "win", [128, 8, 3328], BF16)
                for k in range(8):
                    S.dma(w[:, k, 0:3072], w_in[l, k * 128:(k + 1) * 128, :], W=["win"], q="gpsimd")
                for (src, dst) in ((C_RQ, 3072), (C_RK, 3200)):
                    sv = w[:, :, src:src + 128].rearrange("p k (h two i) -> p k h two i", two=2, i=16)
                    dv = w[:, :, dst:dst + 128].rearrange("p k (h two i) -> p k h two i", two=2, i=16)
                    S.copy(dv[:, :, :, 0, :], sv[:, :, :, 1, :], R=["win"], W=["win"])
                    S.copy(dv[:, :, :, 1, :], sv[:, :, :, 0, :], R=["win"], W=["win"], eng="gpsimd")
                if CUT == 1:
                    return
                mb = sb(stg, "mb", [128, 4, D], F32)
                ropet = [sb(stg, "ropet%d" % i, [128, 4, 512], F32) for i in range(2)]
                ht = [sb(stg, "ht%d" % i, [128, D], F32) for i in range(3)]
                junk = sb(stg, "junk", [128, D], BF16)
                xn = [sb(stg, "xn%d" % i, [128, D], F32) for i in range(2)]
                ux = [sb(stg, "ux%d" % i, [128, D], BF16) for i in range(2)]
                uxT = [sb(stg, "uxT%d" % i, [128, 8, 512], BF16) for i in range(2)]
                ss = sb(stg, "ss", [128, 4], F32)
                rstd = sb(stg, "rstd", [128, 4], F32)
                fo = [sb(stg, "fo%d" % i, [128, 512], BF16) for i in range(3)]
                sg = [sb(stg, "sg%d" % i, [128, 512], F32) for i in range(2)]
                r1 = [sb(stg, "r1%d" % i, [128, 512], F32) for i in range(2)]
                r2 = [sb(stg, "r2%d" % i, [128, 512], F32) for i in range(2)]
                to = [sb(stg, "to%d" % i, [128, PXV_COLS], BF16) for i in range(2)]
                pT = [psb(stg, "pT%d" % i) for i in range(2)]
                pA = [psf(stg, "pA%d" % i) for i in range(6)]
                tcnt = 0
                gcnt = 0
                focnt = 0
                pacnt = 0
                for b in bsel:
                    for s_, (j, slot) in enumerate(((2, 1), (2, 0), (b, 1), (b, 0))):
                        S.dma(mb[:, s_, :], modbc(None, l, j, slot), R=["MODS"], W=["mb"])
                    for g in range(5):
                        tiles = list(range(4 * g, min(4 * g + 4, NTILE)))
                        N = 128 * len(tiles)
                        gi = gcnt % 2
                        gcnt += 1
                        c0 = 4 * g * 128
                        S.dma(ropet[gi][:, :, 0:N], rope[:, :, c0:c0 + N].rearrange("f p n -> p f n"), W=["ropet%d" % gi])
                        for si, t in enumerate(tiles):
                            hi = tcnt % 3
                            xi = tcnt % 2
                            tcnt += 1
                            mo = 0 if t < 2 else 2
                            S.dma(ht[hi][:], hsrc(l, b, t), R=["HB"], W=["ht%d" % hi])
                            S.actf(junk[:], ht[hi][:], AF.Square, R=["ht%d" % hi], W=["junk", "ss%d" % xi], accum_out=ss[:, xi:xi + 1])
                            S.actf(rstd[:, xi:xi + 1], ss[:, xi:xi + 1], AF.Sqrt, R=["ss%d" % xi, "epsb"], W=["rstd%d" % xi],
                                   scale=1.0 / D, bias=epsb[:, 0:1])
                            S.recip(rstd[:, xi:xi + 1], rstd[:, xi:xi + 1], R=["rstd%d" % xi], W=["rstd%d" % xi])
                            S.stt(xn[xi][:], ht[hi][:], rstd[:, xi:xi + 1], mb[:, mo, :], ALU.mult, ALU.mult,
                                  R=["ht%d" % hi, "rstd%d" % xi, "mb"], W=["xn%d" % xi])
                            S.tt(ux[xi][:], xn[xi][:], mb[:, mo + 1, :], ALU.add, R=["xn%d" % xi, "mb"], W=["ux%d" % xi])
                            for k in range(8):
                                S.tr(pT[xi][:, k * 128:(k + 1) * 128], ux[xi][:, k * 128:(k + 1) * 128], idb[:],
                                     R=["ux%d" % xi, "idb"], W=["pT%d" % xi])
                            S.copy(uxT[gi][:, :, si * 128:(si + 1) * 128], pT[xi][:].rearrange("p (k n) -> p k n", k=8),
                                   R=["pT%d" % xi], W=["uxT%d" % gi], eng="scalar")
                        uk = "uxT%d" % gi
                        if CUT == 2:
                            return

                        def fm(col):
                            nonlocal pacnt
                            pi = pacnt % 6
                            pacnt += 1
                            for k in range(8):
                                S.mm(pA[pi][:, 0:N], w[:, k, col:col + 128], uxT[gi][:, k, 0:N], start=(k == 0), stop=(k == 7),
                                     R=["win", uk], W=["pA%d" % pi])
                            return pi

                        def fo_store(fi, row):
                            S.dma(PXT[b, row:row + 128, c0:c0 + N], fo[fi][:, 0:N], R=["fo%d" % fi], W=["PXT"], q="gpsimd")

                        for (col, row) in ((C_NAQ, R_NAQ), (C_NAK, R_NAK)):
                            for m in range(4):
                                pi = fm(col + m * 128)
                                fi = focnt % 3
                                focnt += 1
                                if m % 2 == 0:
                                    S.copy(fo[fi][:, 0:N], pA[pi][:, 0:N], R=["pA%d" % pi], W=["fo%d" % fi], eng="scalar")
                                else:
                                    S.copy(fo[fi][:, 0:N], pA[pi][:, 0:N], R=["pA%d" % pi], W=["fo%d" % fi])
                                fo_store(fi, row + m * 128)
                        if CUT == 3:
                            return
                        for m in range(2):
                            pa = fm(C_CVA + m * 128)
                            pg = fm(C_CVG + m * 128)
                            si_ = m
                            S.actf(sg[si_][:, 0:N], pA[pg][:, 0:N], AF.Sigmoid, R=["pA%d" % pg], W=["sg%d" % si_])
                            fi = focnt % 3
                            focnt += 1
                            S.tt(fo[fi][:, 0:N], pA[pa][:, 0:N], sg[si_][:, 0:N], ALU.mult, R=["pA%d" % pa, "sg%d" % si_], W=["fo%d" % fi])
                            fo_store(fi, R_CV + m * 128)
                        if CUT == 4:
                            return
                        for qi, (col, scol, row) in enumerate(((C_RQ, 3072, R_RQ), (C_RK, 3200, R_RK))):
                            p0 = fm(col)
                            p1 = fm(scol)
                            S.tt(r1[qi][:, 0:N], pA[p0][:, 0:N], ropet[gi][:, 2 * qi, 0:N], ALU.mult,
                                 R=["pA%d" % p0, "ropet%d" % gi], W=["r1%d" % qi])
                            S.tt(r2[qi][:, 0:N], pA[p1][:, 0:N], ropet[gi][:, 2 * qi + 1, 0:N], ALU.mult,
                                 R=["pA%d" % p1, "ropet%d" % gi], W=["r2%d" % qi])
                            fi = focnt % 3
                            focnt += 1
                            S.tt(fo[fi][:, 0:N], r1[qi][:, 0:N], r2[qi][:, 0:N], ALU.add, R=["r1%d" % qi, "r2%d" % qi],
                                 W=["fo%d" % fi], eng="gpsimd")
                            fo_store(fi, row)
                        if CUT == 5:
                            return
                        for si, t in enumerate(tiles):
                            ti = (tcnt + si) % 2
                            for (col, ncol, ocol, kind) in ((C_NAV, 512, 0, 0), (C_RV, 512, 512, 1), (C_GB, 256, 1024, 2)):
                                pi = pacnt % 6
                                pacnt += 1
                                for k in range(8):
                                    S.mm(pA[pi][:, 0:ncol], uxT[gi][:, k, si * 128:(si + 1) * 128], w[:, k, col:col + ncol],
                                         start=(k == 0), stop=(k == 7), R=["win", uk], W=["pA%d" % pi])
                                if kind == 0:
                                    S.copy(to[ti][:, 0:512], pA[pi][:, 0:512], R=["pA%d" % pi], W=["to%d" % ti])
                                elif kind == 1:
                                    S.copy(to[ti][:, 512:768], pA[pi][:, 0:256], R=["pA%d" % pi], W=["to%d" % ti], eng="scalar")
                                    S.actf(to[ti][:, 768:1024], pA[pi][:, 256:512], AF.Silu, R=["pA%d" % pi], W=["to%d" % ti])
                                else:
                                    S.actf(to[ti][:, 1024:1280], pA[pi][:, 0:256], AF.Silu, R=["pA%d" % pi], W=["to%d" % ti])
                            S.dma(PXV[b, t * 128:(t + 1) * 128, :], to[ti][:], R=["to%d" % ti], W=["PXV"], q="gpsimd")
                        if CUT == 6:
                            return
                        if (CUT == 7 and g == 3) or (CUT == 8 and g == 4):
                            return
            return f

        def stage_c(l, b):
            def f(stg):
                qT = sb(stg, "qT", [128, 4, NT], BF16)
                kT = sb(stg, "kT", [128, 4, NT], BF16)
                V = sb(stg, "V", [128, NTILE, NH, 65], BF16)
                ET = sb(stg, "ET", [128, NH * 15 * 64], F32)
                ETb = sb(stg, "ETb", [128, NH, 15, 64], BF16)
                PT = sb(stg, "PT", [128, NH, NPAT, 128], BF16)
                Pt = [sb(stg, "Pt%d" % i, [128, 7, 128], BF16) for i in range(3)]
                yc = [sb(stg, "yc%d" % i, [128, 512], BF16) for i in range(2)]
                rec = [sb(stg, "rec%d" % i, [128, 4], F32) for i in range(2)]
                pS = [psf(stg, "pS%d" % i) for i in range(4)]
                pO = [psf(stg, "pO%d" % i) for i in range(2)]
                S.dma(qT[:], PXT[b, R_NAQ:R_NAQ + 512, :].rearrange("(c p) t -> p c t", p=128), R=["PXT"], W=["qT"])
                S.dma(kT[:], PXT[b, R_NAK:R_NAK + 512, :].rearrange("(c p) t -> p c t", p=128), R=["PXT"], W=["kT"])
                S.memset(V[:], 1.0, W=["V"])
                for t0 in range(NTILE):
                    S.dma(V[:, t0, :, 0:64],
                          PXV[b, t0 * 128:(t0 + 1) * 128, 0:512].rearrange("p (h d) -> p h d", d=64),
                          R=["PXV"], W=["V"])
                S.dma(ET[0:64, :], napb[l], W=["ET"])
                S.dma(ET[64:128, :], napb[l], W=["ET"])
                S.actf(ETb[:].rearrange("p h r c -> p (h r c)"), ET[:], AF.Exp, R=["ET"], W=["ETb"])
                S.memset(PT[:], 0.0, W=["PT"])
                ci = 0
                for pid, quad in enumerate(pats):
                    for kr in range(2):
                        for qr in range(2):
                            dr = quad[kr * 2 + qr]
                            if dr < 0:
                                continue
                            eng = ("vector", "gpsimd")[ci % 2]
                            ci += 1
                            S.copy(PT[kr * 64:(kr + 1) * 64, :, pid, qr * 64:(qr + 1) * 64], ETb[kr * 64:(kr + 1) * 64, :, dr, :],
                                   R=["ETb"], W=["PT"], eng=eng)
                qtiles = list(range(2, NTILE)) + ([0, 1] if l == 0 else [])
                iters = []
                for qi_, tq in enumerate(qtiles):
                    if tq >= 2:
                        kts, pids = ptable[tq - 2]
                        ktoks = [kt + 2 for kt in kts] + [0, 1]
                        nl = len(kts)
                        assert pids == list(range(pids[0], pids[0] + nl))
                    else:
                        ktoks, nl, pids = [0, 1], 0, [0]
                    for h in range(NH):
                        iters.append((qi_, tq, h, ktoks, nl, pids[0]))

                def rec_scores(n):
                    qi_, tq, h, ktoks, nl, p0 = iters[n]
                    hp, hc = h % 2, h // 2
                    sl = slice(hp * 64, (hp + 1) * 64)
                    i2 = n % 2
                    for s_, kt in enumerate(ktoks):
                        bank = pS[2 * i2 + s_ // 4]
                        S.mm(bank[:, (s_ % 4) * 128:(s_ % 4 + 1) * 128], kT[sl, hc, kt * 128:(kt + 1) * 128],
                             qT[sl, hc, tq * 128:(tq + 1) * 128], R=["qT", "kT"], W=["pS%d" % (2 * i2 + s_ // 4)])

                def rec_softmax(n):
                    qi_, tq, h, ktoks, nl, p0 = iters[n]
                    i2, i3 = n % 2, n % 3
                    ns = len(ktoks)
                    n0 = min(ns, 4)
                    S.actf(Pt[i3][:, 0:n0, :].rearrange("p s q -> p (s q)"), pS[2 * i2][:, 0:n0 * 128], AF.Exp,
                           R=["pS%d" % (2 * i2)], W=["Pt%d" % i3], scale=0.125)
                    if ns > 4:
                        S.actf(Pt[i3][:, 4:ns, :].rearrange("p s q -> p (s q)"), pS[2 * i2 + 1][:, 0:(ns - 4) * 128], AF.Exp,
                               R=["pS%d" % (2 * i2 + 1)], W=["Pt%d" % i3], scale=0.125)
                    if nl:
                        S.tt(Pt[i3][:, 0:nl, :], Pt[i3][:, 0:nl, :], PT[:, h, p0:p0 + nl, :], ALU.mult,
                             R=["Pt%d" % i3, "PT"], W=["Pt%d" % i3], eng=("vector", "gpsimd")[n % 2])

                def rec_pv(n):
                    qi_, tq, h, ktoks, nl, p0 = iters[n]
                    i3 = n % 3
                    ns = len(ktoks)
                    yi = qi_ % 2
                    og = h // 4
                    hq = h % 4
                    for s_, kt in enumerate(ktoks):
                        S.mm(pO[og][:, hq * 65:(hq + 1) * 65], Pt[i3][:, s_, :], V[:, kt, h, :], start=(s_ == 0), stop=(s_ == ns - 1),
                             R=["Pt%d" % i3, "V"], W=["pO%d" % og])
                    if hq == 3:
                        pv = pO[og][:, 0:260].rearrange("p (h d) -> p h d", d=65)
                        S.recip(rec[og][:].unsqueeze(2), pv[:, :, 64:65], R=["pO%d" % og], W=["rec%d" % og])
                        S.tt(yc[yi][:, (h - 3) * 64:(h + 1) * 64].rearrange("p (h d) -> p h d", d=64), pv[:, :, 0:64],
                             rec[og][:].unsqueeze(2).to_broadcast([128, 4, 64]), ALU.mult,
                             R=["pO%d" % og, "rec%d" % og], W=["yc%d" % yi])
                    if h == NH - 1:
                        S.dma(YC[b, tq * 128:(tq + 1) * 128, 0:512], yc[yi][:], R=["yc%d" % yi], W=["YC"], q="gpsimd")

                rec_scores(0)
                for n in range(len(iters)):
                    rec_softmax(n)
                    if n + 1 < len(iters):
                        rec_scores(n + 1)
                    rec_pv(n)
            return f

        def stage_d(l, b):
            def f(stg):
                ylat = sb(stg, "ylat", [128, 2, T + 32], BF16)
                yctx = sb(stg, "yctx", [128, 2, LC + 32], BF16)
                cw = sb(stg, "cw", [128, 2, 31], F32)
                cpar = sb(stg, "cpar", [128, 3, 2], F32)
                diag = sb(stg, "diag", [128, 2, 31, 128], BF16)
                ones = sb(stg, "ones", [128, 128], F32)
                zc = [sb(stg, "zc%d" % i, [128, 512], F32) for i in range(2)]
                sq = [sb(stg, "sq%d" % i, [128, 512], F32) for i in range(2)]
                mean = sb(stg, "mean", [128, 512], F32)
                var = sb(stg, "var", [128, 512], F32)
                dd = [sb(stg, "dd%d" % i, [128, 512], F32) for i in range(2)]
                yo = [sb(stg, "yo%d" % i, [128, 512], BF16) for i in range(2)]
                pc = [psf(stg, "pc%d" % i) for i in range(4)]
                pm = [psf(stg, "pmn%d" % i) for i in range(2)]
                S.memset(ylat[:], 0.0, W=["ylat"])
                S.memset(yctx[:], 0.0, W=["yctx"])
                S.memset(ones[:], 1.0 / 256, W=["ones"])
                S.dma(ylat[:, :, 15:15 + T], PXT[b, R_CV:R_CV + 256, LC:NT].rearrange("(c p) t -> p c t", p=128), R=["PXT"], W=["ylat"])
                if l == 0:
                    S.dma(yctx[:, :, 15:15 + LC], PXT[b, R_CV:R_CV + 256, 0:LC].rearrange("(c p) t -> p c t", p=128), R=["PXT"], W=["yctx"])
                S.dma(cw[:], convwT[l], W=["cw"])
                S.dma(cpar[:], cpard[l], W=["cpar"])
                for c in range(2):
                    for j in range(31):
                        S.ts(diag[:, c, j, :], id32[:], cw[:, c, j:j + 1], None, ALU.mult, R=["cw", "id32"], W=["diag"],
                             eng=("vector", "gpsimd")[j % 2])
                blocks = [("lat", ylat, tb * 512, 512, LC + tb * 512) for tb in range(4)]
                if l == 0:
                    blocks.append(("ctx", yctx, 0, 256, 0))
                for bi, (_, ybuf, off, N, tok0) in enumerate(blocks):
                    ykey = "ylat" if ybuf is ylat else "yctx"
                    for c in range(2):
                        p = pc[(2 * bi + c) % 4]
                        pk = "pc%d" % ((2 * bi + c) % 4)
                        for j in range(31):
                            S.mm(p[:, 0:N], diag[:, c, j, :], ybuf[:, c, off + j:off + j + N], start=(j == 0), stop=(j == 30),
                                 R=["diag", ykey], W=[pk])
                        S.actf(zc[c][:, 0:N], p[:, 0:N], AF.Identity, R=[pk, "cpar"], W=["zc%d" % c], bias=cpar[:, 0, c:c + 1])
                        S.actf(sq[c][:, 0:N], p[:, 0:N], AF.Square, R=[pk, "cpar"], W=["sq%d" % c], bias=cpar[:, 0, c:c + 1])
                    for c in range(2):
                        S.mm(pm[0][:, 0:N], ones[:], zc[c][:, 0:N], start=(c == 0), stop=(c == 1), R=["ones", "zc%d" % c], W=["pmn0"])
                    for c in range(2):
                        S.mm(pm[1][:, 0:N], ones[:], sq[c][:, 0:N], start=(c == 0), stop=(c == 1), R=["ones", "sq%d" % c], W=["pmn1"])
                    S.copy(mean[:, 0:N], pm[0][:, 0:N], R=["pmn0"], W=["mean"], eng="scalar")
                    S.tt(var[:, 0:N], mean[:, 0:N], mean[:, 0:N], ALU.mult, R=["mean"], W=["var"])
                    S.tt(var[:, 0:N], pm[1][:, 0:N], var[:, 0:N], ALU.subtract, R=["pmn1", "var"], W=["var"])
                    S.actf(var[:, 0:N], var[:, 0:N], AF.Sqrt, R=["var", "epsb"], W=["var"], bias=epsb[:, 0:1])
                    S.recip(var[:, 0:N], var[:, 0:N], R=["var"], W=["var"])
                    for c in range(2):
                        S.tt(dd[c][:, 0:N], zc[c][:, 0:N], mean[:, 0:N], ALU.subtract, R=["zc%d" % c, "mean"], W=["dd%d" % c])
                        S.tt(dd[c][:, 0:N], dd[c][:, 0:N], var[:, 0:N], ALU.mult, R=["dd%d" % c, "var"], W=["dd%d" % c], eng="gpsimd")
                        S.actf(yo[c][:, 0:N], dd[c][:, 0:N], AF.Silu, R=["dd%d" % c, "cpar"], W=["yo%d" % c],
                               scale=cpar[:, 1, c:c + 1], bias=cpar[:, 2, c:c + 1])
                        S.dma(YT[b, c * 128:(c + 1) * 128, tok0:tok0 + N], yo[c][:, 0:N], R=["yo%d" % c], W=["YT"], q="gpsimd")
            return f

        def stage_e(l, b):
            def f(stg):
                qT = sb(stg, "rqT", [32, 4, NT], BF16)
                kT = sb(stg, "rkT", [32, 4, NT], BF16)
                ktm = sb(stg, "ktm", [128, NTILE, 128], BF16)
                V = sb(stg, "rV", [128, NTILE, 768], BF16)
                rc = sb(stg, "rc", [128, 772], F32)
                dec = sb(stg, "dec", [128, 8], F32)
                lg = sb(stg, "lg", [128, 8], F32)
                intra = sb(stg, "intra", [128, 2, 4, 128], F32)
                QD = sb(stg, "QD", [32, 2, 4, 128], F32)
                KD = sb(stg, "KD", [128, 2, 4], F32)
                CD = sb(stg, "CD", [32, 2, 4], F32)
                gnw = sb(stg, "gnw", [128, 256], F32)
                S32 = [sb(stg, "S32_%d" % i, [32, 4, 64], F32) for i in range(2)]
                Sbf = [sb(stg, "Sbf_%d" % i, [32, 4, 64], BF16) for i in range(2)]
                Pm = [sb(stg, "Pm%d" % i, [128, 4, 128], BF16) for i in range(2)]
                qd = [sb(stg, "qd%d" % i, [32, 4, 128], BF16) for i in range(2)]
                kd = [sb(stg, "kd%d" % i, [128, 4, 32], BF16) for i in range(2)]
                Oall = [sb(stg, "Oall%d" % i, [128, NTILE, 256], F32) for i in range(2)]
                sqa = sb(stg, "sqa", [128, NTILE, 256], F32)
                stt_ = [sb(stg, "stt%d" % i, [128, 3, 4 * NTILE], F32) for i in range(2)]
                Yo = sb(stg, "Yo", [128, NTILE, 256], BF16)
                pST = [psf(stg, "pST%d" % i) for i in range(2)]
                pOo = [psf(stg, "pOo%d" % i) for i in range(2)]
                pSs = [psf(stg, "pSs%d" % i) for i in range(2)]
                pK = psb(stg, "pK")
                S.dma(qT[:], PXT[b, R_RQ:R_RQ + 128, :].rearrange("(h d) t -> d h t", d=32), R=["PXT"], W=["rqT"])
                S.dma(kT[:], PXT[b, R_RK:R_RK + 128, :].rearrange("(h d) t -> d h t", d=32), R=["PXT"], W=["rkT"])
                for t0 in range(0, NTILE, 6):
                    S.dma(V[:, t0:t0 + 6, :], PXV[b, t0 * 128:(t0 + 6) * 128, 512:1280].rearrange("(t p) c -> p t c", p=128),
                          R=["PXV"], W=["rV"])
                S.dma(rc[:], retc, W=["rc"])
                S.dma(dec[:], ret_decay[l:l + 1, :].partition_broadcast(128), W=["dec"])
                S.dma(gnw[:], ret_gn_w[l:l + 1, :].partition_broadcast(128), W=["gnw"])
                S.actf(lg[:], dec[:], AF.Exp, R=["dec"], W=["lg"], scale=-float(np.log(2.0)))
                S.ts(lg[:], lg[:], -1.0, 1.0, ALU.mult, ALU.add, R=["lg"], W=["lg"])
                S.actf(lg[:], lg[:], AF.Ln, R=["lg"], W=["lg"])
                Dm = {0: rc[:, 0:128], 1: rc[:, 256:384]}
                Mm = {0: rc[:, 128:256], 1: rc[:, 384:512]}
                R12 = {0: rc[0:32, 512:640], 1: rc[0:32, 640:768]}
                C12 = {0: rc[:, 768:769], 1: rc[:, 769:770]}
                for d_ in range(2):
                    for h in range(4):
                        S.actf(intra[:, d_, h, :], Dm[d_], AF.Exp, R=["rc", "lg"], W=["intra"], scale=lg[:, d_ * 4 + h:d_ * 4 + h + 1])
                        S.tt(intra[:, d_, h, :], intra[:, d_, h, :], Mm[d_], ALU.mult, R=["intra", "rc"], W=["intra"])
                        S.actf(QD[:, d_, h, :], R12[d_], AF.Exp, R=["rc", "lg"], W=["QD"], scale=lg[0:32, d_ * 4 + h:d_ * 4 + h + 1])
                    S.actf(KD[:, d_, :], lg[:, d_ * 4:d_ * 4 + 4], AF.Exp, R=["rc", "lg"], W=["KD"], scale=C12[d_])
                    S.actf(CD[:, d_, :], lg[0:32, d_ * 4:d_ * 4 + 4], AF.Exp, R=["lg"], W=["CD"], scale=128.0)
                for t in range(NTILE):
                    for h in range(4):
                        S.tr(pK[:, h * 32:(h + 1) * 32], kT[:, h, t * 128:(t + 1) * 128], idb[0:32, 0:32], R=["rkT", "idb"], W=["pK"])
                    S.copy(ktm[:, t, :], pK[:, 0:128], R=["pK"], W=["ktm"], eng=("vector", "scalar")[t % 2])
                for d_ in range(2):
                    S.memset(S32[d_][:], 0.0, W=["S32_%d" % d_])
                    S.memset(Sbf[d_][:], 0.0, W=["Sbf_%d" % d_])
                order = {0: list(range(NTILE)), 1: [1, 0] + list(range(NTILE - 1, 1, -1))}
                for step in range(NTILE):
                    for d_ in range(2):
                        c = order[d_][step]
                        cs = slice(c * 128, (c + 1) * 128)
                        D_ = "%d" % d_
                        for h in range(4):
                            S.mm(pST[d_][:, h * 128:(h + 1) * 128], kT[:, h, cs], qT[:, h, cs], R=["rkT", "rqT"], W=["pST" + D_])
                        S.tt(Pm[d_][:], pST[d_][:].rearrange("p (h i) -> p h i", h=4), intra[:, d_, :, :], ALU.mult,
                             R=["pST" + D_, "intra"], W=["Pm" + D_])
                        S.tt(qd[d_][:], qT[:, :, cs], QD[:, d_, :, :], ALU.mult, R=["rqT", "QD"], W=["qd" + D_], eng="gpsimd")
                        S.tt(kd[d_][:], ktm[:, c, :].rearrange("p (h d) -> p h d", h=4),
                             KD[:, d_, :].unsqueeze(2).to_broadcast([128, 4, 32]), ALU.mult, R=["ktm", "KD"], W=["kd" + D_], eng="gpsimd")
                        for h in range(4):
                            S.mm(pOo[d_][:, h * 64:(h + 1) * 64], Pm[d_][:, h, :], V[:, c, h * 64:(h + 1) * 64], start=True, stop=False,
                                 R=["Pm" + D_, "rV"], W=["pOo" + D_])
                            S.mm(pOo[d_][:, h * 64:(h + 1) * 64], qd[d_][:, h, :], Sbf[d_][:, h, :], start=False, stop=True,
                                 R=["qd" + D_, "Sbf_" + D_], W=["pOo" + D_])
                        for h in range(4):
                            S.mm(pSs[d_][0:32, h * 64:(h + 1) * 64], kd[d_][:, h, :], V[:, c, h * 64:(h + 1) * 64],
                                 R=["kd" + D_, "rV"], W=["pSs" + D_])
                        S.tt(S32[d_][:], S32[d_][:], CD[:, d_, :].unsqueeze(2).to_broadcast([32, 4, 64]), ALU.mult,
                             R=["S32_" + D_, "CD"], W=["S32_" + D_])
                        S.tt(S32[d_][:], S32[d_][:], pSs[d_][0:32, 0:256].rearrange("p (h e) -> p h e", h=4), ALU.add,
                             R=["S32_" + D_, "pSs" + D_], W=["S32_" + D_])
                        S.copy(Sbf[d_][:], S32[d_][:], R=["S32_" + D_], W=["Sbf_" + D_])
                        S.copy(Oall[d_][:, c, :], pOo[d_][:, 0:256], R=["pOo" + D_], W=["Oall" + D_], eng="scalar")
                for d_ in range(2):
                    D_ = "%d" % d_
                    ok = "Oall" + D_
                    Of = Oall[d_][:].rearrange("p c f -> p (c f)")
                    O3 = Oall[d_][:].rearrange("p c (h e) -> p (c h) e", e=64)
                    s1, s3 = stt_[d_][:, 0, :], stt_[d_][:, 1, :]
                    sk = "stt" + D_
                    S.actf(sqa[:].rearrange("p c f -> p (c f)"), Of, AF.Square, R=[ok], W=["sqa"])
                    S.rsum(s1, O3, R=[ok], W=[sk])
                    S.rsum(s3, sqa[:].rearrange("p c (h e) -> p (c h) e", e=64), R=["sqa"], W=[sk])
                    S.ts(s1, s1, 1.0 / 64, None, ALU.mult, R=[sk], W=[sk])
                    S.ts(s3, s3, 1.0 / 64, None, ALU.mult, R=[sk], W=[sk])
                    S.tt(stt_[d_][:, 2, :], s1, s1, ALU.mult, R=[sk], W=[sk])
                    S.tt(s3, s3, stt_[d_][:, 2, :], ALU.subtract, R=[sk], W=[sk])
                    S.actf(s3, s3, AF.Sqrt, R=[sk, "epsb"], W=[sk], bias=epsb[:, 0:1])
                    S.recip(s3, s3, R=[sk], W=[sk])
                    S.tt(O3, O3, s1.unsqueeze(2).to_broadcast([128, 4 * NTILE, 64]), ALU.subtract, R=[ok, sk], W=[ok])
                    S.tt(O3, O3, s3.unsqueeze(2).to_broadcast([128, 4 * NTILE, 64]), ALU.mult, R=[ok, sk], W=[ok],
                         eng=("gpsimd", "vector")[d_])
                    S.tt(Oall[d_][:], Oall[d_][:], gnw[:].unsqueeze(1).to_broadcast([128, NTILE, 256]), ALU.mult, R=[ok, "gnw"], W=[ok],
                         eng=("vector", "gpsimd")[d_])
                    S.tt(Oall[d_][:], Oall[d_][:], V[:, :, 256 + 256 * d_:512 + 256 * d_], ALU.mult, R=[ok, "rV"], W=[ok],
                         eng=("gpsimd", "vector")[d_])
                S.tt(Yo[:], Oall[0][:], Oall[1][:], ALU.add, R=["Oall0", "Oall1"], W=["Yo"])
                S.dma(YC[b, :, 512:768].rearrange("(t p) c -> p t c", p=128), Yo[:], R=["Yo"], W=["YC"], q="gpsimd")
            return f

        def stage_f(l, b):
            last = l == 1

            def f(stg):
                wo = sb(stg, "wo", [128, 8, D], BF16)
                mb = sb(stg, "mbF", [128, 6, D], F32)
                ycs = [sb(stg, "ycs%d" % i, [128, 768], BF16) for i in range(2)]
                ycv = [sb(stg, "ycv%d" % i, [128, 2, 128], BF16) for i in range(2)]
                ycT = [sb(stg, "ycT%d" % i, [128, 6, 128], BF16) for i in range(2)]
                ht = [sb(stg, "htF%d" % i, [128, D], F32) for i in range(2)]
                hn = [sb(stg, "hn%d" % i, [128, D], F32) for i in range(2)]
                tmp = sb(stg, "tmpF", [128, D], F32)
                junk = sb(stg, "junkF", [128, D], BF16)
                u32 = [sb(stg, "u32_%d" % i, [128, D], F32) for i in range(2)]
                ubf = [sb(stg, "ubf%d" % i, [128, D], BF16) for i in range(2)]
                uTs = [sb(stg, "uTs%d" % i, [128, 8, 128], BF16) for i in range(2)]
                ss = sb(stg, "ssF", [128, 2], F32)
                rstd = sb(stg, "rstdF", [128, 2], F32)
                pT = [psb(stg, "pTF%d" % i) for i in range(2)]
                pY = [psf(stg, "pY%d" % i) for i in range(4)]
                for k in range(8):
                    S.dma(wo[:, k, :], w_out[l, k * 128:(k + 1) * 128, :], W=["wo"], q="gpsimd")
                for s_, (j, slot) in enumerate(((2, 2), (2, 4), (2, 3), (b, 2), (b, 4), (b, 3))):
                    if last and s_ < 3:
                        continue
                    S.dma(mb[:, s_, :], modbc(None, l, j, slot), R=["MODS"], W=["mbF"])
                if last:
                    wr = sb(stg, "wr", [128, NE, D], F32)
                    rb = sb(stg, "rb", [128, NE], F32)
                    lgt = [sb(stg, "lgt%d" % i, [128, 32], F32) for i in range(2)]
                    for e_ in range(NE):
                        S.dma(wr[:, e_, :], moe_routerT[e_:e_ + 1, :].partition_broadcast(128), W=["wr"])
                    S.dma(rb[:], moe_rb.partition_broadcast(128), W=["rb"])
                tiles = list(range(2, NTILE)) if last else list(range(NTILE))
                junkR = sb(stg, "junkR", [128, D], BF16)

                def front(ti, t):
                    i = ti % 2
                    I_ = "%d" % i
                    mo = 0 if t < 2 else 3
                    tsl = slice(t * 128, (t + 1) * 128)
                    S.dma(ycs[i][:], YC[b, tsl, :], R=["YC"], W=["ycs" + I_])
                    S.dma(ycv[i][:], YT[b, :, tsl].rearrange("(c p) t -> p c t", p=128), R=["YT"], W=["ycv" + I_])
                    S.dma(ht[i][:], hsrc(l, b, t), R=["HB"], W=["htF" + I_])
                    for k in range(6):
                        S.tr(pT[i][:, k * 128:(k + 1) * 128], ycs[i][:, k * 128:(k + 1) * 128], idb[:], R=["ycs" + I_, "idb"], W=["pTF" + I_])
                    S.copy(ycT[i][:].rearrange("p k n -> p (k n)"), pT[i][:, 0:768], R=["pTF" + I_], W=["ycT" + I_], eng="scalar")
                    lhs = [ycT[i][:, 0, :], ycT[i][:, 1, :], ycT[i][:, 2, :], ycT[i][:, 3, :], ycv[i][:, 0, :], ycv[i][:, 1, :],
                           ycT[i][:, 4, :], ycT[i][:, 5, :]]
                    for nb in range(2):
                        pk = "pY%d" % (2 * i + nb)
                        for k in range(8):
                            S.mm(pY[2 * i + nb][:], lhs[k], wo[:, k, nb * 512:(nb + 1) * 512], start=(k == 0), stop=(k == 7),
                                 R=["ycT" + I_, "ycv" + I_, "wo"], W=[pk])
                        hs = slice(nb * 512, (nb + 1) * 512)
                        S.tt(tmp[:, hs], pY[2 * i + nb][:], mb[:, mo, hs], ALU.mult, R=[pk, "mbF"], W=["tmpF%d" % nb])
                        S.tt(hn[i][:, hs], ht[i][:, hs], tmp[:, hs], ALU.add, R=["htF" + I_, "tmpF%d" % nb], W=["hn" + I_], eng="gpsimd")
                    S.dma(HA[b, tsl, :], hn[i][:], R=["hn" + I_], W=["HA"], q="gpsimd")
                    S.actf(junk[:], hn[i][:], AF.Square, R=["hn" + I_], W=["junkF", "ssF" + I_], accum_out=ss[:, i:i + 1])
                    S.actf(rstd[:, i:i + 1], ss[:, i:i + 1], AF.Sqrt, R=["ssF" + I_, "epsb"], W=["rstdF" + I_], scale=1.0 / D, bias=epsb[:, 0:1])
                    S.recip(rstd[:, i:i + 1], rstd[:, i:i + 1], R=["rstdF" + I_], W=["rstdF" + I_])
                    S.stt(u32[i][:], hn[i][:], rstd[:, i:i + 1], mb[:, mo + 1, :], ALU.mult, ALU.mult,
                          R=["hn" + I_, "rstdF" + I_, "mbF"], W=["u32_" + I_])
                    S.tt(u32[i][:], u32[i][:], mb[:, mo + 2, :], ALU.add, R=["u32_" + I_, "mbF"], W=["u32_" + I_])
                    S.copy(ubf[i][:], u32[i][:], R=["u32_" + I_], W=["ubf" + I_], eng="scalar")

                def back(ti, t):
                    i = ti % 2
                    I_ = "%d" % i
                    mo = 0 if t < 2 else 3
                    tsl = slice(t * 128, (t + 1) * 128)
                    for k in range(8):
                        S.tr(pT[i][:, k * 128:(k + 1) * 128], ubf[i][:, k * 128:(k + 1) * 128], idb[:], R=["ubf" + I_, "idb"], W=["pTF" + I_])
                    S.copy(uTs[i][:].rearrange("p k n -> p (k n)"), pT[i][:], R=["pTF" + I_], W=["uTs" + I_])
                    S.dma(UT[b, :, tsl].rearrange("(k p) t -> p k t", p=128), uTs[i][:], R=["uTs" + I_], W=["UT"], q="gpsimd")
                    if last:
                        lg_ = lgt[i]
                        lk = "lgt" + I_
                        for e_ in range(NE):
                            S.stt(junkR[:], u32[i][:], 1.0, wr[:, e_, :], ALU.mult, ALU.mult, R=["u32_" + I_, "wr"],
                                  W=["junkR", lk], accum_out=lg_[:, e_:e_ + 1])
                        S.tt(lg_[:, 0:8], lg_[:, 0:8], rb[:], ALU.add, R=[lk, "rb"], W=[lk])
                        S.op("vector", lambda e, a=lg_: e.max(out=a[:, 8:16], in_=a[:, 0:8]), R=[lk], W=[lk])
                        S.ts(lg_[:, 16:24], lg_[:, 0:8], lg_[:, 8:9], None, ALU.is_equal, R=[lk], W=[lk])
                        S.ts(lg_[:, 24:32], lg_[:, 0:8], lg_[:, 9:10], None, ALU.is_equal, R=[lk], W=[lk])
                        S.tt(lg_[:, 10:11], lg_[:, 8:9], lg_[:, 9:10], ALU.subtract, R=[lk], W=[lk])
                        S.actf(lg_[:, 11:12], lg_[:, 10:11], AF.Sigmoid, R=[lk], W=[lk])
                        S.actf(lg_[:, 12:13], lg_[:, 10:11], AF.Sigmoid, R=[lk], W=[lk], scale=-1.0)
                        S.ts(lg_[:, 16:24], lg_[:, 16:24], lg_[:, 11:12], None, ALU.mult, R=[lk], W=[lk])
                        S.stt(lg_[:, 0:8], lg_[:, 24:32], lg_[:, 12:13], lg_[:, 16:24], ALU.mult, ALU.add, R=[lk], W=[lk])
                        S.dma(COMB[b, tsl, :], lg_[:, 0:8], R=[lk], W=["COMB"], q="gpsimd")

                segs = [list(enumerate(tiles))]
                for si_, seg in enumerate(segs):
                    if si_ > 0:
                        yield
                    front(*seg[0])
                    for j in range(len(seg)):
                        if j + 1 < len(seg):
                            front(*seg[j + 1])
                        back(*seg[j])
            return f

        def stage_g(l, b):
            last = l == 1

            def f(stg):
                t0 = 2 if last else 0
                ntile = NTILE - t0
                ntok = ntile * 128
                tok0 = t0 * 128
                nexp = NE if last else 1
                dff = D_FFE if last else D_FF
                uT = sb(stg, "uT", [128, 8, ntok], BF16)
                yacc = sb(stg, "yacc", [128, ntile, D], F32)
                wg = [sb(stg, "wg%d" % i, [128, 8, 512], BF16) for i in range(2)]
                wu = [sb(stg, "wu%d" % i, [128, 8, 512], BF16) for i in range(2)]
                wd = [sb(stg, "wd%d" % i, [128, 4, D], BF16) for i in range(2)]
                act = [sb(stg, "act%d" % i, [128, 4, 512], BF16) for i in range(2)]
                sg = [sb(stg, "sgG%d" % i, [128, 512], F32) for i in range(2)]
                comb = sb(stg, "comb", [128, ntile, NE], F32)
                pG = [psf(stg, "pG%d" % i) for i in range(2)]
                pU = [psf(stg, "pU%d" % i) for i in range(2)]
                pD = [psf(stg, "pD%d" % i) for i in range(4)]
                for k in range(8):
                    S.dma(uT[:, k, :], UT[b, k * 128:(k + 1) * 128, tok0:NT], R=["UT"], W=["uT"])
                if last:
                    S.dma(comb[:], COMB[b, tok0:NT, :].rearrange("(t p) e -> p t e", p=128), R=["COMB"], W=["comb"])
                S.memset(yacc[:], 0.0, W=["yacc"])
                groups = []
                for e_ in range(nexp):
                    for f0 in range(0, dff, 512):
                        groups.append((e_, f0, min(512, dff - f0)))
                tgs = [(s0, min(512, ntok - s0)) for s0 in range(0, ntok, 512)]
                cnt = {"gu": 0, "pd": 0, "sg": 0, "act": 0}

                def rec_gu(u):
                    gi, e_, f0, fw, s0, N, ai = u
                    wi = gi % 2
                    W_ = "%d" % wi
                    A_ = "%d" % ai
                    for c in range(fw // 128):
                        pi = cnt["gu"] % 2
                        cnt["gu"] += 1
                        for k in range(8):
                            S.mm(pG[pi][:, 0:N], wg[wi][:, k, c * 128:(c + 1) * 128], uT[:, k, s0:s0 + N], start=(k == 0), stop=(k == 7),
                                 R=["wg" + W_, "uT"], W=["pG%d" % pi])
                        for k in range(8):
                            S.mm(pU[pi][:, 0:N], wu[wi][:, k, c * 128:(c + 1) * 128], uT[:, k, s0:s0 + N], start=(k == 0), stop=(k == 7),
                                 R=["wu" + W_, "uT"], W=["pU%d" % pi])
                        si = cnt["sg"] % 2
                        cnt["sg"] += 1
                        S.actf(sg[si][:, 0:N], pG[pi][:, 0:N], AF.Silu, R=["pG%d" % pi], W=["sgG%d" % si])
                        S.tt(act[ai][:, c, 0:N], pU[pi][:, 0:N], sg[si][:, 0:N], ALU.mult, R=["pU%d" % pi, "sgG%d" % si], W=["act" + A_])

                def rec_down(u):
                    gi, e_, f0, fw, s0, N, ai = u
                    wi = gi % 2
                    W_ = "%d" % wi
                    A_ = "%d" % ai
                    nch = fw // 128
                    for s_ in range(N // 128):
                        tl = s0 // 128 + s_
                        for nb in range(2):
                            di = cnt["pd"] % 4
                            cnt["pd"] += 1
                            for c in range(nch):
                                S.mm(pD[di][:], act[ai][:, c, s_ * 128:(s_ + 1) * 128], wd[wi][:, c, nb * 512:(nb + 1) * 512],
                                     start=(c == 0), stop=(c == nch - 1), R=["act" + A_, "wd" + W_], W=["pD%d" % di])
                            ya = yacc[:, tl, nb * 512:(nb + 1) * 512]
                            yk = "yacc%d_%d" % (tl, nb)
                            sc = comb[:, tl, e_:e_ + 1] if last else 1.0
                            S.stt(ya, pD[di][:], sc, ya, ALU.mult, ALU.add, R=["pD%d" % di, "comb", "yacc", yk], W=[yk])

                def rec_wload(gi, e_, f0, fw):
                    wi = gi % 2
                    W_ = "%d" % wi
                    nch = fw // 128
                    gsrc = (moe_wg[e_] if last else ffn_wg[0])
                    usrc = (moe_wu[e_] if last else ffn_wu[0])
                    dsrc = (moe_wd[e_] if last else ffn_wd[0])
                    S.dma(wg[wi][:, :, 0:fw], gsrc[:, f0:f0 + fw].rearrange("(k p) n -> p k n", p=128), W=["wg" + W_], q="gpsimd")
                    S.dma(wu[wi][:, :, 0:fw], usrc[:, f0:f0 + fw].rearrange("(k p) n -> p k n", p=128), W=["wu" + W_], q="gpsimd")
                    S.dma(wd[wi][:, 0:nch, :], dsrc[f0:f0 + fw, :].rearrange("(c p) n -> p c n", p=128), W=["wd" + W_], q="gpsimd")

                units = []
                for gi, (e_, f0, fw) in enumerate(groups):
                    for (s0, N) in tgs:
                        units.append((gi, e_, f0, fw, s0, N, cnt["act"] % 2))
                        cnt["act"] += 1
                loaded = set()

                def ensure(u):
                    if u[0] not in loaded:
                        loaded.add(u[0])
                        rec_wload(u[0], u[1], u[2], u[3])

                ensure(units[0])
                rec_gu(units[0])
                for n, u in enumerate(units):
                    if n + 1 < len(units):
                        ensure(units[n + 1])
                        rec_gu(units[n + 1])
                    rec_down(u)
                yield
                g2 = sb(stg, "g2", [128, 2, D], F32)
                S.dma(g2[:, 1, :], modbc(None, l, b, 5), R=["MODS"], W=["g2"])
                if not last:
                    S.dma(g2[:, 0, :], modbc(None, l, 2, 5), R=["MODS"], W=["g2"])
                else:
                    S.dma(g2[:, 0, :], final_w.partition_broadcast(128), W=["g2"])
                hh = [sb(stg, "hh%d" % i, [128, D], F32) for i in range(2)]
                ssb = sb(stg, "ssG", [128, 2], F32)
                rsb = sb(stg, "rsG", [128, 2], F32)
                junk = sb(stg, "junkG", [128, D], BF16)
                for tl in range(ntile):
                    t = t0 + tl
                    i = tl % 2
                    I_ = "%d" % i
                    tsl = slice(t * 128, (t + 1) * 128)
                    ykeys = ["yacc", "yacc%d_0" % tl, "yacc%d_1" % tl]
                    S.dma(hh[i][:], HA[b, tsl, :], R=["HA"], W=["hh" + I_])
                    gsel = 1 if (t >= 2) else 0
                    S.tt(yacc[:, tl, :], yacc[:, tl, :], g2[:, gsel if not last else 1, :], ALU.mult, R=ykeys + ["g2"], W=ykeys[1:])
                    S.tt(hh[i][:], hh[i][:], yacc[:, tl, :], ALU.add, R=ykeys + ["hh" + I_], W=["hh" + I_], eng="gpsimd")
                    if not last:
                        S.dma(HB[b, tsl, :], hh[i][:], R=["hh" + I_], W=["HB"], q="gpsimd")
                    else:
                        S.actf(junk[:], hh[i][:], AF.Square, R=["hh" + I_], W=["junkG", "ssG" + I_], accum_out=ssb[:, i:i + 1])
                        S.actf(rsb[:, i:i + 1], ssb[:, i:i + 1], AF.Sqrt, R=["ssG" + I_, "epsb"], W=["rsG" + I_], scale=1.0 / D, bias=epsb[:, 0:1])
                        S.recip(rsb[:, i:i + 1], rsb[:, i:i + 1], R=["rsG" + I_], W=["rsG" + I_])
                        S.stt(hh[i][:], hh[i][:], rsb[:, i:i + 1], g2[:, 0, :], ALU.mult, ALU.mult, R=["hh" + I_, "rsG" + I_, "g2"], W=["hh" + I_])
                        S.dma(out[b, (t - 2) * 128:(t - 1) * 128, :], hh[i][:], R=["hh" + I_], W=["out"], q="gpsimd")
            return f

        prog = []
        for l in range(2):
            if P1:
                prog.append(stage_b(l, (0, 1)))
                for b in range(2):
                    prog.append(stage_c(l, b))
                    prog.append(stage_d(l, b))
                    prog.append(stage_e(l, b))
                for b in range(2):
                    prog.append(stage_f(l, b))
            for b in range(2):
                if (l == 0 and P1) or (l == 1 and P2 and b in bs):
                    prog.append(stage_g(l, b))
        for i, fn in enumerate(prog):
            if i >= upto:
                break
            stage(fn)
    return nc


def host_consts(na_rpb):
    ident = np.eye(128, dtype=np.float32)
    t = np.arange(T)
    row = (t // GRID_W).astype(np.float32)
    col = (t % GRID_W).astype(np.float32)
    inv = (np.float32(10000.0) ** (-np.arange(0, 16, 2, dtype=np.float32) / np.float32(16))).astype(np.float32)
    ang = np.concatenate([row[:, None] * inv, col[:, None] * inv], axis=-1).astype(np.float32)
    cos, sin = np.cos(ang).astype(np.float32), np.sin(ang).astype(np.float32)
    cos2 = np.ones((32, NT), np.float32)
    sinS = np.zeros((32, NT), np.float32)
    cos2[0:16, LC:] = cos.T
    cos2[16:32, LC:] = cos.T
    sinS[0:16, LC:] = -sin.T
    sinS[16:32, LC:] = sin.T
    ks = np.float32(32 ** -0.5)
    rope = np.stack([np.tile(cos2, (4, 1)), np.tile(sinS, (4, 1)), np.tile(cos2, (4, 1)) * ks, np.tile(sinS, (4, 1)) * ks]).astype(np.float32)
    j = np.arange(128)[:, None].astype(np.float32)
    i = np.arange(128)[None, :].astype(np.float32)
    retc = np.zeros((128, 772), np.float32)
    retc[:, 0:128] = np.maximum(i - j, 0)
    retc[:, 128:256] = (i >= j)
    retc[:, 256:384] = np.maximum(j - i, 0)
    retc[:, 384:512] = (j >= i)
    retc[:, 512:640] = i + 1.0
    retc[:, 640:768] = 128.0 - i
    retc[:, 768] = 127.0 - j[:, 0]
    retc[:, 769] = j[:, 0]
    kc = np.arange(64)[:, None]
    qc = np.arange(64)[None, :]
    w0 = np.clip(qc - 8, 0, 48)
    ok = (kc >= w0) & (kc < w0 + 16)
    dc = np.clip(kc - qc + 15, 0, 30)
    g = na_rpb[:, :, :, dc]
    g = np.where(ok[None, None, None], g, np.float32(-30000.0)).astype(np.float32)
    napb = np.ascontiguousarray(g.transpose(0, 3, 1, 2, 4)).reshape(2, 64, NH * 15 * 64)
    return ident, rope, retc, napb


_CACHE = {}
LAUNCH2 = ((0,), (1,))
SINGLE = True


def kernel(x, c, ctx, c_ctx, w_mod, b_mod, norm1_w, norm2_w, w_in, w_out, na_rpb, conv_w, conv_b,
           conv_ln_w, conv_ln_b, ret_decay, ret_gn_w, ffn_w_gate, ffn_w_up, ffn_w_down,
           moe_router, moe_router_b, moe_w_gate, moe_w_up, moe_w_down, final_norm_w):
    f = lambda a: np.ascontiguousarray(np.asarray(a, dtype=np.float32))
    ident, rope, retc, napb = host_consts(f(na_rpb))
    shared = {
        "w_mod": f(w_mod), "b_mod": f(b_mod), "norm1_w": f(norm1_w), "norm2_w": f(norm2_w), "w_in": f(w_in), "w_out": f(w_out),
        "convwT": np.ascontiguousarray(f(conv_w).reshape(2, 31, 2, 128).transpose(0, 3, 2, 1)),
        "cpard": np.ascontiguousarray(np.stack([f(conv_b), f(conv_ln_w), f(conv_ln_b)], axis=1).reshape(2, 3, 2, 128).transpose(0, 3, 1, 2)),
        "ret_decay": f(ret_decay).reshape(2, 8), "ret_gn_w": f(ret_gn_w),
        "ffn_w_gate": f(ffn_w_gate), "ffn_w_up": f(ffn_w_up), "ffn_w_down": f(ffn_w_down),
        "moe_routerT": np.ascontiguousarray(f(moe_router)[0].T), "moe_router_b": f(moe_router_b).reshape(1, NE),
        "moe_w_gate": f(moe_w_gate)[0], "moe_w_up": f(moe_w_up)[0], "moe_w_down": f(moe_w_down)[0],
        "final_norm_w": f(final_norm_w).reshape(1, D),
        "ident": ident, "rope": rope, "retc": retc, "napb": napb,
    }
    x = f(x); c = f(c); ctx = f(ctx); c_ctx = f(c_ctx)
    p2names = ("moe_w_gate", "moe_w_up", "moe_w_down", "final_norm_w")
    in1 = []
    for i in range(8):
        m = {k: v for k, v in shared.items() if k not in p2names}
        m["x2"] = x[2 * i:2 * i + 2]
        m["ctx2"] = ctx[2 * i:2 * i + 2]
        cv = np.concatenate([c[2 * i:2 * i + 2], c_ctx[None, :]], axis=0)
        m["cvecT"] = np.ascontiguousarray(cv.reshape(3, 8, 128).transpose(2, 1, 0))
        in1.append(m)
    if SINGLE:
        for i in range(8):
            for k in p2names:
                in1[i][k] = shared[k]
        if "nc" not in _CACHE:
            _CACHE["nc"] = build_program(part=0)
        r0 = run_bass_kernel_spmd(_CACHE["nc"], in1, core_ids=list(range(8))).results
        return np.concatenate([r["out"] for r in r0], axis=0)
    if "nc1" not in _CACHE:
        _CACHE["nc1"] = build_program(part=1)
        _CACHE["nc2"] = [build_program(part=2, bs=bs_) for bs_ in LAUNCH2]
    r1 = run_bass_kernel_spmd(_CACHE["nc1"], in1, core_ids=list(range(8))).results
    outs = [np.zeros((2, T, D), np.float32) for _ in range(8)]
    for bs_, nc2 in zip(LAUNCH2, _CACHE["nc2"]):
        in2 = []
        for i in range(8):
            m = {k: shared[k] for k in p2names}
            for k in ("MODS", "HA", "UT", "COMB"):
                m[k] = r1[i][k]
            in2.append(m)
        r2 = run_bass_kernel_spmd(nc2, in2, core_ids=list(range(8))).results
        for i in range(8):
            for b in bs_:
                outs[i][b] = r2[i]["out"][b]
    return np.concatenate(outs, axis=0)
```

```python
import contextlib
import numpy as np
CUT = 0
import concourse.bass as bass
import concourse.mybir as mybir
from concourse.bass_utils import run_bass_kernel_spmd

F32 = mybir.dt.float32
BF16 = mybir.dt.bfloat16
ALU = mybir.AluOpType
AF = mybir.ActivationFunctionType
AX = mybir.AxisListType

ENGS = ("tensor", "vector", "scalar", "gpsimd", "sync")
NDMA = 12


class Op:
    __slots__ = ("eng", "fn", "dma", "deps", "signal", "sem", "val", "idx")

    def __init__(self, eng, fn, dma):
        self.eng, self.fn, self.dma = eng, fn, dma
        self.deps = []
        self.signal = dma
        self.sem = None
        self.val = None


class Sched:
    def __init__(self, nc, stack):
        self.nc = nc
        self.csem = {e: stack.enter_context(nc.semaphore("cs_" + e)) for e in ENGS}
        self.ccnt = {e: 0 for e in ENGS}
        self.dsem = {e: [stack.enter_context(nc.semaphore("ds_%s%d" % (e, i))) for i in range(NDMA)]
                     for e in ("sync", "gpsimd")}
        self.dcnt = {e: [0] * NDMA for e in self.dsem}
        self.drr = {e: 0 for e in self.dsem}
        self.known = {e: {} for e in ENGS}
        self.same_engine_sync = True
        self.reset()

    def reset(self):
        self.ops = {e: [] for e in ENGS}
        self.lastw = {}
        self.readers = {}
        self.allops = []

    def op(self, eng, fn, R=(), W=(), dma=False):
        o = Op(eng, fn, dma)
        deps = []
        for r in R:
            w = self.lastw.get(r)
            if w is not None:
                deps.append(w)
        for w_ in W:
            w = self.lastw.get(w_)
            if w is not None:
                deps.append(w)
            deps.extend(self.readers.get(w_, ()))
        seen = set()
        for d in deps:
            if id(d) in seen or d is o:
                continue
            seen.add(id(d))
            if d.eng == eng and not d.dma and not dma:
                if eng == "tensor" or not self.same_engine_sync:
                    continue
            o.deps.append(d)
        for r in R:
            lst = self.readers.setdefault(r, [])
            if not dma:
                lst[:] = [x for x in lst if not (x.eng == eng and not x.dma)]
            lst.append(o)
        for w_ in W:
            self.lastw[w_] = o
            self.readers[w_] = []
        self.ops[eng].append(o)
        self.allops.append(o)
        return o

    def mm(self, out, lhsT, rhs, start=True, stop=True, R=(), W=()):
        return self.op("tensor", lambda e: e.matmul(out, lhsT=lhsT, rhs=rhs, start=start, stop=stop), R, W)

    def tr(self, out, in_, ident, R=(), W=()):
        return self.op("tensor", lambda e: e.transpose(out=out, in_=in_, identity=ident), R, W)

    def actf(self, out, in_, func, R=(), W=(), **kw):
        return self.op("scalar", lambda e: e.activation(out=out, in_=in_, func=func, **kw), R, W)

    def tt(self, out, in0, in1, op, R=(), W=(), eng="vector"):
        return self.op(eng, lambda e: e.tensor_tensor(out=out, in0=in0, in1=in1, op=op), R, W)

    def ts(self, out, in0, s1, s2, op0, op1=None, R=(), W=(), eng="vector", **kw):
        if op1 is None:
            return self.op(eng, lambda e: e.tensor_scalar(out=out, in0=in0, scalar1=s1, scalar2=None, op0=op0, **kw), R, W)
        return self.op(eng, lambda e: e.tensor_scalar(out=out, in0=in0, scalar1=s1, scalar2=s2, op0=op0, op1=op1, **kw), R, W)

    def stt(self, out, in0, scalar, in1, op0, op1, R=(), W=(), **kw):
        return self.op("vector", lambda e: e.scalar_tensor_tensor(out=out, in0=in0, scalar=scalar, in1=in1,
                                                                  op0=op0, op1=op1, **kw), R, W)

    def copy(self, out, in_, R=(), W=(), eng="vector"):
        if eng == "scalar":
            return self.op(eng, lambda e: e.activation(out=out, in_=in_, func=AF.Copy), R, W)
        return self.op(eng, lambda e: e.tensor_copy(out=out, in_=in_), R, W)

    def recip(self, out, in_, R=(), W=()):
        return self.op("vector", lambda e: e.reciprocal(out=out, in_=in_), R, W)

    def rsum(self, out, in_, R=(), W=()):
        return self.op("vector", lambda e: e.reduce_sum(out=out, in_=in_, axis=AX.X), R, W)

    def memset(self, ap, val, W=(), eng="gpsimd"):
        return self.op(eng, lambda e: e.memset(ap, val), (), W)

    def dma(self, out, in_, R=(), W=(), q="sync", **kw):
        return self.op(q, lambda e: e.dma_start(out=out, in_=in_, **kw), R, W, dma=True)

    def emit(self, block):
        for o in self.allops:
            for d in o.deps:
                d.signal = True
        for e in ENGS:
            for o in self.ops[e]:
                if o.dma:
                    i = self.drr[e]
                    self.drr[e] = (i + 1) % NDMA
                    prev = self.dcnt[e][i]
                    self.dcnt[e][i] = prev + 16
                    o.sem = self.dsem[e][i]
                    o.val = prev + 16
                    o.idx = (e, i, prev)
                elif o.signal:
                    self.ccnt[e] += 1
                    o.sem = self.csem[e]
                    o.val = self.ccnt[e]
        sched = self

        def make(e):
            def body(eng):
                known = sched.known[e]
                for o in sched.ops[e]:
                    waits = {}
                    for d in o.deps:
                        k = id(d.sem)
                        if k not in waits or waits[k][1] < d.val:
                            waits[k] = (d.sem, d.val)
                    if o.dma and o.idx[2] > 0:
                        s = sched.dsem[o.idx[0]][o.idx[1]]
                        k = id(s)
                        if k not in waits or waits[k][1] < o.idx[2]:
                            waits[k] = (s, o.idx[2])
                    for k, (s, v) in waits.items():
                        if known.get(k, 0) >= v:
                            continue
                        eng.wait_ge(s, v)
                        known[k] = v
                    inst = o.fn(eng)
                    if o.signal:
                        inst.then_inc(o.sem, 16 if o.dma else 1)
                if e in sched.dsem:
                    for i, s in enumerate(sched.dsem[e]):
                        v = sched.dcnt[e][i]
                        if v > 0 and known.get(id(s), 0) < v:
                            eng.wait_ge(s, v)
                            known[id(s)] = v
            return body

        for e in ENGS:
            if self.ops[e]:
                getattr(block, e)(make(e))
        self.reset()


D = 1024
T = 2048
LC = 256
NT = T + LC
NTILE = NT // 128
GRID_W = 64
EPS = 1e-6
NH = 8
D_FF = 2816
D_FFE = 3584
NE = 8
C_NAQ, C_NAK, C_NAV, C_CVA, C_CVG, C_RQ, C_RK, C_RV, C_GF, C_GB = 0, 512, 1024, 1536, 1792, 2048, 2176, 2304, 2560, 2816
R_NAQ, R_NAK, R_CV, R_RQ, R_RK, PXT_ROWS = 0, 512, 1024, 1280, 1408, 1536
PXV_COLS = 1280


def na_patterns():
    pats, index, table = [], {}, {}
    for a in range(16):
        kts, pids = [], []
        for kt in range(16):
            quad = []
            anyv = False
            for kr in range(2):
                for qr in range(2):
                    krow, qrow = 2 * kt + kr, 2 * a + qr
                    st = min(max(qrow - 4, 0), 24)
                    if st <= krow <= st + 7:
                        quad.append(krow - qrow + 7)
                        anyv = True
                    else:
                        quad.append(-1)
            if not anyv:
                continue
            atype = a if a in (0, 1, 14, 15) else -1
            key = (atype, kt - a) + tuple(quad)
            if key not in index:
                index[key] = len(pats)
                pats.append(tuple(quad))
            kts.append(kt)
            pids.append(index[key])
        table[a] = (kts, pids)
    return pats, table


def build_program(debug=False, upto=99, part=0, bs=(0, 1)):
    nc = bass.Bass("TRN2", target_bir_lowering=False)
    dk = "ExternalOutput" if debug else "Internal"

    def din(name, shape, dt=F32):
        return nc.dram_tensor(name, list(shape), dt, kind="ExternalInput").ap()

    def dscr(name, shape, dt, handoff=False):
        kind = dk
        if handoff and part == 1:
            kind = "ExternalOutput"
        if handoff and part == 2:
            kind = "ExternalInput"
        return nc.dram_tensor(name, list(shape), dt, kind=kind).ap()

    P1 = part in (0, 1)
    P2 = part in (0, 2)
    _din = din

    def din1(name, shape, dt=F32):
        return _din(name, shape, dt) if P1 else None

    def din2(name, shape, dt=F32):
        return _din(name, shape, dt) if P2 else None

    x2 = din1("x2", [2, T, D]); ctx2 = din1("ctx2", [2, LC, D]); cvecT = din1("cvecT", [128, 8, 3])
    w_mod = din1("w_mod", [2, D, 6 * D]); b_mod = din1("b_mod", [2, 6 * D])
    norm1_w = din1("norm1_w", [2, D]); norm2_w = din1("norm2_w", [2, D])
    w_in = din1("w_in", [2, D, 3072]); w_out = din1("w_out", [2, D, D])
    convwT = din1("convwT", [2, 128, 2, 31]); cpard = din1("cpard", [2, 128, 3, 2])
    ret_decay = din1("ret_decay", [2, 8]); ret_gn_w = din1("ret_gn_w", [2, 256])
    ffn_wg = din1("ffn_w_gate", [1, D, D_FF]); ffn_wu = din1("ffn_w_up", [1, D, D_FF]); ffn_wd = din1("ffn_w_down", [1, D_FF, D])
    moe_routerT = din1("moe_routerT", [NE, D]); moe_rb = din1("moe_router_b", [1, NE])
    moe_wg = din2("moe_w_gate", [NE, D, D_FFE]); moe_wu = din2("moe_w_up", [NE, D, D_FFE]); moe_wd = din2("moe_w_down", [NE, D_FFE, D])
    final_w = din2("final_norm_w", [1, D])
    identd = din1("ident", [128, 128]); rope = din1("rope", [4, 128, NT]); retc = din1("retc", [128, 772])
    napb = din1("napb", [2, 64, NH * 15 * 64])
    out = nc.dram_tensor("out", [2, T, D], F32, kind="ExternalOutput").ap() if P2 else None

    MODS = dscr("MODS", [2, 3, 6 * D], F32, True)
    PXT = dscr("PXT", [2, PXT_ROWS, NT], BF16)
    PXV = dscr("PXV", [2, NT, PXV_COLS], BF16)
    YC = dscr("YC", [2, NT, 768], BF16)
    YT = dscr("YT", [2, 256, NT], BF16)
    HA = dscr("HA", [2, NT, D], F32, True)
    HB = dscr("HB", [2, NT, D], F32)
    UT = dscr("UT", [2, D, NT], BF16, True)
    COMB = dscr("COMB", [2, NT, NE], F32, True)

    pats, ptable = na_patterns()
    NPAT = len(pats)

    with contextlib.ExitStack() as glob:
        S = Sched(nc, glob)
        idb = glob.enter_context(nc.sbuf_tensor("idb", [128, 128], BF16))
        id32 = glob.enter_context(nc.sbuf_tensor("id32", [128, 128], F32))

        def emit_block():
            if not S.allops:
                return
            with nc.Block() as blk:
                S.emit(blk)

        def stage(fn):
            with contextlib.ExitStack() as stg:
                r = fn(stg)
                if r is not None:
                    for _ in r:
                        emit_block()
                emit_block()

        uid = [0]

        def un(name):
            uid[0] += 1
            return "%s_%d" % (name, uid[0])

        def sb(stg, name, shape, dt):
            return stg.enter_context(nc.sbuf_tensor(un(name), list(shape), dt))

        def psf(stg, name, n=512):
            return stg.enter_context(nc.psum_tensor(un(name), [128, n], F32))

        def psb(stg, name, n=1024):
            return stg.enter_context(nc.psum_tensor(un(name), [128, n], BF16))

        def hsrc(l, b, t):
            if l == 0:
                return ctx2[b, t * 128:(t + 1) * 128, :] if t < 2 else x2[b, (t - 2) * 128:(t - 1) * 128, :]
            return HB[b, t * 128:(t + 1) * 128, :]

        def rms_rstd(stg_bufs, h_ap, hkey, junk, ss, rstd, tag):
            S.actf(junk, h_ap, AF.Square, R=[hkey], W=["junk" + tag, "ss" + tag], accum_out=ss)
            S.actf(rstd, ss, AF.Sqrt, R=["ss" + tag], W=["rstd" + tag], scale=1.0 / D, bias=epsb[:, 0:1])
            S.recip(rstd, rstd, R=["rstd" + tag], W=["rstd" + tag])

        epsb = glob.enter_context(nc.sbuf_tensor("epsb", [128, 1], F32))

        def stage_a(stg):
            S.dma(idb[:], identd, W=["idb"], q="gpsimd")
            S.dma(id32[:], identd, W=["id32"])
            S.memset(epsb[:], EPS, W=["epsb"])
            cT = sb(stg, "cT", [128, 8, 3], F32)
            sT = sb(stg, "sT", [128, 8, 3], F32)
            S.dma(cT[:], cvecT, W=["cT"])
            S.actf(sT[:], cT[:], AF.Silu, R=["cT"], W=["sT"])
            wb = [sb(stg, "wmb%d" % i, [128, 8, 512], F32) for i in range(2)]
            modrow = sb(stg, "modrow", [3, 6 * D], F32)
            bm3 = sb(stg, "bm3", [3, 6 * D], F32)
            nw3 = sb(stg, "nw3", [3, 2, D], F32)
            pm = [psf(stg, "pm%d" % i) for i in range(2)]
            it = 0
            for l in range(2):
                S.dma(bm3[:], b_mod[l:l + 1, :].partition_broadcast(3), R=["bm3"], W=["bm3"])
                S.dma(nw3[:, 0, :], norm1_w[l:l + 1, :].partition_broadcast(3), W=["nw3"])
                S.dma(nw3[:, 1, :], norm2_w[l:l + 1, :].partition_broadcast(3), W=["nw3"])
                for nb in range(12):
                    i = it % 2
                    it += 1
                    S.dma(wb[i][:], w_mod[l, :, nb * 512:(nb + 1) * 512].rearrange("(k p) n -> p k n", p=128),
                          W=["wmb%d" % i])
                    for k in range(8):
                        S.mm(pm[i][0:3, :], sT[:, k, :], wb[i][:, k, :], start=(k == 0), stop=(k == 7),
                             R=["sT", "wmb%d" % i], W=["pm%d" % i])
                    S.copy(modrow[:, nb * 512:(nb + 1) * 512], pm[i][0:3, :], R=["pm%d" % i], W=["modrow"], eng="scalar")
                S.tt(modrow[:], modrow[:], bm3[:], ALU.add, R=["modrow", "bm3"], W=["modrow"])
                for s_, wsel in ((1, 0), (4, 1)):
                    seg = modrow[:, s_ * D:(s_ + 1) * D]
                    S.stt(seg, seg, 1.0, nw3[:, wsel, :], ALU.add, ALU.mult, R=["modrow", "nw3"], W=["modrow"])
                S.dma(MODS[l], modrow[:], R=["modrow"], W=["MODS"])

        def stage_a2(stg):
            S.memset(epsb[:], EPS, W=["epsb"])

        stage(stage_a if P1 else stage_a2)

        def modbc(dst, l, j, s):
            return MODS[l, j:j + 1, s * D:(s + 1) * D].partition_broadcast(128)

        def stage_b(l, bsel):
            def f(stg):
                w = sb(stg, "win", [128, 8, 3328], BF16)
                for k in range(8):
                    S.dma(w[:, k, 0:3072], w_in[l, k * 128:(k + 1) * 128, :], W=["win"], q="gpsimd")
                for (src, dst) in ((C_RQ, 3072), (C_RK, 3200)):
                    sv = w[:, :, src:src + 128].rearrange("p k (h two i) -> p k h two i", two=2, i=16)
                    dv = w[:, :, dst:dst + 128].rearrange("p k (h two i) -> p k h two i", two=2, i=16)
                    S.copy(dv[:, :, :, 0, :], sv[:, :, :, 1, :], R=["win"], W=["win"])
                    S.copy(dv[:, :, :, 1, :], sv[:, :, :, 0, :], R=["win"], W=["win"], eng="gpsimd")
                if CUT == 1:
                    return
                mb = sb(stg, "mb", [128, 4, D], F32)
                ropet = [sb(stg, "ropet%d" % i, [128, 4, 512], F32) for i in range(2)]
                ht = [sb(stg, "ht%d" % i, [128, D], F32) for i in range(3)]
                junk = sb(stg, "junk", [128, D], BF16)
                xn = [sb(stg, "xn%d" % i, [128, D], F32) for i in range(2)]
                ux = [sb(stg, "ux%d" % i, [128, D], BF16) for i in range(2)]
                uxT = [sb(stg, "uxT%d" % i, [128, 8, 512], BF16) for i in range(2)]
                ss = sb(stg, "ss", [128, 4], F32)
                rstd = sb(stg, "rstd", [128, 4], F32)
                fo = [sb(stg, "fo%d" % i, [128, 512], BF16) for i in range(3)]
                sg = [sb(stg, "sg%d" % i, [128, 512], F32) for i in range(2)]
                r1 = [sb(stg, "r1%d" % i, [128, 512], F32) for i in range(2)]
                r2 = [sb(stg, "r2%d" % i, [128, 512], F32) for i in range(2)]
                to = [sb(stg, "to%d" % i, [128, PXV_COLS], BF16) for i in range(2)]
                pT = [psb(stg, "pT%d" % i) for i in range(2)]
                pA = [psf(stg, "pA%d" % i) for i in range(6)]
                tcnt = 0
                gcnt = 0
                focnt = 0
                pacnt = 0
                for b in bsel:
                    for s_, (j, slot) in enumerate(((2, 1), (2, 0), (b, 1), (b, 0))):
                        S.dma(mb[:, s_, :], modbc(None, l, j, slot), R=["MODS"], W=["mb"])
                    for g in range(5):
                        tiles = list(range(4 * g, min(4 * g + 4, NTILE)))
                        N = 128 * len(tiles)
                        gi = gcnt % 2
                        gcnt += 1
                        c0 = 4 * g * 128
                        S.dma(ropet[gi][:, :, 0:N], rope[:, :, c0:c0 + N].rearrange("f p n -> p f n"), W=["ropet%d" % gi])
                        for si, t in enumerate(tiles):
                            hi = tcnt % 3
                            xi = tcnt % 2
                            tcnt += 1
                            mo = 0 if t < 2 else 2
                            S.dma(ht[hi][:], hsrc(l, b, t), R=["HB"], W=["ht%d" % hi])
                            S.actf(junk[:], ht[hi][:], AF.Square, R=["ht%d" % hi], W=["junk", "ss%d" % xi], accum_out=ss[:, xi:xi + 1])
                            S.actf(rstd[:, xi:xi + 1], ss[:, xi:xi + 1], AF.Sqrt, R=["ss%d" % xi, "epsb"], W=["rstd%d" % xi],
                                   scale=1.0 / D, bias=epsb[:, 0:1])
                            S.recip(rstd[:, xi:xi + 1], rstd[:, xi:xi + 1], R=["rstd%d" % xi], W=["rstd%d" % xi])
                            S.stt(xn[xi][:], ht[hi][:], rstd[:, xi:xi + 1], mb[:, mo, :], ALU.mult, ALU.mult,
                                  R=["ht%d" % hi, "rstd%d" % xi, "mb"], W=["xn%d" % xi])
                            S.tt(ux[xi][:], xn[xi][:], mb[:, mo + 1, :], ALU.add, R=["xn%d" % xi, "mb"], W=["ux%d" % xi])
                            for k in range(8):
                                S.tr(pT[xi][:, k * 128:(k + 1) * 128], ux[xi][:, k * 128:(k + 1) * 128], idb[:],
                                     R=["ux%d" % xi, "idb"], W=["pT%d" % xi])
                            S.copy(uxT[gi][:, :, si * 128:(si + 1) * 128], pT[xi][:].rearrange("p (k n) -> p k n", k=8),
                                   R=["pT%d" % xi], W=["uxT%d" % gi], eng="scalar")
                        uk = "uxT%d" % gi
                        if CUT == 2:
                            return

                        def fm(col):
                            nonlocal pacnt
                            pi = pacnt % 6
                            pacnt += 1
                            for k in range(8):
                                S.mm(pA[pi][:, 0:N], w[:, k, col:col + 128], uxT[gi][:, k, 0:N], start=(k == 0), stop=(k == 7),
                                     R=["win", uk], W=["pA%d" % pi])
                            return pi

                        def fo_store(fi, row):
                            S.dma(PXT[b, row:row + 128, c0:c0 + N], fo[fi][:, 0:N], R=["fo%d" % fi], W=["PXT"], q="gpsimd")

                        for (col, row) in ((C_NAQ, R_NAQ), (C_NAK, R_NAK)):
                            for m in range(4):
                                pi = fm(col + m * 128)
                                fi = focnt % 3
                                focnt += 1
                                if m % 2 == 0:
                                    S.copy(fo[fi][:, 0:N], pA[pi][:, 0:N], R=["pA%d" % pi], W=["fo%d" % fi], eng="scalar")
                                else:
                                    S.copy(fo[fi][:, 0:N], pA[pi][:, 0:N], R=["pA%d" % pi], W=["fo%d" % fi])
                                fo_store(fi, row + m * 128)
                        if CUT == 3:
                            return
                        for m in range(2):
                            pa = fm(C_CVA + m * 128)
                            pg = fm(C_CVG + m * 128)
                            si_ = m
                            S.actf(sg[si_][:, 0:N], pA[pg][:, 0:N], AF.Sigmoid, R=["pA%d" % pg], W=["sg%d" % si_])
                            fi = focnt % 3
                            focnt += 1
                            S.tt(fo[fi][:, 0:N], pA[pa][:, 0:N], sg[si_][:, 0:N], ALU.mult, R=["pA%d" % pa, "sg%d" % si_], W=["fo%d" % fi])
                            fo_store(fi, R_CV + m * 128)
                        if CUT == 4:
                            return
                        for qi, (col, scol, row) in enumerate(((C_RQ, 3072, R_RQ), (C_RK, 3200, R_RK))):
                            p0 = fm(col)
                            p1 = fm(scol)
                            S.tt(r1[qi][:, 0:N], pA[p0][:, 0:N], ropet[gi][:, 2 * qi, 0:N], ALU.mult,
                                 R=["pA%d" % p0, "ropet%d" % gi], W=["r1%d" % qi])
                            S.tt(r2[qi][:, 0:N], pA[p1][:, 0:N], ropet[gi][:, 2 * qi + 1, 0:N], ALU.mult,
                                 R=["pA%d" % p1, "ropet%d" % gi], W=["r2%d" % qi])
                            fi = focnt % 3
                            focnt += 1
                            S.tt(fo[fi][:, 0:N], r1[qi][:, 0:N], r2[qi][:, 0:N], ALU.add, R=["r1%d" % qi, "r2%d" % qi],
                                 W=["fo%d" % fi], eng="gpsimd")
                            fo_store(fi, row)
                        if CUT == 5:
                            return
                        for si, t in enumerate(tiles):
                            ti = (tcnt + si) % 2
                            for (col, ncol, ocol, kind) in ((C_NAV, 512, 0, 0), (C_RV, 512, 512, 1), (C_GB, 256, 1024, 2)):
                                pi = pacnt % 6
                                pacnt += 1
                                for k in range(8):
                                    S.mm(pA[pi][:, 0:ncol], uxT[gi][:, k, si * 128:(si + 1) * 128], w[:, k, col:col + ncol],
                                         start=(k == 0), stop=(k == 7), R=["win", uk], W=["pA%d" % pi])
                                if kind == 0:
                                    S.copy(to[ti][:, 0:512], pA[pi][:, 0:512], R=["pA%d" % pi], W=["to%d" % ti])
                                elif kind == 1:
                                    S.copy(to[ti][:, 512:768], pA[pi][:, 0:256], R=["pA%d" % pi], W=["to%d" % ti], eng="scalar")
                                    S.actf(to[ti][:, 768:1024], pA[pi][:, 256:512], AF.Silu, R=["pA%d" % pi], W=["to%d" % ti])
                                else:
                                    S.actf(to[ti][:, 1024:1280], pA[pi][:, 0:256], AF.Silu, R=["pA%d" % pi], W=["to%d" % ti])
                            S.dma(PXV[b, t * 128:(t + 1) * 128, :], to[ti][:], R=["to%d" % ti], W=["PXV"], q="gpsimd")
                        if CUT == 6:
                            return
                        if (CUT == 7 and g == 3) or (CUT == 8 and g == 4):
                            return
            return f

        def stage_c(l, b):
            def f(stg):
                qT = sb(stg, "qT", [128, 4, NT], BF16)
                kT = sb(stg, "kT", [128, 4, NT], BF16)
                V = sb(stg, "V", [128, NTILE, NH, 65], BF16)
                ET = sb(stg, "ET", [128, NH * 15 * 64], F32)
                ETb = sb(stg, "ETb", [128, NH, 15, 64], BF16)
                PT = sb(stg, "PT", [128, NH, NPAT, 128], BF16)
                Pt = [sb(stg, "Pt%d" % i, [128, 7, 128], BF16) for i in range(3)]
                yc = [sb(stg, "yc%d" % i, [128, 512], BF16) for i in range(2)]
                rec = [sb(stg, "rec%d" % i, [128, 4], F32) for i in range(2)]
                pS = [psf(stg, "pS%d" % i) for i in range(4)]
                pO = [psf(stg, "pO%d" % i) for i in range(2)]
                S.dma(qT[:], PXT[b, R_NAQ:R_NAQ + 512, :].rearrange("(c p) t -> p c t", p=128), R=["PXT"], W=["qT"])
                S.dma(kT[:], PXT[b, R_NAK:R_NAK + 512, :].rearrange("(c p) t -> p c t", p=128), R=["PXT"], W=["kT"])
                S.memset(V[:], 1.0, W=["V"])
                for t0 in range(NTILE):
                    S.dma(V[:, t0, :, 0:64],
                          PXV[b, t0 * 128:(t0 + 1) * 128, 0:512].rearrange("p (h d) -> p h d", d=64),
                          R=["PXV"], W=["V"])
                S.dma(ET[0:64, :], napb[l], W=["ET"])
                S.dma(ET[64:128, :], napb[l], W=["ET"])
                S.actf(ETb[:].rearrange("p h r c -> p (h r c)"), ET[:], AF.Exp, R=["ET"], W=["ETb"])
                S.memset(PT[:], 0.0, W=["PT"])
                ci = 0
                for pid, quad in enumerate(pats):
                    for kr in range(2):
                        for qr in range(2):
                            dr = quad[kr * 2 + qr]
                            if dr < 0:
                                continue
                            eng = ("vector", "gpsimd")[ci % 2]
                            ci += 1
                            S.copy(PT[kr * 64:(kr + 1) * 64, :, pid, qr * 64:(qr + 1) * 64], ETb[kr * 64:(kr + 1) * 64, :, dr, :],
                                   R=["ETb"], W=["PT"], eng=eng)
                qtiles = list(range(2, NTILE)) + ([0, 1] if l == 0 else [])
                iters = []
                for qi_, tq in enumerate(qtiles):
                    if tq >= 2:
                        kts, pids = ptable[tq - 2]
                        ktoks = [kt + 2 for kt in kts] + [0, 1]
                        nl = len(kts)
                        assert pids == list(range(pids[0], pids[0] + nl))
                    else:
                        ktoks, nl, pids = [0, 1], 0, [0]
                    for h in range(NH):
                        iters.append((qi_, tq, h, ktoks, nl, pids[0]))

                def rec_scores(n):
                    qi_, tq, h, ktoks, nl, p0 = iters[n]
                    hp, hc = h % 2, h // 2
                    sl = slice(hp * 64, (hp + 1) * 64)
                    i2 = n % 2
                    for s_, kt in enumerate(ktoks):
                        bank = pS[2 * i2 + s_ // 4]
                        S.mm(bank[:, (s_ % 4) * 128:(s_ % 4 + 1) * 128], kT[sl, hc, kt * 128:(kt + 1) * 128],
                             qT[sl, hc, tq * 128:(tq + 1) * 128], R=["qT", "kT"], W=["pS%d" % (2 * i2 + s_ // 4)])

                def rec_softmax(n):
                    qi_, tq, h, ktoks, nl, p0 = iters[n]
                    i2, i3 = n % 2, n % 3
                    ns = len(ktoks)
                    n0 = min(ns, 4)
                    S.actf(Pt[i3][:, 0:n0, :].rearrange("p s q -> p (s q)"), pS[2 * i2][:, 0:n0 * 128], AF.Exp,
                           R=["pS%d" % (2 * i2)], W=["Pt%d" % i3], scale=0.125)
                    if ns > 4:
                        S.actf(Pt[i3][:, 4:ns, :].rearrange("p s q -> p (s q)"), pS[2 * i2 + 1][:, 0:(ns - 4) * 128], AF.Exp,
                               R=["pS%d" % (2 * i2 + 1)], W=["Pt%d" % i3], scale=0.125)
                    if nl:
                        S.tt(Pt[i3][:, 0:nl, :], Pt[i3][:, 0:nl, :], PT[:, h, p0:p0 + nl, :], ALU.mult,
                             R=["Pt%d" % i3, "PT"], W=["Pt%d" % i3], eng=("vector", "gpsimd")[n % 2])

                def rec_pv(n):
                    qi_, tq, h, ktoks, nl, p0 = iters[n]
                    i3 = n % 3
                    ns = len(ktoks)
                    yi = qi_ % 2
                    og = h // 4
                    hq = h % 4
                    for s_, kt in enumerate(ktoks):
                        S.mm(pO[og][:, hq * 65:(hq + 1) * 65], Pt[i3][:, s_, :], V[:, kt, h, :], start=(s_ == 0), stop=(s_ == ns - 1),
                             R=["Pt%d" % i3, "V"], W=["pO%d" % og])
                    if hq == 3:
                        pv = pO[og][:, 0:260].rearrange("p (h d) -> p h d", d=65)
                        S.recip(rec[og][:].unsqueeze(2), pv[:, :, 64:65], R=["pO%d" % og], W=["rec%d" % og])
                        S.tt(yc[yi][:, (h - 3) * 64:(h + 1) * 64].rearrange("p (h d) -> p h d", d=64), pv[:, :, 0:64],
                             rec[og][:].unsqueeze(2).to_broadcast([128, 4, 64]), ALU.mult,
                             R=["pO%d" % og, "rec%d" % og], W=["yc%d" % yi])
                    if h == NH - 1:
                        S.dma(YC[b, tq * 128:(tq + 1) * 128, 0:512], yc[yi][:], R=["yc%d" % yi], W=["YC"], q="gpsimd")

                rec_scores(0)
                for n in range(len(iters)):
                    rec_softmax(n)
                    if n + 1 < len(iters):
                        rec_scores(n + 1)
                    rec_pv(n)
            return f

        def stage_d(l, b):
            def f(stg):
                ylat = sb(stg, "ylat", [128, 2, T + 32], BF16)
                yctx = sb(stg, "yctx", [128, 2, LC + 32], BF16)
                cw = sb(stg, "cw", [128, 2, 31], F32)
                cpar = sb(stg, "cpar", [128, 3, 2], F32)
                diag = sb(stg, "diag", [128, 2, 31, 128], BF16)
                ones = sb(stg, "ones", [128, 128], F32)
                zc = [sb(stg, "zc%d" % i, [128, 512], F32) for i in range(2)]
                sq = [sb(stg, "sq%d" % i, [128, 512], F32) for i in range(2)]
                mean = sb(stg, "mean", [128, 512], F32)
                var = sb(stg, "var", [128, 512], F32)
                dd = [sb(stg, "dd%d" % i, [128, 512], F32) for i in range(2)]
                yo = [sb(stg, "yo%d" % i, [128, 512], BF16) for i in range(2)]
                pc = [psf(stg, "pc%d" % i) for i in range(4)]
                pm = [psf(stg, "pmn%d" % i) for i in range(2)]
                S.memset(ylat[:], 0.0, W=["ylat"])
                S.memset(yctx[:], 0.0, W=["yctx"])
                S.memset(ones[:], 1.0 / 256, W=["ones"])
                S.dma(ylat[:, :, 15:15 + T], PXT[b, R_CV:R_CV + 256, LC:NT].rearrange("(c p) t -> p c t", p=128), R=["PXT"], W=["ylat"])
                if l == 0:
                    S.dma(yctx[:, :, 15:15 + LC], PXT[b, R_CV:R_CV + 256, 0:LC].rearrange("(c p) t -> p c t", p=128), R=["PXT"], W=["yctx"])
                S.dma(cw[:], convwT[l], W=["cw"])
                S.dma(cpar[:], cpard[l], W=["cpar"])
                for c in range(2):
                    for j in range(31):
                        S.ts(diag[:, c, j, :], id32[:], cw[:, c, j:j + 1], None, ALU.mult, R=["cw", "id32"], W=["diag"],
                             eng=("vector", "gpsimd")[j % 2])
                blocks = [("lat", ylat, tb * 512, 512, LC + tb * 512) for tb in range(4)]
                if l == 0:
                    blocks.append(("ctx", yctx, 0, 256, 0))
                for bi, (_, ybuf, off, N, tok0) in enumerate(blocks):
                    ykey = "ylat" if ybuf is ylat else "yctx"
                    for c in range(2):
                        p = pc[(2 * bi + c) % 4]
                        pk = "pc%d" % ((2 * bi + c) % 4)
                        for j in range(31):
                            S.mm(p[:, 0:N], diag[:, c, j, :], ybuf[:, c, off + j:off + j + N], start=(j == 0), stop=(j == 30),
                                 R=["diag", ykey], W=[pk])
                        S.actf(zc[c][:, 0:N], p[:, 0:N], AF.Identity, R=[pk, "cpar"], W=["zc%d" % c], bias=cpar[:, 0, c:c + 1])
                        S.actf(sq[c][:, 0:N], p[:, 0:N], AF.Square, R=[pk, "cpar"], W=["sq%d" % c], bias=cpar[:, 0, c:c + 1])
                    for c in range(2):
                        S.mm(pm[0][:, 0:N], ones[:], zc[c][:, 0:N], start=(c == 0), stop=(c == 1), R=["ones", "zc%d" % c], W=["pmn0"])
                    for c in range(2):
                        S.mm(pm[1][:, 0:N], ones[:], sq[c][:, 0:N], start=(c == 0), stop=(c == 1), R=["ones", "sq%d" % c], W=["pmn1"])
                    S.copy(mean[:, 0:N], pm[0][:, 0:N], R=["pmn0"], W=["mean"], eng="scalar")
                    S.tt(var[:, 0:N], mean[:, 0:N], mean[:, 0:N], ALU.mult, R=["mean"], W=["var"])
                    S.tt(var[:, 0:N], pm[1][:, 0:N], var[:, 0:N], ALU.subtract, R=["pmn1", "var"], W=["var"])
                    S.actf(var[:, 0:N], var[:, 0:N], AF.Sqrt, R=["var", "epsb"], W=["var"], bias=epsb[:, 0:1])
                    S.recip(var[:, 0:N], var[:, 0:N], R=["var"], W=["var"])
                    for c in range(2):
                        S.tt(dd[c][:, 0:N], zc[c][:, 0:N], mean[:, 0:N], ALU.subtract, R=["zc%d" % c, "mean"], W=["dd%d" % c])
                        S.tt(dd[c][:, 0:N], dd[c][:, 0:N], var[:, 0:N], ALU.mult, R=["dd%d" % c, "var"], W=["dd%d" % c], eng="gpsimd")
                        S.actf(yo[c][:, 0:N], dd[c][:, 0:N], AF.Silu, R=["dd%d" % c, "cpar"], W=["yo%d" % c],
                               scale=cpar[:, 1, c:c + 1], bias=cpar[:, 2, c:c + 1])
                        S.dma(YT[b, c * 128:(c + 1) * 128, tok0:tok0 + N], yo[c][:, 0:N], R=["yo%d" % c], W=["YT"], q="gpsimd")
            return f

        def stage_e(l, b):
            def f(stg):
                qT = sb(stg, "rqT", [32, 4, NT], BF16)
                kT = sb(stg, "rkT", [32, 4, NT], BF16)
                ktm = sb(stg, "ktm", [128, NTILE, 128], BF16)
                V = sb(stg, "rV", [128, NTILE, 768], BF16)
                rc = sb(stg, "rc", [128, 772], F32)
                dec = sb(stg, "dec", [128, 8], F32)
                lg = sb(stg, "lg", [128, 8], F32)
                intra = sb(stg, "intra", [128, 2, 4, 128], F32)
                QD = sb(stg, "QD", [32, 2, 4, 128], F32)
                KD = sb(stg, "KD", [128, 2, 4], F32)
                CD = sb(stg, "CD", [32, 2, 4], F32)
                gnw = sb(stg, "gnw", [128, 256], F32)
                S32 = [sb(stg, "S32_%d" % i, [32, 4, 64], F32) for i in range(2)]
                Sbf = [sb(stg, "Sbf_%d" % i, [32, 4, 64], BF16) for i in range(2)]
                Pm = [sb(stg, "Pm%d" % i, [128, 4, 128], BF16) for i in range(2)]
                qd = [sb(stg, "qd%d" % i, [32, 4, 128], BF16) for i in range(2)]
                kd = [sb(stg, "kd%d" % i, [128, 4, 32], BF16) for i in range(2)]
                Oall = [sb(stg, "Oall%d" % i, [128, NTILE, 256], F32) for i in range(2)]
                sqa = sb(stg, "sqa", [128, NTILE, 256], F32)
                stt_ = [sb(stg, "stt%d" % i, [128, 3, 4 * NTILE], F32) for i in range(2)]
                Yo = sb(stg, "Yo", [128, NTILE, 256], BF16)
                pST = [psf(stg, "pST%d" % i) for i in range(2)]
                pOo = [psf(stg, "pOo%d" % i) for i in range(2)]
                pSs = [psf(stg, "pSs%d" % i) for i in range(2)]
                pK = psb(stg, "pK")
                S.dma(qT[:], PXT[b, R_RQ:R_RQ + 128, :].rearrange("(h d) t -> d h t", d=32), R=["PXT"], W=["rqT"])
                S.dma(kT[:], PXT[b, R_RK:R_RK + 128, :].rearrange("(h d) t -> d h t", d=32), R=["PXT"], W=["rkT"])
                for t0 in range(0, NTILE, 6):
                    S.dma(V[:, t0:t0 + 6, :], PXV[b, t0 * 128:(t0 + 6) * 128, 512:1280].rearrange("(t p) c -> p t c", p=128),
                          R=["PXV"], W=["rV"])
                S.dma(rc[:], retc, W=["rc"])
                S.dma(dec[:], ret_decay[l:l + 1, :].partition_broadcast(128), W=["dec"])
                S.dma(gnw[:], ret_gn_w[l:l + 1, :].partition_broadcast(128), W=["gnw"])
                S.actf(lg[:], dec[:], AF.Exp, R=["dec"], W=["lg"], scale=-float(np.log(2.0)))
                S.ts(lg[:], lg[:], -1.0, 1.0, ALU.mult, ALU.add, R=["lg"], W=["lg"])
                S.actf(lg[:], lg[:], AF.Ln, R=["lg"], W=["lg"])
                Dm = {0: rc[:, 0:128], 1: rc[:, 256:384]}
                Mm = {0: rc[:, 128:256], 1: rc[:, 384:512]}
                R12 = {0: rc[0:32, 512:640], 1: rc[0:32, 640:768]}
                C12 = {0: rc[:, 768:769], 1: rc[:, 769:770]}
                for d_ in range(2):
                    for h in range(4):
                        S.actf(intra[:, d_, h, :], Dm[d_], AF.Exp, R=["rc", "lg"], W=["intra"], scale=lg[:, d_ * 4 + h:d_ * 4 + h + 1])
                        S.tt(intra[:, d_, h, :], intra[:, d_, h, :], Mm[d_], ALU.mult, R=["intra", "rc"], W=["intra"])
                        S.actf(QD[:, d_, h, :], R12[d_], AF.Exp, R=["rc", "lg"], W=["QD"], scale=lg[0:32, d_ * 4 + h:d_ * 4 + h + 1])
                    S.actf(KD[:, d_, :], lg[:, d_ * 4:d_ * 4 + 4], AF.Exp, R=["rc", "lg"], W=["KD"], scale=C12[d_])
                    S.actf(CD[:, d_, :], lg[0:32, d_ * 4:d_ * 4 + 4], AF.Exp, R=["lg"], W=["CD"], scale=128.0)
                for t in range(NTILE):
                    for h in range(4):
                        S.tr(pK[:, h * 32:(h + 1) * 32], kT[:, h, t * 128:(t + 1) * 128], idb[0:32, 0:32], R=["rkT", "idb"], W=["pK"])
                    S.copy(ktm[:, t, :], pK[:, 0:128], R=["pK"], W=["ktm"], eng=("vector", "scalar")[t % 2])
                for d_ in range(2):
                    S.memset(S32[d_][:], 0.0, W=["S32_%d" % d_])
                    S.memset(Sbf[d_][:], 0.0, W=["Sbf_%d" % d_])
                order = {0: list(range(NTILE)), 1: [1, 0] + list(range(NTILE - 1, 1, -1))}
                for step in range(NTILE):
                    for d_ in range(2):
                        c = order[d_][step]
                        cs = slice(c * 128, (c + 1) * 128)
                        D_ = "%d" % d_
                        for h in range(4):
                            S.mm(pST[d_][:, h * 128:(h + 1) * 128], kT[:, h, cs], qT[:, h, cs], R=["rkT", "rqT"], W=["pST" + D_])
                        S.tt(Pm[d_][:], pST[d_][:].rearrange("p (h i) -> p h i", h=4), intra[:, d_, :, :], ALU.mult,
                             R=["pST" + D_, "intra"], W=["Pm" + D_])
                        S.tt(qd[d_][:], qT[:, :, cs], QD[:, d_, :, :], ALU.mult, R=["rqT", "QD"], W=["qd" + D_], eng="gpsimd")
                        S.tt(kd[d_][:], ktm[:, c, :].rearrange("p (h d) -> p h d", h=4),
                             KD[:, d_, :].unsqueeze(2).to_broadcast([128, 4, 32]), ALU.mult, R=["ktm", "KD"], W=["kd" + D_], eng="gpsimd")
                        for h in range(4):
                            S.mm(pOo[d_][:, h * 64:(h + 1) * 64], Pm[d_][:, h, :], V[:, c, h * 64:(h + 1) * 64], start=True, stop=False,
                                 R=["Pm" + D_, "rV"], W=["pOo" + D_])
                            S.mm(pOo[d_][:, h * 64:(h + 1) * 64], qd[d_][:, h, :], Sbf[d_][:, h, :], start=False, stop=True,
                                 R=["qd" + D_, "Sbf_" + D_], W=["pOo" + D_])
                        for h in range(4):
                            S.mm(pSs[d_][0:32, h * 64:(h + 1) * 64], kd[d_][:, h, :], V[:, c, h * 64:(h + 1) * 64],
                                 R=["kd" + D_, "rV"], W=["pSs" + D_])
                        S.tt(S32[d_][:], S32[d_][:], CD[:, d_, :].unsqueeze(2).to_broadcast([32, 4, 64]), ALU.mult,
                             R=["S32_" + D_, "CD"], W=["S32_" + D_])
                        S.tt(S32[d_][:], S32[d_][:], pSs[d_][0:32, 0:256].rearrange("p (h e) -> p h e", h=4), ALU.add,
                             R=["S32_" + D_, "pSs" + D_], W=["S32_" + D_])
                        S.copy(Sbf[d_][:], S32[d_][:], R=["S32_" + D_], W=["Sbf_" + D_])
                        S.copy(Oall[d_][:, c, :], pOo[d_][:, 0:256], R=["pOo" + D_], W=["Oall" + D_], eng="scalar")
                for d_ in range(2):
                    D_ = "%d" % d_
                    ok = "Oall" + D_
                    Of = Oall[d_][:].rearrange("p c f -> p (c f)")
                    O3 = Oall[d_][:].rearrange("p c (h e) -> p (c h) e", e=64)
                    s1, s3 = stt_[d_][:, 0, :], stt_[d_][:, 1, :]
                    sk = "stt" + D_
                    S.actf(sqa[:].rearrange("p c f -> p (c f)"), Of, AF.Square, R=[ok], W=["sqa"])
                    S.rsum(s1, O3, R=[ok], W=[sk])
                    S.rsum(s3, sqa[:].rearrange("p c (h e) -> p (c h) e", e=64), R=["sqa"], W=[sk])
                    S.ts(s1, s1, 1.0 / 64, None, ALU.mult, R=[sk], W=[sk])
                    S.ts(s3, s3, 1.0 / 64, None, ALU.mult, R=[sk], W=[sk])
                    S.tt(stt_[d_][:, 2, :], s1, s1, ALU.mult, R=[sk], W=[sk])
                    S.tt(s3, s3, stt_[d_][:, 2, :], ALU.subtract, R=[sk], W=[sk])
                    S.actf(s3, s3, AF.Sqrt, R=[sk, "epsb"], W=[sk], bias=epsb[:, 0:1])
                    S.recip(s3, s3, R=[sk], W=[sk])
                    S.tt(O3, O3, s1.unsqueeze(2).to_broadcast([128, 4 * NTILE, 64]), ALU.subtract, R=[ok, sk], W=[ok])
                    S.tt(O3, O3, s3.unsqueeze(2).to_broadcast([128, 4 * NTILE, 64]), ALU.mult, R=[ok, sk], W=[ok],
                         eng=("gpsimd", "vector")[d_])
                    S.tt(Oall[d_][:], Oall[d_][:], gnw[:].unsqueeze(1).to_broadcast([128, NTILE, 256]), ALU.mult, R=[ok, "gnw"], W=[ok],
                         eng=("vector", "gpsimd")[d_])
                    S.tt(Oall[d_][:], Oall[d_][:], V[:, :, 256 + 256 * d_:512 + 256 * d_], ALU.mult, R=[ok, "rV"], W=[ok],
                         eng=("gpsimd", "vector")[d_])
                S.tt(Yo[:], Oall[0][:], Oall[1][:], ALU.add, R=["Oall0", "Oall1"], W=["Yo"])
                S.dma(YC[b, :, 512:768].rearrange("(t p) c -> p t c", p=128), Yo[:], R=["Yo"], W=["YC"], q="gpsimd")
            return f

        def stage_f(l, b):
            last = l == 1

            def f(stg):
                wo = sb(stg, "wo", [128, 8, D], BF16)
                mb = sb(stg, "mbF", [128, 6, D], F32)
                ycs = [sb(stg, "ycs%d" % i, [128, 768], BF16) for i in range(2)]
                ycv = [sb(stg, "ycv%d" % i, [128, 2, 128], BF16) for i in range(2)]
                ycT = [sb(stg, "ycT%d" % i, [128, 6, 128], BF16) for i in range(2)]
                ht = [sb(stg, "htF%d" % i, [128, D], F32) for i in range(2)]
                hn = [sb(stg, "hn%d" % i, [128, D], F32) for i in range(2)]
                tmp = sb(stg, "tmpF", [128, D], F32)
                junk = sb(stg, "junkF", [128, D], BF16)
                u32 = [sb(stg, "u32_%d" % i, [128, D], F32) for i in range(2)]
                ubf = [sb(stg, "ubf%d" % i, [128, D], BF16) for i in range(2)]
                uTs = [sb(stg, "uTs%d" % i, [128, 8, 128], BF16) for i in range(2)]
                ss = sb(stg, "ssF", [128, 2], F32)
                rstd = sb(stg, "rstdF", [128, 2], F32)
                pT = [psb(stg, "pTF%d" % i) for i in range(2)]
                pY = [psf(stg, "pY%d" % i) for i in range(4)]
                for k in range(8):
                    S.dma(wo[:, k, :], w_out[l, k * 128:(k + 1) * 128, :], W=["wo"], q="gpsimd")
                for s_, (j, slot) in enumerate(((2, 2), (2, 4), (2, 3), (b, 2), (b, 4), (b, 3))):
                    if last and s_ < 3:
                        continue
                    S.dma(mb[:, s_, :], modbc(None, l, j, slot), R=["MODS"], W=["mbF"])
                if last:
                    wr = sb(stg, "wr", [128, NE, D], F32)
                    rb = sb(stg, "rb", [128, NE], F32)
                    lgt = [sb(stg, "lgt%d" % i, [128, 32], F32) for i in range(2)]
                    for e_ in range(NE):
                        S.dma(wr[:, e_, :], moe_routerT[e_:e_ + 1, :].partition_broadcast(128), W=["wr"])
                    S.dma(rb[:], moe_rb.partition_broadcast(128), W=["rb"])
                tiles = list(range(2, NTILE)) if last else list(range(NTILE))
                junkR = sb(stg, "junkR", [128, D], BF16)

                def front(ti, t):
                    i = ti % 2
                    I_ = "%d" % i
                    mo = 0 if t < 2 else 3
                    tsl = slice(t * 128, (t + 1) * 128)
                    S.dma(ycs[i][:], YC[b, tsl, :], R=["YC"], W=["ycs" + I_])
                    S.dma(ycv[i][:], YT[b, :, tsl].rearrange("(c p) t -> p c t", p=128), R=["YT"], W=["ycv" + I_])
                    S.dma(ht[i][:], hsrc(l, b, t), R=["HB"], W=["htF" + I_])
                    for k in range(6):
                        S.tr(pT[i][:, k * 128:(k + 1) * 128], ycs[i][:, k * 128:(k + 1) * 128], idb[:], R=["ycs" + I_, "idb"], W=["pTF" + I_])
                    S.copy(ycT[i][:].rearrange("p k n -> p (k n)"), pT[i][:, 0:768], R=["pTF" + I_], W=["ycT" + I_], eng="scalar")
                    lhs = [ycT[i][:, 0, :], ycT[i][:, 1, :], ycT[i][:, 2, :], ycT[i][:, 3, :], ycv[i][:, 0, :], ycv[i][:, 1, :],
                           ycT[i][:, 4, :], ycT[i][:, 5, :]]
                    for nb in range(2):
                        pk = "pY%d" % (2 * i + nb)
                        for k in range(8):
                            S.mm(pY[2 * i + nb][:], lhs[k], wo[:, k, nb * 512:(nb + 1) * 512], start=(k == 0), stop=(k == 7),
                                 R=["ycT" + I_, "ycv" + I_, "wo"], W=[pk])
                        hs = slice(nb * 512, (nb + 1) * 512)
                        S.tt(tmp[:, hs], pY[2 * i + nb][:], mb[:, mo, hs], ALU.mult, R=[pk, "mbF"], W=["tmpF%d" % nb])
                        S.tt(hn[i][:, hs], ht[i][:, hs], tmp[:, hs], ALU.add, R=["htF" + I_, "tmpF%d" % nb], W=["hn" + I_], eng="gpsimd")
                    S.dma(HA[b, tsl, :], hn[i][:], R=["hn" + I_], W=["HA"], q="gpsimd")
                    S.actf(junk[:], hn[i][:], AF.Square, R=["hn" + I_], W=["junkF", "ssF" + I_], accum_out=ss[:, i:i + 1])
                    S.actf(rstd[:, i:i + 1], ss[:, i:i + 1], AF.Sqrt, R=["ssF" + I_, "epsb"], W=["rstdF" + I_], scale=1.0 / D, bias=epsb[:, 0:1])
                    S.recip(rstd[:, i:i + 1], rstd[:, i:i + 1], R=["rstdF" + I_], W=["rstdF" + I_])
                    S.stt(u32[i][:], hn[i][:], rstd[:, i:i + 1], mb[:, mo + 1, :], ALU.mult, ALU.mult,
                          R=["hn" + I_, "rstdF" + I_, "mbF"], W=["u32_" + I_])
                    S.tt(u32[i][:], u32[i][:], mb[:, mo + 2, :], ALU.add, R=["u32_" + I_, "mbF"], W=["u32_" + I_])
                    S.copy(ubf[i][:], u32[i][:], R=["u32_" + I_], W=["ubf" + I_], eng="scalar")

                def back(ti, t):
                    i = ti % 2
                    I_ = "%d" % i
                    mo = 0 if t < 2 else 3
                    tsl = slice(t * 128, (t + 1) * 128)
                    for k in range(8):
                        S.tr(pT[i][:, k * 128:(k + 1) * 128], ubf[i][:, k * 128:(k + 1) * 128], idb[:], R=["ubf" + I_, "idb"], W=["pTF" + I_])
                    S.copy(uTs[i][:].rearrange("p k n -> p (k n)"), pT[i][:], R=["pTF" + I_], W=["uTs" + I_])
                    S.dma(UT[b, :, tsl].rearrange("(k p) t -> p k t", p=128), uTs[i][:], R=["uTs" + I_], W=["UT"], q="gpsimd")
                    if last:
                        lg_ = lgt[i]
                        lk = "lgt" + I_
                        for e_ in range(NE):
                            S.stt(junkR[:], u32[i][:], 1.0, wr[:, e_, :], ALU.mult, ALU.mult, R=["u32_" + I_, "wr"],
                                  W=["junkR", lk], accum_out=lg_[:, e_:e_ + 1])
                        S.tt(lg_[:, 0:8], lg_[:, 0:8], rb[:], ALU.add, R=[lk, "rb"], W=[lk])
                        S.op("vector", lambda e, a=lg_: e.max(out=a[:, 8:16], in_=a[:, 0:8]), R=[lk], W=[lk])
                        S.ts(lg_[:, 16:24], lg_[:, 0:8], lg_[:, 8:9], None, ALU.is_equal, R=[lk], W=[lk])
                        S.ts(lg_[:, 24:32], lg_[:, 0:8], lg_[:, 9:10], None, ALU.is_equal, R=[lk], W=[lk])
                        S.tt(lg_[:, 10:11], lg_[:, 8:9], lg_[:, 9:10], ALU.subtract, R=[lk], W=[lk])
                        S.actf(lg_[:, 11:12], lg_[:, 10:11], AF.Sigmoid, R=[lk], W=[lk])
                        S.actf(lg_[:, 12:13], lg_[:, 10:11], AF.Sigmoid, R=[lk], W=[lk], scale=-1.0)
                        S.ts(lg_[:, 16:24], lg_[:, 16:24], lg_[:, 11:12], None, ALU.mult, R=[lk], W=[lk])
                        S.stt(lg_[:, 0:8], lg_[:, 24:32], lg_[:, 12:13], lg_[:, 16:24], ALU.mult, ALU.add, R=[lk], W=[lk])
                        S.dma(COMB[b, tsl, :], lg_[:, 0:8], R=[lk], W=["COMB"], q="gpsimd")

                segs = [list(enumerate(tiles))]
                for si_, seg in enumerate(segs):
                    if si_ > 0:
                        yield
                    front(*seg[0])
                    for j in range(len(seg)):
                        if j + 1 < len(seg):
                            front(*seg[j + 1])
                        back(*seg[j])
            return f

        def stage_g(l, b):
            last = l == 1

            def f(stg):
                t0 = 2 if last else 0
                ntile = NTILE - t0
                ntok = ntile * 128
                tok0 = t0 * 128
                nexp = NE if last else 1
                dff = D_FFE if last else D_FF
                uT = sb(stg, "uT", [128, 8, ntok], BF16)
                yacc = sb(stg, "yacc", [128, ntile, D], F32)
                wg = [sb(stg, "wg%d" % i, [128, 8, 512], BF16) for i in range(2)]
                wu = [sb(stg, "wu%d" % i, [128, 8, 512], BF16) for i in range(2)]
                wd = [sb(stg, "wd%d" % i, [128, 4, D], BF16) for i in range(2)]
                act = [sb(stg, "act%d" % i, [128, 4, 512], BF16) for i in range(2)]
                sg = [sb(stg, "sgG%d" % i, [128, 512], F32) for i in range(2)]
                comb = sb(stg, "comb", [128, ntile, NE], F32)
                pG = [psf(stg, "pG%d" % i) for i in range(2)]
                pU = [psf(stg, "pU%d" % i) for i in range(2)]
                pD = [psf(stg, "pD%d" % i) for i in range(4)]
                for k in range(8):
                    S.dma(uT[:, k, :], UT[b, k * 128:(k + 1) * 128, tok0:NT], R=["UT"], W=["uT"])
                if last:
                    S.dma(comb[:], COMB[b, tok0:NT, :].rearrange("(t p) e -> p t e", p=128), R=["COMB"], W=["comb"])
                S.memset(yacc[:], 0.0, W=["yacc"])
                groups = []
                for e_ in range(nexp):
                    for f0 in range(0, dff, 512):
                        groups.append((e_, f0, min(512, dff - f0)))
                tgs = [(s0, min(512, ntok - s0)) for s0 in range(0, ntok, 512)]
                cnt = {"gu": 0, "pd": 0, "sg": 0, "act": 0}

                def rec_gu(u):
                    gi, e_, f0, fw, s0, N, ai = u
                    wi = gi % 2
                    W_ = "%d" % wi
                    A_ = "%d" % ai
                    for c in range(fw // 128):
                        pi = cnt["gu"] % 2
                        cnt["gu"] += 1
                        for k in range(8):
                            S.mm(pG[pi][:, 0:N], wg[wi][:, k, c * 128:(c + 1) * 128], uT[:, k, s0:s0 + N], start=(k == 0), stop=(k == 7),
                                 R=["wg" + W_, "uT"], W=["pG%d" % pi])
                        for k in range(8):
                            S.mm(pU[pi][:, 0:N], wu[wi][:, k, c * 128:(c + 1) * 128], uT[:, k, s0:s0 + N], start=(k == 0), stop=(k == 7),
                                 R=["wu" + W_, "uT"], W=["pU%d" % pi])
                        si = cnt["sg"] % 2
                        cnt["sg"] += 1
                        S.actf(sg[si][:, 0:N], pG[pi][:, 0:N], AF.Silu, R=["pG%d" % pi], W=["sgG%d" % si])
                        S.tt(act[ai][:, c, 0:N], pU[pi][:, 0:N], sg[si][:, 0:N], ALU.mult, R=["pU%d" % pi, "sgG%d" % si], W=["act" + A_])

                def rec_down(u):
                    gi, e_, f0, fw, s0, N, ai = u
                    wi = gi % 2
                    W_ = "%d" % wi
                    A_ = "%d" % ai
                    nch = fw // 128
                    for s_ in range(N // 128):
                        tl = s0 // 128 + s_
                        for nb in range(2):
                            di = cnt["pd"] % 4
                            cnt["pd"] += 1
                            for c in range(nch):
                                S.mm(pD[di][:], act[ai][:, c, s_ * 128:(s_ + 1) * 128], wd[wi][:, c, nb * 512:(nb + 1) * 512],
                                     start=(c == 0), stop=(c == nch - 1), R=["act" + A_, "wd" + W_], W=["pD%d" % di])
                            ya = yacc[:, tl, nb * 512:(nb + 1) * 512]
                            yk = "yacc%d_%d" % (tl, nb)
                            sc = comb[:, tl, e_:e_ + 1] if last else 1.0
                            S.stt(ya, pD[di][:], sc, ya, ALU.mult, ALU.add, R=["pD%d" % di, "comb", "yacc", yk], W=[yk])

                def rec_wload(gi, e_, f0, fw):
                    wi = gi % 2
                    W_ = "%d" % wi
                    nch = fw // 128
                    gsrc = (moe_wg[e_] if last else ffn_wg[0])
                    usrc = (moe_wu[e_] if last else ffn_wu[0])
                    dsrc = (moe_wd[e_] if last else ffn_wd[0])
                    S.dma(wg[wi][:, :, 0:fw], gsrc[:, f0:f0 + fw].rearrange("(k p) n -> p k n", p=128), W=["wg" + W_], q="gpsimd")
                    S.dma(wu[wi][:, :, 0:fw], usrc[:, f0:f0 + fw].rearrange("(k p) n -> p k n", p=128), W=["wu" + W_], q="gpsimd")
                    S.dma(wd[wi][:, 0:nch, :], dsrc[f0:f0 + fw, :].rearrange("(c p) n -> p c n", p=128), W=["wd" + W_], q="gpsimd")

                units = []
                for gi, (e_, f0, fw) in enumerate(groups):
                    for (s0, N) in tgs:
                        units.append((gi, e_, f0, fw, s0, N, cnt["act"] % 2))
                        cnt["act"] += 1
                loaded = set()

                def ensure(u):
                    if u[0] not in loaded:
                        loaded.add(u[0])
                        rec_wload(u[0], u[1], u[2], u[3])

                ensure(units[0])
                rec_gu(units[0])
                for n, u in enumerate(units):
                    if n + 1 < len(units):
                        ensure(units[n + 1])
                        rec_gu(units[n + 1])
                    rec_down(u)
                g2 = sb(stg, "g2", [128, 2, D], F32)
                S.dma(g2[:, 1, :], modbc(None, l, b, 5), R=["MODS"], W=["g2"])
                if not last:
                    S.dma(g2[:, 0, :], modbc(None, l, 2, 5), R=["MODS"], W=["g2"])
                else:
                    S.dma(g2[:, 0, :], final_w.partition_broadcast(128), W=["g2"])
                hh = [sb(stg, "hh%d" % i, [128, D], F32) for i in range(2)]
                ssb = sb(stg, "ssG", [128, 2], F32)
                rsb = sb(stg, "rsG", [128, 2], F32)
                junk = sb(stg, "junkG", [128, D], BF16)
                for tl in range(ntile):
                    t = t0 + tl
                    i = tl % 2
                    I_ = "%d" % i
                    tsl = slice(t * 128, (t + 1) * 128)
                    ykeys = ["yacc", "yacc%d_0" % tl, "yacc%d_1" % tl]
                    S.dma(hh[i][:], HA[b, tsl, :], R=["HA"], W=["hh" + I_])
                    gsel = 1 if (t >= 2) else 0
                    S.tt(yacc[:, tl, :], yacc[:, tl, :], g2[:, gsel if not last else 1, :], ALU.mult, R=ykeys + ["g2"], W=ykeys[1:])
                    S.tt(hh[i][:], hh[i][:], yacc[:, tl, :], ALU.add, R=ykeys + ["hh" + I_], W=["hh" + I_], eng="gpsimd")
                    if not last:
                        S.dma(HB[b, tsl, :], hh[i][:], R=["hh" + I_], W=["HB"], q="gpsimd")
                    else:
                        S.actf(junk[:], hh[i][:], AF.Square, R=["hh" + I_], W=["junkG", "ssG" + I_], accum_out=ssb[:, i:i + 1])
                        S.actf(rsb[:, i:i + 1], ssb[:, i:i + 1], AF.Sqrt, R=["ssG" + I_, "epsb"], W=["rsG" + I_], scale=1.0 / D, bias=epsb[:, 0:1])
                        S.recip(rsb[:, i:i + 1], rsb[:, i:i + 1], R=["rsG" + I_], W=["rsG" + I_])
                        S.stt(hh[i][:], hh[i][:], rsb[:, i:i + 1], g2[:, 0, :], ALU.mult, ALU.mult, R=["hh" + I_, "rsG" + I_, "g2"], W=["hh" + I_])
                        S.dma(out[b, (t - 2) * 128:(t - 1) * 128, :], hh[i][:], R=["hh" + I_], W=["out"], q="gpsimd")
            return f

        prog = []
        for l in range(2):
            if P1:
                prog.append(stage_b(l, (0, 1)))
                for b in range(2):
                    prog.append(stage_c(l, b))
                    prog.append(stage_d(l, b))
                    prog.append(stage_e(l, b))
                for b in range(2):
                    prog.append(stage_f(l, b))
            for b in range(2):
                if (l == 0 and P1) or (l == 1 and P2 and b in bs):
                    prog.append(stage_g(l, b))
        for i, fn in enumerate(prog):
            if i >= upto:
                break
            stage(fn)
    return nc


def host_consts(na_rpb):
    ident = np.eye(128, dtype=np.float32)
    t = np.arange(T)
    row = (t // GRID_W).astype(np.float32)
    col = (t % GRID_W).astype(np.float32)
    inv = (np.float32(10000.0) ** (-np.arange(0, 16, 2, dtype=np.float32) / np.float32(16))).astype(np.float32)
    ang = np.concatenate([row[:, None] * inv, col[:, None] * inv], axis=-1).astype(np.float32)
    cos, sin = np.cos(ang).astype(np.float32), np.sin(ang).astype(np.float32)
    cos2 = np.ones((32, NT), np.float32)
    sinS = np.zeros((32, NT), np.float32)
    cos2[0:16, LC:] = cos.T
    cos2[16:32, LC:] = cos.T
    sinS[0:16, LC:] = -sin.T
    sinS[16:32, LC:] = sin.T
    ks = np.float32(32 ** -0.5)
    rope = np.stack([np.tile(cos2, (4, 1)), np.tile(sinS, (4, 1)), np.tile(cos2, (4, 1)) * ks, np.tile(sinS, (4, 1)) * ks]).astype(np.float32)
    j = np.arange(128)[:, None].astype(np.float32)
    i = np.arange(128)[None, :].astype(np.float32)
    retc = np.zeros((128, 772), np.float32)
    retc[:, 0:128] = np.maximum(i - j, 0)
    retc[:, 128:256] = (i >= j)
    retc[:, 256:384] = np.maximum(j - i, 0)
    retc[:, 384:512] = (j >= i)
    retc[:, 512:640] = i + 1.0
    retc[:, 640:768] = 128.0 - i
    retc[:, 768] = 127.0 - j[:, 0]
    retc[:, 769] = j[:, 0]
    kc = np.arange(64)[:, None]
    qc = np.arange(64)[None, :]
    w0 = np.clip(qc - 8, 0, 48)
    ok = (kc >= w0) & (kc < w0 + 16)
    dc = np.clip(kc - qc + 15, 0, 30)
    g = na_rpb[:, :, :, dc]
    g = np.where(ok[None, None, None], g, np.float32(-30000.0)).astype(np.float32)
    napb = np.ascontiguousarray(g.transpose(0, 3, 1, 2, 4)).reshape(2, 64, NH * 15 * 64)
    return ident, rope, retc, napb


_CACHE = {}
LAUNCH2 = ((0,), (1,))
SINGLE = True


def kernel(x, c, ctx, c_ctx, w_mod, b_mod, norm1_w, norm2_w, w_in, w_out, na_rpb, conv_w, conv_b,
           conv_ln_w, conv_ln_b, ret_decay, ret_gn_w, ffn_w_gate, ffn_w_up, ffn_w_down,
           moe_router, moe_router_b, moe_w_gate, moe_w_up, moe_w_down, final_norm_w):
    f = lambda a: np.ascontiguousarray(np.asarray(a, dtype=np.float32))
    ident, rope, retc, napb = host_consts(f(na_rpb))
    shared = {
        "w_mod": f(w_mod), "b_mod": f(b_mod), "norm1_w": f(norm1_w), "norm2_w": f(norm2_w), "w_in": f(w_in), "w_out": f(w_out),
        "convwT": np.ascontiguousarray(f(conv_w).reshape(2, 31, 2, 128).transpose(0, 3, 2, 1)),
        "cpard": np.ascontiguousarray(np.stack([f(conv_b), f(conv_ln_w), f(conv_ln_b)], axis=1).reshape(2, 3, 2, 128).transpose(0, 3, 1, 2)),
        "ret_decay": f(ret_decay).reshape(2, 8), "ret_gn_w": f(ret_gn_w),
        "ffn_w_gate": f(ffn_w_gate), "ffn_w_up": f(ffn_w_up), "ffn_w_down": f(ffn_w_down),
        "moe_routerT": np.ascontiguousarray(f(moe_router)[0].T), "moe_router_b": f(moe_router_b).reshape(1, NE),
        "moe_w_gate": f(moe_w_gate)[0], "moe_w_up": f(moe_w_up)[0], "moe_w_down": f(moe_w_down)[0],
        "final_norm_w": f(final_norm_w).reshape(1, D),
        "ident": ident, "rope": rope, "retc": retc, "napb": napb,
    }
    x = f(x); c = f(c); ctx = f(ctx); c_ctx = f(c_ctx)
    p2names = ("moe_w_gate", "moe_w_up", "moe_w_down", "final_norm_w")
    in1 = []
    for i in range(8):
        m = {k: v for k, v in shared.items() if k not in p2names}
        m["x2"] = x[2 * i:2 * i + 2]
        m["ctx2"] = ctx[2 * i:2 * i + 2]
        cv = np.concatenate([c[2 * i:2 * i + 2], c_ctx[None, :]], axis=0)
        m["cvecT"] = np.ascontiguousarray(cv.reshape(3, 8, 128).transpose(2, 1, 0))
        in1.append(m)
    if SINGLE:
        for i in range(8):
            for k in p2names:
                in1[i][k] = shared[k]
        if "nc" not in _CACHE:
            _CACHE["nc"] = build_program(part=0)
        r0 = run_bass_kernel_spmd(_CACHE["nc"], in1, core_ids=list(range(8))).results
        return np.concatenate([r["out"] for r in r0], axis=0)
    if "nc1" not in _CACHE:
        _CACHE["nc1"] = build_program(part=1)
        _CACHE["nc2"] = [build_program(part=2, bs=bs_) for bs_ in LAUNCH2]
    r1 = run_bass_kernel_spmd(_CACHE["nc1"], in1, core_ids=list(range(8))).results
    outs = [np.zeros((2, T, D), np.float32) for _ in range(8)]
    for bs_, nc2 in zip(LAUNCH2, _CACHE["nc2"]):
        in2 = []
        for i in range(8):
            m = {k: shared[k] for k in p2names}
            for k in ("MODS", "HA", "UT", "COMB"):
                m[k] = r1[i][k]
            in2.append(m)
        r2 = run_bass_kernel_spmd(nc2, in2, core_ids=list(range(8))).results
        for i in range(8):
            for b in bs_:
                outs[i][b] = r2[i]["out"][b]
    return np.concatenate(outs, axis=0)
```

```python
import contextlib
import numpy as np
CUT = 0
import concourse.bass as bass
import concourse.mybir as mybir
from concourse.bass_utils import run_bass_kernel_spmd

F32 = mybir.dt.float32
BF16 = mybir.dt.bfloat16
ALU = mybir.AluOpType
AF = mybir.ActivationFunctionType
AX = mybir.AxisListType

ENGS = ("tensor", "vector", "scalar", "gpsimd", "sync")
NDMA = 12


class Op:
    __slots__ = ("eng", "fn", "dma", "deps", "signal", "sem", "val", "idx")

    def __init__(self, eng, fn, dma):
        self.eng, self.fn, self.dma = eng, fn, dma
        self.deps = []
        self.signal = dma
        self.sem = None
        self.val = None


class Sched:
    def __init__(self, nc, stack):
        self.nc = nc
        self.csem = {e: stack.enter_context(nc.semaphore("cs_" + e)) for e in ENGS}
        self.ccnt = {e: 0 for e in ENGS}
        self.dsem = {e: [stack.enter_context(nc.semaphore("ds_%s%d" % (e, i))) for i in range(NDMA)]
                     for e in ("sync", "gpsimd")}
        self.dcnt = {e: [0] * NDMA for e in self.dsem}
        self.drr = {e: 0 for e in self.dsem}
        self.known = {e: {} for e in ENGS}
        self.same_engine_sync = True
        self.reset()

    def reset(self):
        self.ops = {e: [] for e in ENGS}
        self.lastw = {}
        self.readers = {}
        self.allops = []

    def op(self, eng, fn, R=(), W=(), dma=False):
        o = Op(eng, fn, dma)
        deps = []
        for r in R:
            w = self.lastw.get(r)
            if w is not None:
                deps.append(w)
        for w_ in W:
            w = self.lastw.get(w_)
            if w is not None:
                deps.append(w)
            deps.extend(self.readers.get(w_, ()))
        seen = set()
        for d in deps:
            if id(d) in seen or d is o:
                continue
            seen.add(id(d))
            if d.eng == eng and not d.dma and not dma:
                if eng == "tensor" or not self.same_engine_sync:
                    continue
            o.deps.append(d)
        for r in R:
            lst = self.readers.setdefault(r, [])
            if not dma:
                lst[:] = [x for x in lst if not (x.eng == eng and not x.dma)]
            lst.append(o)
        for w_ in W:
            self.lastw[w_] = o
            self.readers[w_] = []
        self.ops[eng].append(o)
        self.allops.append(o)
        return o

    def mm(self, out, lhsT, rhs, start=True, stop=True, R=(), W=()):
        return self.op("tensor", lambda e: e.matmul(out, lhsT=lhsT, rhs=rhs, start=start, stop=stop), R, W)

    def tr(self, out, in_, ident, R=(), W=()):
        return self.op("tensor", lambda e: e.transpose(out=out, in_=in_, identity=ident), R, W)

    def actf(self, out, in_, func, R=(), W=(), **kw):
        return self.op("scalar", lambda e: e.activation(out=out, in_=in_, func=func, **kw), R, W)

    def tt(self, out, in0, in1, op, R=(), W=(), eng="vector"):
        return self.op(eng, lambda e: e.tensor_tensor(out=out, in0=in0, in1=in1, op=op), R, W)

    def ts(self, out, in0, s1, s2, op0, op1=None, R=(), W=(), eng="vector", **kw):
        if op1 is None:
            return self.op(eng, lambda e: e.tensor_scalar(out=out, in0=in0, scalar1=s1, scalar2=None, op0=op0, **kw), R, W)
        return self.op(eng, lambda e: e.tensor_scalar(out=out, in0=in0, scalar1=s1, scalar2=s2, op0=op0, op1=op1, **kw), R, W)

    def stt(self, out, in0, scalar, in1, op0, op1, R=(), W=(), **kw):
        return self.op("vector", lambda e: e.scalar_tensor_tensor(out=out, in0=in0, scalar=scalar, in1=in1,
                                                                  op0=op0, op1=op1, **kw), R, W)

    def copy(self, out, in_, R=(), W=(), eng="vector"):
        if eng == "scalar":
            return self.op(eng, lambda e: e.activation(out=out, in_=in_, func=AF.Copy), R, W)
        return self.op(eng, lambda e: e.tensor_copy(out=out, in_=in_), R, W)

    def recip(self, out, in_, R=(), W=()):
        return self.op("vector", lambda e: e.reciprocal(out=out, in_=in_), R, W)

    def rsum(self, out, in_, R=(), W=()):
        return self.op("vector", lambda e: e.reduce_sum(out=out, in_=in_, axis=AX.X), R, W)

    def memset(self, ap, val, W=(), eng="gpsimd"):
        return self.op(eng, lambda e: e.memset(ap, val), (), W)

    def dma(self, out, in_, R=(), W=(), q="sync", **kw):
        return self.op(q, lambda e: e.dma_start(out=out, in_=in_, **kw), R, W, dma=True)

    def emit(self, block):
        for o in self.allops:
            for d in o.deps:
                d.signal = True
        for e in ENGS:
            for o in self.ops[e]:
                if o.dma:
                    i = self.drr[e]
                    self.drr[e] = (i + 1) % NDMA
                    prev = self.dcnt[e][i]
                    self.dcnt[e][i] = prev + 16
                    o.sem = self.dsem[e][i]
                    o.val = prev + 16
                    o.idx = (e, i, prev)
                elif o.signal:
                    self.ccnt[e] += 1
                    o.sem = self.csem[e]
                    o.val = self.ccnt[e]
        sched = self

        def make(e):
            def body(eng):
                known = sched.known[e]
                for o in sched.ops[e]:
                    waits = {}
                    for d in o.deps:
                        k = id(d.sem)
                        if k not in waits or waits[k][1] < d.val:
                            waits[k] = (d.sem, d.val)
                    if o.dma and o.idx[2] > 0:
                        s = sched.dsem[o.idx[0]][o.idx[1]]
                        k = id(s)
                        if k not in waits or waits[k][1] < o.idx[2]:
                            waits[k] = (s, o.idx[2])
                    for k, (s, v) in waits.items():
                        if known.get(k, 0) >= v:
                            continue
                        eng.wait_ge(s, v)
                        known[k] = v
                    inst = o.fn(eng)
                    if o.signal:
                        inst.then_inc(o.sem, 16 if o.dma else 1)
                if e in sched.dsem:
                    for i, s in enumerate(sched.dsem[e]):
                        v = sched.dcnt[e][i]
                        if v > 0 and known.get(id(s), 0) < v:
                            eng.wait_ge(s, v)
                            known[id(s)] = v
            return body

        for e in ENGS:
            if self.ops[e]:
                getattr(block, e)(make(e))
        self.reset()


D = 1024
T = 2048
LC = 256
NT = T + LC
NTILE = NT // 128
GRID_W = 64
EPS = 1e-6
NH = 8
D_FF = 2816
D_FFE = 3584
NE = 8
C_NAQ, C_NAK, C_NAV, C_CVA, C_CVG, C_RQ, C_RK, C_RV, C_GF, C_GB = 0, 512, 1024, 1536, 1792, 2048, 2176, 2304, 2560, 2816
R_NAQ, R_NAK, R_CV, R_RQ, R_RK, PXT_ROWS = 0, 512, 1024, 1280, 1408, 1536
PXV_COLS = 1280


def na_patterns():
    pats, index, table = [], {}, {}
    for a in range(16):
        kts, pids = [], []
        for kt in range(16):
            quad = []
            anyv = False
            for kr in range(2):
                for qr in range(2):
                    krow, qrow = 2 * kt + kr, 2 * a + qr
                    st = min(max(qrow - 4, 0), 24)
                    if st <= krow <= st + 7:
                        quad.append(krow - qrow + 7)
                        anyv = True
                    else:
                        quad.append(-1)
            if not anyv:
                continue
            atype = a if a in (0, 1, 14, 15) else -1
            key = (atype, kt - a) + tuple(quad)
            if key not in index:
                index[key] = len(pats)
                pats.append(tuple(quad))
            kts.append(kt)
            pids.append(index[key])
        table[a] = (kts, pids)
    return pats, table


def build_program(debug=False, upto=99, part=0, bs=(0, 1)):
    nc = bass.Bass("TRN2", target_bir_lowering=False)
    dk = "ExternalOutput" if debug else "Internal"

    def din(name, shape, dt=F32):
        return nc.dram_tensor(name, list(shape), dt, kind="ExternalInput").ap()

    def dscr(name, shape, dt, handoff=False):
        kind = dk
        if handoff and part == 1:
            kind = "ExternalOutput"
        if handoff and part == 2:
            kind = "ExternalInput"
        return nc.dram_tensor(name, list(shape), dt, kind=kind).ap()

    P1 = part in (0, 1)
    P2 = part in (0, 2)
    _din = din

    def din1(name, shape, dt=F32):
        return _din(name, shape, dt) if P1 else None

    def din2(name, shape, dt=F32):
        return _din(name, shape, dt) if P2 else None

    x2 = din1("x2", [2, T, D]); ctx2 = din1("ctx2", [2, LC, D]); cvecT = din1("cvecT", [128, 8, 3])
    w_mod = din1("w_mod", [2, D, 6 * D]); b_mod = din1("b_mod", [2, 6 * D])
    norm1_w = din1("norm1_w", [2, D]); norm2_w = din1("norm2_w", [2, D])
    w_in = din1("w_in", [2, D, 3072]); w_out = din1("w_out", [2, D, D])
    convwT = din1("convwT", [2, 128, 2, 31]); cpard = din1("cpard", [2, 128, 3, 2])
    ret_decay = din1("ret_decay", [2, 8]); ret_gn_w = din1("ret_gn_w", [2, 256])
    ffn_wg = din1("ffn_w_gate", [1, D, D_FF]); ffn_wu = din1("ffn_w_up", [1, D, D_FF]); ffn_wd = din1("ffn_w_down", [1, D_FF, D])
    moe_routerT = din1("moe_routerT", [NE, D]); moe_rb = din1("moe_router_b", [1, NE])
    moe_wg = din2("moe_w_gate", [NE, D, D_FFE]); moe_wu = din2("moe_w_up", [NE, D, D_FFE]); moe_wd = din2("moe_w_down", [NE, D_FFE, D])
    final_w = din2("final_norm_w", [1, D])
    identd = din1("ident", [128, 128]); rope = din1("rope", [4, 128, NT]); retc = din1("retc", [128, 772])
    napb = din1("napb", [2, 64, NH * 15 * 64])
    out = nc.dram_tensor("out", [2, T, D], F32, kind="ExternalOutput").ap() if P2 else None

    MODS = dscr("MODS", [2, 3, 6 * D], F32, True)
    PXT = dscr("PXT", [2, PXT_ROWS, NT], BF16)
    PXV = dscr("PXV", [2, NT, PXV_COLS], BF16)
    YC = dscr("YC", [2, NT, 768], BF16)
    YT = dscr("YT", [2, 256, NT], BF16)
    HA = dscr("HA", [2, NT, D], F32, True)
    HB = dscr("HB", [2, NT, D], F32)
    UT = dscr("UT", [2, D, NT], BF16, True)
    COMB = dscr("COMB", [2, NT, NE], F32, True)

    pats, ptable = na_patterns()
    NPAT = len(pats)

    with contextlib.ExitStack() as glob:
        S = Sched(nc, glob)
        idb = glob.enter_context(nc.sbuf_tensor("idb", [128, 128], BF16))
        id32 = glob.enter_context(nc.sbuf_tensor("id32", [128, 128], F32))

        def emit_block():
            if not S.allops:
                return
            with nc.Block() as blk:
                S.emit(blk)

        def stage(fn):
            with contextlib.ExitStack() as stg:
                r = fn(stg)
                if r is not None:
                    for _ in r:
                        emit_block()
                emit_block()

        uid = [0]

        def un(name):
            uid[0] += 1
            return "%s_%d" % (name, uid[0])

        def sb(stg, name, shape, dt):
            return stg.enter_context(nc.sbuf_tensor(un(name), list(shape), dt))

        def psf(stg, name, n=512):
            return stg.enter_context(nc.psum_tensor(un(name), [128, n], F32))

        def psb(stg, name, n=1024):
            return stg.enter_context(nc.psum_tensor(un(name), [128, n], BF16))

        def hsrc(l, b, t):
            if l == 0:
                return ctx2[b, t * 128:(t + 1) * 128, :] if t < 2 else x2[b, (t - 2) * 128:(t - 1) * 128, :]
            return HB[b, t * 128:(t + 1) * 128, :]

        def rms_rstd(stg_bufs, h_ap, hkey, junk, ss, rstd, tag):
            S.actf(junk, h_ap, AF.Square, R=[hkey], W=["junk" + tag, "ss" + tag], accum_out=ss)
            S.actf(rstd, ss, AF.Sqrt, R=["ss" + tag], W=["rstd" + tag], scale=1.0 / D, bias=epsb[:, 0:1])
            S.recip(rstd, rstd, R=["rstd" + tag], W=["rstd" + tag])

        epsb = glob.enter_context(nc.sbuf_tensor("epsb", [128, 1], F32))

        def stage_a(stg):
            S.dma(idb[:], identd, W=["idb"], q="gpsimd")
            S.dma(id32[:], identd, W=["id32"])
            S.memset(epsb[:], EPS, W=["epsb"])
            cT = sb(stg, "cT", [128, 8, 3], F32)
            sT = sb(stg, "sT", [128, 8, 3], F32)
            S.dma(cT[:], cvecT, W=["cT"])
            S.actf(sT[:], cT[:], AF.Silu, R=["cT"], W=["sT"])
            wb = [sb(stg, "wmb%d" % i, [128, 8, 512], F32) for i in range(2)]
            modrow = sb(stg, "modrow", [3, 6 * D], F32)
            bm3 = sb(stg, "bm3", [3, 6 * D], F32)
            nw3 = sb(stg, "nw3", [3, 2, D], F32)
            pm = [psf(stg, "pm%d" % i) for i in range(2)]
            it = 0
            for l in range(2):
                S.dma(bm3[:], b_mod[l:l + 1, :].partition_broadcast(3), R=["bm3"], W=["bm3"])
                S.dma(nw3[:, 0, :], norm1_w[l:l + 1, :].partition_broadcast(3), W=["nw3"])
                S.dma(nw3[:, 1, :], norm2_w[l:l + 1, :].partition_broadcast(3), W=["nw3"])
                for nb in range(12):
                    i = it % 2
                    it += 1
                    S.dma(wb[i][:], w_mod[l, :, nb * 512:(nb + 1) * 512].rearrange("(k p) n -> p k n", p=128),
                          W=["wmb%d" % i])
                    for k in range(8):
                        S.mm(pm[i][0:3, :], sT[:, k, :], wb[i][:, k, :], start=(k == 0), stop=(k == 7),
                             R=["sT", "wmb%d" % i], W=["pm%d" % i])
                    S.copy(modrow[:, nb * 512:(nb + 1) * 512], pm[i][0:3, :], R=["pm%d" % i], W=["modrow"], eng="scalar")
                S.tt(modrow[:], modrow[:], bm3[:], ALU.add, R=["modrow", "bm3"], W=["modrow"])
                for s_, wsel in ((1, 0), (4, 1)):
                    seg = modrow[:, s_ * D:(s_ + 1) * D]
                    S.stt(seg, seg, 1.0, nw3[:, wsel, :], ALU.add, ALU.mult, R=["modrow", "nw3"], W=["modrow"])
                S.dma(MODS[l], modrow[:], R=["modrow"], W=["MODS"])

        def stage_a2(stg):
            S.memset(epsb[:], EPS, W=["epsb"])

        stage(stage_a if P1 else stage_a2)

        def modbc(dst, l, j, s):
            return MODS[l, j:j + 1, s * D:(s + 1) * D].partition_broadcast(128)

        def stage_b(l, bsel):
            def f(stg):
                w = sb(stg, "win", [128, 8, 3328], BF16)
                for k in range(8):
                    S.dma(w[:, k, 0:3072], w_in[l, k * 128:(k + 1) * 128, :], W=["win"], q="gpsimd")
                for (src, dst) in ((C_RQ, 3072), (C_RK, 3200)):
                    sv = w[:, :, src:src + 128].rearrange("p k (h two i) -> p k h two i", two=2, i=16)
                    dv = w[:, :, dst:dst + 128].rearrange("p k (h two i) -> p k h two i", two=2, i=16)
                    S.copy(dv[:, :, :, 0, :], sv[:, :, :, 1, :], R=["win"], W=["win"])
                    S.copy(dv[:, :, :, 1, :], sv[:, :, :, 0, :], R=["win"], W=["win"], eng="gpsimd")
                if CUT == 1:
                    return
                mb = sb(stg, "mb", [128, 4, D], F32)
                ropet = [sb(stg, "ropet%d" % i, [128, 4, 512], F32) for i in range(2)]
                ht = [sb(stg, "ht%d" % i, [128, D], F32) for i in range(3)]
                junk = sb(stg, "junk", [128, D], BF16)
                xn = [sb(stg, "xn%d" % i, [128, D], F32) for i in range(2)]
                ux = [sb(stg, "ux%d" % i, [128, D], BF16) for i in range(2)]
                uxT = [sb(stg, "uxT%d" % i, [128, 8, 512], BF16) for i in range(2)]
                ss = sb(stg, "ss", [128, 4], F32)
                rstd = sb(stg, "rstd", [128, 4], F32)
                fo = [sb(stg, "fo%d" % i, [128, 512], BF16) for i in range(3)]
                sg = [sb(stg, "sg%d" % i, [128, 512], F32) for i in range(2)]
                r1 = [sb(stg, "r1%d" % i, [128, 512], F32) for i in range(2)]
                r2 = [sb(stg, "r2%d" % i, [128, 512], F32) for i in range(2)]
                to = [sb(stg, "to%d" % i, [128, PXV_COLS], BF16) for i in range(2)]
                pT = [psb(stg, "pT%d" % i) for i in range(2)]
                pA = [psf(stg, "pA%d" % i) for i in range(6)]
                tcnt = 0
                gcnt = 0
                focnt = 0
                pacnt = 0
                for b in bsel:
                    for s_, (j, slot) in enumerate(((2, 1), (2, 0), (b, 1), (b, 0))):
                        S.dma(mb[:, s_, :], modbc(None, l, j, slot), R=["MODS"], W=["mb"])
                    for g in range(5):
                        tiles = list(range(4 * g, min(4 * g + 4, NTILE)))
                        N = 128 * len(tiles)
                        gi = gcnt % 2
                        gcnt += 1
                        c0 = 4 * g * 128
                        S.dma(ropet[gi][:, :, 0:N], rope[:, :, c0:c0 + N].rearrange("f p n -> p f n"), W=["ropet%d" % gi])
                        for si, t in enumerate(tiles):
                            hi = tcnt % 3
                            xi = tcnt % 2
                            tcnt += 1
                            mo = 0 if t < 2 else 2
                            S.dma(ht[hi][:], hsrc(l, b, t), R=["HB"], W=["ht%d" % hi])
                            S.actf(junk[:], ht[hi][:], AF.Square, R=["ht%d" % hi], W=["junk", "ss%d" % xi], accum_out=ss[:, xi:xi + 1])
                            S.actf(rstd[:, xi:xi + 1], ss[:, xi:xi + 1], AF.Sqrt, R=["ss%d" % xi, "epsb"], W=["rstd%d" % xi],
                                   scale=1.0 / D, bias=epsb[:, 0:1])
                            S.recip(rstd[:, xi:xi + 1], rstd[:, xi:xi + 1], R=["rstd%d" % xi], W=["rstd%d" % xi])
                            S.stt(xn[xi][:], ht[hi][:], rstd[:, xi:xi + 1], mb[:, mo, :], ALU.mult, ALU.mult,
                                  R=["ht%d" % hi, "rstd%d" % xi, "mb"], W=["xn%d" % xi])
                            S.tt(ux[xi][:], xn[xi][:], mb[:, mo + 1, :], ALU.add, R=["xn%d" % xi, "mb"], W=["ux%d" % xi])
                            for k in range(8):
                                S.tr(pT[xi][:, k * 128:(k + 1) * 128], ux[xi][:, k * 128:(k + 1) * 128], idb[:],
                                     R=["ux%d" % xi, "idb"], W=["pT%d" % xi])
                            S.copy(uxT[gi][:, :, si * 128:(si + 1) * 128], pT[xi][:].rearrange("p (k n) -> p k n", k=8),
                                   R=["pT%d" % xi], W=["uxT%d" % gi], eng="scalar")
                        uk = "uxT%d" % gi
                        if CUT == 2:
                            return

                        def fm(col):
                            nonlocal pacnt
                            pi = pacnt % 6
                            pacnt += 1
                            for k in range(8):
                                S.mm(pA[pi][:, 0:N], w[:, k, col:col + 128], uxT[gi][:, k, 0:N], start=(k == 0), stop=(k == 7),
                                     R=["win", uk], W=["pA%d" % pi])
                            return pi

                        def fo_store(fi, row):
                            S.dma(PXT[b, row:row + 128, c0:c0 + N], fo[fi][:, 0:N], R=["fo%d" % fi], W=["PXT"], q="gpsimd")

                        for (col, row) in ((C_NAQ, R_NAQ), (C_NAK, R_NAK)):
                            for m in range(4):
                                pi = fm(col + m * 128)
                                fi = focnt % 3
                                focnt += 1
                                if m % 2 == 0:
                                    S.copy(fo[fi][:, 0:N], pA[pi][:, 0:N], R=["pA%d" % pi], W=["fo%d" % fi], eng="scalar")
                                else:
                                    S.copy(fo[fi][:, 0:N], pA[pi][:, 0:N], R=["pA%d" % pi], W=["fo%d" % fi])
                                fo_store(fi, row + m * 128)
                        if CUT == 3:
                            return
                        for m in range(2):
                            pa = fm(C_CVA + m * 128)
                            pg = fm(C_CVG + m * 128)
                            si_ = m
                            S.actf(sg[si_][:, 0:N], pA[pg][:, 0:N], AF.Sigmoid, R=["pA%d" % pg], W=["sg%d" % si_])
                            fi = focnt % 3
                            focnt += 1
                            S.tt(fo[fi][:, 0:N], pA[pa][:, 0:N], sg[si_][:, 0:N], ALU.mult, R=["pA%d" % pa, "sg%d" % si_], W=["fo%d" % fi])
                            fo_store(fi, R_CV + m * 128)
                        if CUT == 4:
                            return
                        for qi, (col, scol, row) in enumerate(((C_RQ, 3072, R_RQ), (C_RK, 3200, R_RK))):
                            p0 = fm(col)
                            p1 = fm(scol)
                            S.tt(r1[qi][:, 0:N], pA[p0][:, 0:N], ropet[gi][:, 2 * qi, 0:N], ALU.mult,
                                 R=["pA%d" % p0, "ropet%d" % gi], W=["r1%d" % qi])
                            S.tt(r2[qi][:, 0:N], pA[p1][:, 0:N], ropet[gi][:, 2 * qi + 1, 0:N], ALU.mult,
                                 R=["pA%d" % p1, "ropet%d" % gi], W=["r2%d" % qi])
                            fi = focnt % 3
                            focnt += 1
                            S.tt(fo[fi][:, 0:N], r1[qi][:, 0:N], r2[qi][:, 0:N], ALU.add, R=["r1%d" % qi, "r2%d" % qi],
                                 W=["fo%d" % fi], eng="gpsimd")
                            fo_store(fi, row)
                        if CUT == 5:
                            return
                        for si, t in enumerate(tiles):
                            ti = (tcnt + si) % 2
                            for (col, ncol, ocol, kind) in ((C_NAV, 512, 0, 0), (C_RV, 512, 512, 1), (C_GB, 256, 1024, 2)):
                                pi = pacnt % 6
                                pacnt += 1
                                for k in range(8):
                                    S.mm(pA[pi][:, 0:ncol], uxT[gi][:, k, si * 128:(si + 1) * 128], w[:, k, col:col + ncol],
                                         start=(k == 0), stop=(k == 7), R=["win", uk], W=["pA%d" % pi])
                                if kind == 0:
                                    S.copy(to[ti][:, 0:512], pA[pi][:, 0:512], R=["pA%d" % pi], W=["to%d" % ti])
                                elif kind == 1:
                                    S.copy(to[ti][:, 512:768], pA[pi][:, 0:256], R=["pA%d" % pi], W=["to%d" % ti], eng="scalar")
                                    S.actf(to[ti][:, 768:1024], pA[pi][:, 256:512], AF.Silu, R=["pA%d" % pi], W=["to%d" % ti])
                                else:
                                    S.actf(to[ti][:, 1024:1280], pA[pi][:, 0:256], AF.Silu, R=["pA%d" % pi], W=["to%d" % ti])
                            S.dma(PXV[b, t * 128:(t + 1) * 128, :], to[ti][:], R=["to%d" % ti], W=["PXV"], q="gpsimd")
                        if CUT == 6:
                            return
                        if (CUT == 7 and g == 3) or (CUT == 8 and g == 4):
                            return
            return f

        def stage_c(l, b):
            def f(stg):
                qT = sb(stg, "qT", [128, 4, NT], BF16)
                kT = sb(stg, "kT", [128, 4, NT], BF16)
                V = sb(stg, "V", [128, NTILE, NH, 65], BF16)
                ET = sb(stg, "ET", [128, NH * 15 * 64], F32)
                ETb = sb(stg, "ETb", [128, NH, 15, 64], BF16)
                PT = sb(stg, "PT", [128, NH, NPAT, 128], BF16)
                Pt = [sb(stg, "Pt%d" % i, [128, 7, 128], BF16) for i in range(3)]
                yc = [sb(stg, "yc%d" % i, [128, 512], BF16) for i in range(2)]
                rec = [sb(stg, "rec%d" % i, [128, 4], F32) for i in range(2)]
                pS = [psf(stg, "pS%d" % i) for i in range(4)]
                pO = [psf(stg, "pO%d" % i) for i in range(2)]
                S.dma(qT[:], PXT[b, R_NAQ:R_NAQ + 512, :].rearrange("(c p) t -> p c t", p=128), R=["PXT"], W=["qT"])
                S.dma(kT[:], PXT[b, R_NAK:R_NAK + 512, :].rearrange("(c p) t -> p c t", p=128), R=["PXT"], W=["kT"])
                S.memset(V[:], 1.0, W=["V"])
                for t0 in range(NTILE):
                    S.dma(V[:, t0, :, 0:64],
                          PXV[b, t0 * 128:(t0 + 1) * 128, 0:512].rearrange("p (h d) -> p h d", d=64),
                          R=["PXV"], W=["V"])
                S.dma(ET[0:64, :], napb[l], W=["ET"])
                S.dma(ET[64:128, :], napb[l], W=["ET"])
                S.actf(ETb[:].rearrange("p h r c -> p (h r c)"), ET[:], AF.Exp, R=["ET"], W=["ETb"])
                S.memset(PT[:], 0.0, W=["PT"])
                ci = 0
                for pid, quad in enumerate(pats):
                    for kr in range(2):
                        for qr in range(2):
                            dr = quad[kr * 2 + qr]
                            if dr < 0:
                                continue
                            eng = ("vector", "gpsimd")[ci % 2]
                            ci += 1
                            S.copy(PT[kr * 64:(kr + 1) * 64, :, pid, qr * 64:(qr + 1) * 64], ETb[kr * 64:(kr + 1) * 64, :, dr, :],
                                   R=["ETb"], W=["PT"], eng=eng)
                qtiles = list(range(2, NTILE)) + ([0, 1] if l == 0 else [])
                iters = []
                for qi_, tq in enumerate(qtiles):
                    if tq >= 2:
                        kts, pids = ptable[tq - 2]
                        ktoks = [kt + 2 for kt in kts] + [0, 1]
                        nl = len(kts)
                        assert pids == list(range(pids[0], pids[0] + nl))
                    else:
                        ktoks, nl, pids = [0, 1], 0, [0]
                    for h in range(NH):
                        iters.append((qi_, tq, h, ktoks, nl, pids[0]))

                def rec_scores(n):
                    qi_, tq, h, ktoks, nl, p0 = iters[n]
                    hp, hc = h % 2, h // 2
                    sl = slice(hp * 64, (hp + 1) * 64)
                    i2 = n % 2
                    for s_, kt in enumerate(ktoks):
                        bank = pS[2 * i2 + s_ // 4]
                        S.mm(bank[:, (s_ % 4) * 128:(s_ % 4 + 1) * 128], kT[sl, hc, kt * 128:(kt + 1) * 128],
                             qT[sl, hc, tq * 128:(tq + 1) * 128], R=["qT", "kT"], W=["pS%d" % (2 * i2 + s_ // 4)])

                def rec_softmax(n):
                    qi_, tq, h, ktoks, nl, p0 = iters[n]
                    i2, i3 = n % 2, n % 3
                    ns = len(ktoks)
                    n0 = min(ns, 4)
                    S.actf(Pt[i3][:, 0:n0, :].rearrange("p s q -> p (s q)"), pS[2 * i2][:, 0:n0 * 128], AF.Exp,
                           R=["pS%d" % (2 * i2)], W=["Pt%d" % i3], scale=0.125)
                    if ns > 4:
                        S.actf(Pt[i3][:, 4:ns, :].rearrange("p s q -> p (s q)"), pS[2 * i2 + 1][:, 0:(ns - 4) * 128], AF.Exp,
                               R=["pS%d" % (2 * i2 + 1)], W=["Pt%d" % i3], scale=0.125)
                    if nl:
                        S.tt(Pt[i3][:, 0:nl, :], Pt[i3][:, 0:nl, :], PT[:, h, p0:p0 + nl, :], ALU.mult,
                             R=["Pt%d" % i3, "PT"], W=["Pt%d" % i3], eng="vector")

                def rec_pv(n):
                    qi_, tq, h, ktoks, nl, p0 = iters[n]
                    i3 = n % 3
                    ns = len(ktoks)
                    yi = qi_ % 2
                    og = h // 4
                    hq = h % 4
                    for s_, kt in enumerate(ktoks):
                        S.mm(pO[og][:, hq * 65:(hq + 1) * 65], Pt[i3][:, s_, :], V[:, kt, h, :], start=(s_ == 0), stop=(s_ == ns - 1),
                             R=["Pt%d" % i3, "V"], W=["pO%d" % og])
                    if hq == 3:
                        pv = pO[og][:, 0:260].rearrange("p (h d) -> p h d", d=65)
                        S.recip(rec[og][:].unsqueeze(2), pv[:, :, 64:65], R=["pO%d" % og], W=["rec%d" % og])
                        S.tt(yc[yi][:, (h - 3) * 64:(h + 1) * 64].rearrange("p (h d) -> p h d", d=64), pv[:, :, 0:64],
                             rec[og][:].unsqueeze(2).to_broadcast([128, 4, 64]), ALU.mult,
                             R=["pO%d" % og, "rec%d" % og], W=["yc%d" % yi])
                    if h == NH - 1:
                        S.dma(YC[b, tq * 128:(tq + 1) * 128, 0:512], yc[yi][:], R=["yc%d" % yi], W=["YC"], q="gpsimd")

                rec_scores(0)
                for n in range(len(iters)):
                    rec_softmax(n)
                    if n + 1 < len(iters):
                        rec_scores(n + 1)
                    rec_pv(n)
            return f

        def stage_d(l, b):
            def f(stg):
                ylat = sb(stg, "ylat", [128, 2, T + 32], BF16)
                yctx = sb(stg, "yctx", [128, 2, LC + 32], BF16)
                cw = sb(stg, "cw", [128, 2, 31], F32)
                cpar = sb(stg, "cpar", [128, 3, 2], F32)
                diag = sb(stg, "diag", [128, 2, 31, 128], BF16)
                ones = sb(stg, "ones", [128, 128], F32)
                zc = [sb(stg, "zc%d" % i, [128, 512], F32) for i in range(2)]
                sq = [sb(stg, "sq%d" % i, [128, 512], F32) for i in range(2)]
                mean = sb(stg, "mean", [128, 512], F32)
                var = sb(stg, "var", [128, 512], F32)
                dd = [sb(stg, "dd%d" % i, [128, 512], F32) for i in range(2)]
                yo = [sb(stg, "yo%d" % i, [128, 512], BF16) for i in range(2)]
                pc = [psf(stg, "pc%d" % i) for i in range(4)]
                pm = [psf(stg, "pmn%d" % i) for i in range(2)]
                S.memset(ylat[:], 0.0, W=["ylat"])
                S.memset(yctx[:], 0.0, W=["yctx"])
                S.memset(ones[:], 1.0 / 256, W=["ones"])
                S.dma(ylat[:, :, 15:15 + T], PXT[b, R_CV:R_CV + 256, LC:NT].rearrange("(c p) t -> p c t", p=128), R=["PXT"], W=["ylat"])
                if l == 0:
                    S.dma(yctx[:, :, 15:15 + LC], PXT[b, R_CV:R_CV + 256, 0:LC].rearrange("(c p) t -> p c t", p=128), R=["PXT"], W=["yctx"])
                S.dma(cw[:], convwT[l], W=["cw"])
                S.dma(cpar[:], cpard[l], W=["cpar"])
                for c in range(2):
                    for j in range(31):
                        S.ts(diag[:, c, j, :], id32[:], cw[:, c, j:j + 1], None, ALU.mult, R=["cw", "id32"], W=["diag"],
                             eng=("vector", "gpsimd")[j % 2])
                blocks = [("lat", ylat, tb * 512, 512, LC + tb * 512) for tb in range(4)]
                if l == 0:
                    blocks.append(("ctx", yctx, 0, 256, 0))
                for bi, (_, ybuf, off, N, tok0) in enumerate(blocks):
                    ykey = "ylat" if ybuf is ylat else "yctx"
                    for c in range(2):
                        p = pc[(2 * bi + c) % 4]
                        pk = "pc%d" % ((2 * bi + c) % 4)
                        for j in range(31):
                            S.mm(p[:, 0:N], diag[:, c, j, :], ybuf[:, c, off + j:off + j + N], start=(j == 0), stop=(j == 30),
                                 R=["diag", ykey], W=[pk])
                        S.actf(zc[c][:, 0:N], p[:, 0:N], AF.Identity, R=[pk, "cpar"], W=["zc%d" % c], bias=cpar[:, 0, c:c + 1])
                        S.actf(sq[c][:, 0:N], p[:, 0:N], AF.Square, R=[pk, "cpar"], W=["sq%d" % c], bias=cpar[:, 0, c:c + 1])
                    for c in range(2):
                        S.mm(pm[0][:, 0:N], ones[:], zc[c][:, 0:N], start=(c == 0), stop=(c == 1), R=["ones", "zc%d" % c], W=["pmn0"])
                    for c in range(2):
                        S.mm(pm[1][:, 0:N], ones[:], sq[c][:, 0:N], start=(c == 0), stop=(c == 1), R=["ones", "sq%d" % c], W=["pmn1"])
                    S.copy(mean[:, 0:N], pm[0][:, 0:N], R=["pmn0"], W=["mean"], eng="scalar")
                    S.tt(var[:, 0:N], mean[:, 0:N], mean[:, 0:N], ALU.mult, R=["mean"], W=["var"])
                    S.tt(var[:, 0:N], pm[1][:, 0:N], var[:, 0:N], ALU.subtract, R=["pmn1", "var"], W=["var"])
                    S.actf(var[:, 0:N], var[:, 0:N], AF.Sqrt, R=["var", "epsb"], W=["var"], bias=epsb[:, 0:1])
                    S.recip(var[:, 0:N], var[:, 0:N], R=["var"], W=["var"])
                    for c in range(2):
                        S.tt(dd[c][:, 0:N], zc[c][:, 0:N], mean[:, 0:N], ALU.subtract, R=["zc%d" % c, "mean"], W=["dd%d" % c])
                        S.tt(dd[c][:, 0:N], dd[c][:, 0:N], var[:, 0:N], ALU.mult, R=["dd%d" % c, "var"], W=["dd%d" % c], eng="gpsimd")
                        S.actf(yo[c][:, 0:N], dd[c][:, 0:N], AF.Silu, R=["dd%d" % c, "cpar"], W=["yo%d" % c],
                               scale=cpar[:, 1, c:c + 1], bias=cpar[:, 2, c:c + 1])
                        S.dma(YT[b, c * 128:(c + 1) * 128, tok0:tok0 + N], yo[c][:, 0:N], R=["yo%d" % c], W=["YT"], q="gpsimd")
            return f

        def stage_e(l, b):
            def f(stg):
                qT = sb(stg, "rqT", [32, 4, NT], BF16)
                kT = sb(stg, "rkT", [32, 4, NT], BF16)
                ktm = sb(stg, "ktm", [128, NTILE, 128], BF16)
                V = sb(stg, "rV", [128, NTILE, 768], BF16)
                rc = sb(stg, "rc", [128, 772], F32)
                dec = sb(stg, "dec", [128, 8], F32)
                lg = sb(stg, "lg", [128, 8], F32)
                intra = sb(stg, "intra", [128, 2, 4, 128], F32)
                QD = sb(stg, "QD", [32, 2, 4, 128], F32)
                KD = sb(stg, "KD", [128, 2, 4], F32)
                CD = sb(stg, "CD", [32, 2, 4], F32)
                gnw = sb(stg, "gnw", [128, 256], F32)
                S32 = [sb(stg, "S32_%d" % i, [32, 4, 64], F32) for i in range(2)]
                Sbf = [sb(stg, "Sbf_%d" % i, [32, 4, 64], BF16) for i in range(2)]
                Pm = [sb(stg, "Pm%d" % i, [128, 4, 128], BF16) for i in range(2)]
                qd = [sb(stg, "qd%d" % i, [32, 4, 128], BF16) for i in range(2)]
                kd = [sb(stg, "kd%d" % i, [128, 4, 32], BF16) for i in range(2)]
                Oall = [sb(stg, "Oall%d" % i, [128, NTILE, 256], F32) for i in range(2)]
                sqa = sb(stg, "sqa", [128, NTILE, 256], F32)
                stt_ = [sb(stg, "stt%d" % i, [128, 3, 4 * NTILE], F32) for i in range(2)]
                Yo = sb(stg, "Yo", [128, NTILE, 256], BF16)
                pST = [psf(stg, "pST%d" % i) for i in range(2)]
                pOo = [psf(stg, "pOo%d" % i) for i in range(2)]
                pSs = [psf(stg, "pSs%d" % i) for i in range(2)]
                pK = psb(stg, "pK")
                S.dma(qT[:], PXT[b, R_RQ:R_RQ + 128, :].rearrange("(h d) t -> d h t", d=32), R=["PXT"], W=["rqT"])
                S.dma(kT[:], PXT[b, R_RK:R_RK + 128, :].rearrange("(h d) t -> d h t", d=32), R=["PXT"], W=["rkT"])
                for t0 in range(0, NTILE, 6):
                    S.dma(V[:, t0:t0 + 6, :], PXV[b, t0 * 128:(t0 + 6) * 128, 512:1280].rearrange("(t p) c -> p t c", p=128),
                          R=["PXV"], W=["rV"])
                S.dma(rc[:], retc, W=["rc"])
                S.dma(dec[:], ret_decay[l:l + 1, :].partition_broadcast(128), W=["dec"])
                S.dma(gnw[:], ret_gn_w[l:l + 1, :].partition_broadcast(128), W=["gnw"])
                S.actf(lg[:], dec[:], AF.Exp, R=["dec"], W=["lg"], scale=-float(np.log(2.0)))
                S.ts(lg[:], lg[:], -1.0, 1.0, ALU.mult, ALU.add, R=["lg"], W=["lg"])
                S.actf(lg[:], lg[:], AF.Ln, R=["lg"], W=["lg"])
                Dm = {0: rc[:, 0:128], 1: rc[:, 256:384]}
                Mm = {0: rc[:, 128:256], 1: rc[:, 384:512]}
                R12 = {0: rc[0:32, 512:640], 1: rc[0:32, 640:768]}
                C12 = {0: rc[:, 768:769], 1: rc[:, 769:770]}
                for d_ in range(2):
                    for h in range(4):
                        S.actf(intra[:, d_, h, :], Dm[d_], AF.Exp, R=["rc", "lg"], W=["intra"], scale=lg[:, d_ * 4 + h:d_ * 4 + h + 1])
                        S.tt(intra[:, d_, h, :], intra[:, d_, h, :], Mm[d_], ALU.mult, R=["intra", "rc"], W=["intra"])
                        S.actf(QD[:, d_, h, :], R12[d_], AF.Exp, R=["rc", "lg"], W=["QD"], scale=lg[0:32, d_ * 4 + h:d_ * 4 + h + 1])
                    S.actf(KD[:, d_, :], lg[:, d_ * 4:d_ * 4 + 4], AF.Exp, R=["rc", "lg"], W=["KD"], scale=C12[d_])
                    S.actf(CD[:, d_, :], lg[0:32, d_ * 4:d_ * 4 + 4], AF.Exp, R=["lg"], W=["CD"], scale=128.0)
                for t in range(NTILE):
                    for h in range(4):
                        S.tr(pK[:, h * 32:(h + 1) * 32], kT[:, h, t * 128:(t + 1) * 128], idb[0:32, 0:32], R=["rkT", "idb"], W=["pK"])
                    S.copy(ktm[:, t, :], pK[:, 0:128], R=["pK"], W=["ktm"], eng=("vector", "scalar")[t % 2])
                for d_ in range(2):
                    S.memset(S32[d_][:], 0.0, W=["S32_%d" % d_])
                    S.memset(Sbf[d_][:], 0.0, W=["Sbf_%d" % d_])
                order = {0: list(range(NTILE)), 1: [1, 0] + list(range(NTILE - 1, 1, -1))}
                for step in range(NTILE):
                    for d_ in range(2):
                        c = order[d_][step]
                        cs = slice(c * 128, (c + 1) * 128)
                        D_ = "%d" % d_
                        for h in range(4):
                            S.mm(pST[d_][:, h * 128:(h + 1) * 128], kT[:, h, cs], qT[:, h, cs], R=["rkT", "rqT"], W=["pST" + D_])
                        S.tt(Pm[d_][:], pST[d_][:].rearrange("p (h i) -> p h i", h=4), intra[:, d_, :, :], ALU.mult,
                             R=["pST" + D_, "intra"], W=["Pm" + D_])
                        S.tt(qd[d_][:], qT[:, :, cs], QD[:, d_, :, :], ALU.mult, R=["rqT", "QD"], W=["qd" + D_], eng="gpsimd")
                        S.tt(kd[d_][:], ktm[:, c, :].rearrange("p (h d) -> p h d", h=4),
                             KD[:, d_, :].unsqueeze(2).to_broadcast([128, 4, 32]), ALU.mult, R=["ktm", "KD"], W=["kd" + D_], eng="gpsimd")
                        for h in range(4):
                            S.mm(pOo[d_][:, h * 64:(h + 1) * 64], Pm[d_][:, h, :], V[:, c, h * 64:(h + 1) * 64], start=True, stop=False,
                                 R=["Pm" + D_, "rV"], W=["pOo" + D_])
                            S.mm(pOo[d_][:, h * 64:(h + 1) * 64], qd[d_][:, h, :], Sbf[d_][:, h, :], start=False, stop=True,
                                 R=["qd" + D_, "Sbf_" + D_], W=["pOo" + D_])
                        for h in range(4):
                            S.mm(pSs[d_][0:32, h * 64:(h + 1) * 64], kd[d_][:, h, :], V[:, c, h * 64:(h + 1) * 64],
                                 R=["kd" + D_, "rV"], W=["pSs" + D_])
                        S.tt(S32[d_][:], S32[d_][:], CD[:, d_, :].unsqueeze(2).to_broadcast([32, 4, 64]), ALU.mult,
                             R=["S32_" + D_, "CD"], W=["S32_" + D_])
                        S.tt(S32[d_][:], S32[d_][:], pSs[d_][0:32, 0:256].rearrange("p (h e) -> p h e", h=4), ALU.add,
                             R=["S32_" + D_, "pSs" + D_], W=["S32_" + D_])
                        S.copy(Sbf[d_][:], S32[d_][:], R=["S32_" + D_], W=["Sbf_" + D_])
                        S.copy(Oall[d_][:, c, :], pOo[d_][:, 0:256], R=["pOo" + D_], W=["Oall" + D_], eng="scalar")
                for d_ in range(2):
                    D_ = "%d" % d_
                    ok = "Oall" + D_
                    Of = Oall[d_][:].rearrange("p c f -> p (c f)")
                    O3 = Oall[d_][:].rearrange("p c (h e) -> p (c h) e", e=64)
                    s1, s3 = stt_[d_][:, 0, :], stt_[d_][:, 1, :]
                    sk = "stt" + D_
                    S.actf(sqa[:].rearrange("p c f -> p (c f)"), Of, AF.Square, R=[ok], W=["sqa"])
                    S.rsum(s1, O3, R=[ok], W=[sk])
                    S.rsum(s3, sqa[:].rearrange("p c (h e) -> p (c h) e", e=64), R=["sqa"], W=[sk])
                    S.ts(s1, s1, 1.0 / 64, None, ALU.mult, R=[sk], W=[sk])
                    S.ts(s3, s3, 1.0 / 64, None, ALU.mult, R=[sk], W=[sk])
                    S.tt(stt_[d_][:, 2, :], s1, s1, ALU.mult, R=[sk], W=[sk])
                    S.tt(s3, s3, stt_[d_][:, 2, :], ALU.subtract, R=[sk], W=[sk])
                    S.actf(s3, s3, AF.Sqrt, R=[sk, "epsb"], W=[sk], bias=epsb[:, 0:1])
                    S.recip(s3, s3, R=[sk], W=[sk])
                    S.tt(O3, O3, s1.unsqueeze(2).to_broadcast([128, 4 * NTILE, 64]), ALU.subtract, R=[ok, sk], W=[ok])
                    S.tt(O3, O3, s3.unsqueeze(2).to_broadcast([128, 4 * NTILE, 64]), ALU.mult, R=[ok, sk], W=[ok],
                         eng=("gpsimd", "vector")[d_])
                    S.tt(Oall[d_][:], Oall[d_][:], gnw[:].unsqueeze(1).to_broadcast([128, NTILE, 256]), ALU.mult, R=[ok, "gnw"], W=[ok],
                         eng=("vector", "gpsimd")[d_])
                    S.tt(Oall[d_][:], Oall[d_][:], V[:, :, 256 + 256 * d_:512 + 256 * d_], ALU.mult, R=[ok, "rV"], W=[ok],
                         eng=("gpsimd", "vector")[d_])
                S.tt(Yo[:], Oall[0][:], Oall[1][:], ALU.add, R=["Oall0", "Oall1"], W=["Yo"])
                S.dma(YC[b, :, 512:768].rearrange("(t p) c -> p t c", p=128), Yo[:], R=["Yo"], W=["YC"], q="gpsimd")
            return f

        def stage_f(l, b):
            last = l == 1

            def f(stg):
                wo = sb(stg, "wo", [128, 8, D], BF16)
                mb = sb(stg, "mbF", [128, 6, D], F32)
                ycs = [sb(stg, "ycs%d" % i, [128, 768], BF16) for i in range(2)]
                ycv = [sb(stg, "ycv%d" % i, [128, 2, 128], BF16) for i in range(2)]
                ycT = [sb(stg, "ycT%d" % i, [128, 6, 128], BF16) for i in range(2)]
                ht = [sb(stg, "htF%d" % i, [128, D], F32) for i in range(2)]
                hn = [sb(stg, "hn%d" % i, [128, D], F32) for i in range(2)]
                tmp = sb(stg, "tmpF", [128, D], F32)
                junk = sb(stg, "junkF", [128, D], BF16)
                u32 = [sb(stg, "u32_%d" % i, [128, D], F32) for i in range(2)]
                ubf = [sb(stg, "ubf%d" % i, [128, D], BF16) for i in range(2)]
                uTs = [sb(stg, "uTs%d" % i, [128, 8, 128], BF16) for i in range(2)]
                ss = sb(stg, "ssF", [128, 2], F32)
                rstd = sb(stg, "rstdF", [128, 2], F32)
                pT = [psb(stg, "pTF%d" % i) for i in range(2)]
                pY = [psf(stg, "pY%d" % i) for i in range(4)]
                for k in range(8):
                    S.dma(wo[:, k, :], w_out[l, k * 128:(k + 1) * 128, :], W=["wo"], q="gpsimd")
                for s_, (j, slot) in enumerate(((2, 2), (2, 4), (2, 3), (b, 2), (b, 4), (b, 3))):
                    if last and s_ < 3:
                        continue
                    S.dma(mb[:, s_, :], modbc(None, l, j, slot), R=["MODS"], W=["mbF"])
                if last:
                    wr = sb(stg, "wr", [128, NE, D], F32)
                    rb = sb(stg, "rb", [128, NE], F32)
                    lgt = [sb(stg, "lgt%d" % i, [128, 32], F32) for i in range(2)]
                    for e_ in range(NE):
                        S.dma(wr[:, e_, :], moe_routerT[e_:e_ + 1, :].partition_broadcast(128), W=["wr"])
                    S.dma(rb[:], moe_rb.partition_broadcast(128), W=["rb"])
                tiles = list(range(2, NTILE)) if last else list(range(NTILE))
                junkR = sb(stg, "junkR", [128, D], BF16)

                def front(ti, t):
                    i = ti % 2
                    I_ = "%d" % i
                    mo = 0 if t < 2 else 3
                    tsl = slice(t * 128, (t + 1) * 128)
                    S.dma(ycs[i][:], YC[b, tsl, :], R=["YC"], W=["ycs" + I_])
                    S.dma(ycv[i][:], YT[b, :, tsl].rearrange("(c p) t -> p c t", p=128), R=["YT"], W=["ycv" + I_])
                    S.dma(ht[i][:], hsrc(l, b, t), R=["HB"], W=["htF" + I_])
                    for k in range(6):
                        S.tr(pT[i][:, k * 128:(k + 1) * 128], ycs[i][:, k * 128:(k + 1) * 128], idb[:], R=["ycs" + I_, "idb"], W=["pTF" + I_])
                    S.copy(ycT[i][:].rearrange("p k n -> p (k n)"), pT[i][:, 0:768], R=["pTF" + I_], W=["ycT" + I_], eng="scalar")
                    lhs = [ycT[i][:, 0, :], ycT[i][:, 1, :], ycT[i][:, 2, :], ycT[i][:, 3, :], ycv[i][:, 0, :], ycv[i][:, 1, :],
                           ycT[i][:, 4, :], ycT[i][:, 5, :]]
                    for nb in range(2):
                        pk = "pY%d" % (2 * i + nb)
                        for k in range(8):
                            S.mm(pY[2 * i + nb][:], lhs[k], wo[:, k, nb * 512:(nb + 1) * 512], start=(k == 0), stop=(k == 7),
                                 R=["ycT" + I_, "ycv" + I_, "wo"], W=[pk])
                        hs = slice(nb * 512, (nb + 1) * 512)
                        S.tt(tmp[:, hs], pY[2 * i + nb][:], mb[:, mo, hs], ALU.mult, R=[pk, "mbF"], W=["tmpF%d" % nb])
                        S.tt(hn[i][:, hs], ht[i][:, hs], tmp[:, hs], ALU.add, R=["htF" + I_, "tmpF%d" % nb], W=["hn" + I_], eng="gpsimd")
                    S.dma(HA[b, tsl, :], hn[i][:], R=["hn" + I_], W=["HA"], q="gpsimd")
                    S.actf(junk[:], hn[i][:], AF.Square, R=["hn" + I_], W=["junkF", "ssF" + I_], accum_out=ss[:, i:i + 1])
                    S.actf(rstd[:, i:i + 1], ss[:, i:i + 1], AF.Sqrt, R=["ssF" + I_, "epsb"], W=["rstdF" + I_], scale=1.0 / D, bias=epsb[:, 0:1])
                    S.recip(rstd[:, i:i + 1], rstd[:, i:i + 1], R=["rstdF" + I_], W=["rstdF" + I_])
                    S.stt(u32[i][:], hn[i][:], rstd[:, i:i + 1], mb[:, mo + 1, :], ALU.mult, ALU.mult,
                          R=["hn" + I_, "rstdF" + I_, "mbF"], W=["u32_" + I_])
                    S.tt(u32[i][:], u32[i][:], mb[:, mo + 2, :], ALU.add, R=["u32_" + I_, "mbF"], W=["u32_" + I_])
                    S.copy(ubf[i][:], u32[i][:], R=["u32_" + I_], W=["ubf" + I_], eng="scalar")

                def back(ti, t):
                    i = ti % 2
                    I_ = "%d" % i
                    mo = 0 if t < 2 else 3
                    tsl = slice(t * 128, (t + 1) * 128)
                    for k in range(8):
                        S.tr(pT[i][:, k * 128:(k + 1) * 128], ubf[i][:, k * 128:(k + 1) * 128], idb[:], R=["ubf" + I_, "idb"], W=["pTF" + I_])
                    S.copy(uTs[i][:].rearrange("p k n -> p (k n)"), pT[i][:], R=["pTF" + I_], W=["uTs" + I_])
                    S.dma(UT[b, :, tsl].rearrange("(k p) t -> p k t", p=128), uTs[i][:], R=["uTs" + I_], W=["UT"], q="gpsimd")
                    if last:
                        lg_ = lgt[i]
                        lk = "lgt" + I_
                        for e_ in range(NE):
                            S.stt(junkR[:], u32[i][:], 1.0, wr[:, e_, :], ALU.mult, ALU.mult, R=["u32_" + I_, "wr"],
                                  W=["junkR", lk], accum_out=lg_[:, e_:e_ + 1])
                        S.tt(lg_[:, 0:8], lg_[:, 0:8], rb[:], ALU.add, R=[lk, "rb"], W=[lk])
                        S.op("vector", lambda e, a=lg_: e.max(out=a[:, 8:16], in_=a[:, 0:8]), R=[lk], W=[lk])
                        S.ts(lg_[:, 16:24], lg_[:, 0:8], lg_[:, 8:9], None, ALU.is_equal, R=[lk], W=[lk])
                        S.ts(lg_[:, 24:32], lg_[:, 0:8], lg_[:, 9:10], None, ALU.is_equal, R=[lk], W=[lk])
                        S.tt(lg_[:, 10:11], lg_[:, 8:9], lg_[:, 9:10], ALU.subtract, R=[lk], W=[lk])
                        S.actf(lg_[:, 11:12], lg_[:, 10:11], AF.Sigmoid, R=[lk], W=[lk])
                        S.actf(lg_[:, 12:13], lg_[:, 10:11], AF.Sigmoid, R=[lk], W=[lk], scale=-1.0)
                        S.ts(lg_[:, 16:24], lg_[:, 16:24], lg_[:, 11:12], None, ALU.mult, R=[lk], W=[lk])
                        S.stt(lg_[:, 0:8], lg_[:, 24:32], lg_[:, 12:13], lg_[:, 16:24], ALU.mult, ALU.add, R=[lk], W=[lk])
                        S.dma(COMB[b, tsl, :], lg_[:, 0:8], R=[lk], W=["COMB"], q="gpsimd")

                segs = [list(enumerate(tiles))]
                for si_, seg in enumerate(segs):
                    if si_ > 0:
                        yield
                    front(*seg[0])
                    for j in range(len(seg)):
                        if j + 1 < len(seg):
                            front(*seg[j + 1])
                        back(*seg[j])
            return f

        def stage_g(l, b):
            last = l == 1

            def f(stg):
                t0 = 2 if last else 0
                ntile = NTILE - t0
                ntok = ntile * 128
                tok0 = t0 * 128
                nexp = NE if last else 1
                dff = D_FFE if last else D_FF
                uT = sb(stg, "uT", [128, 8, ntok], BF16)
                yacc = sb(stg, "yacc", [128, ntile, D], F32)
                wg = [sb(stg, "wg%d" % i, [128, 8, 512], BF16) for i in range(2)]
                wu = [sb(stg, "wu%d" % i, [128, 8, 512], BF16) for i in range(2)]
                wd = [sb(stg, "wd%d" % i, [128, 4, D], BF16) for i in range(2)]
                act = [sb(stg, "act%d" % i, [128, 4, 512], BF16) for i in range(2)]
                sg = [sb(stg, "sgG%d" % i, [128, 512], F32) for i in range(2)]
                comb = sb(stg, "comb", [128, ntile, NE], F32)
                pG = [psf(stg, "pG%d" % i) for i in range(2)]
                pU = [psf(stg, "pU%d" % i) for i in range(2)]
                pD = [psf(stg, "pD%d" % i) for i in range(4)]
                for k in range(8):
                    S.dma(uT[:, k, :], UT[b, k * 128:(k + 1) * 128, tok0:NT], R=["UT"], W=["uT"])
                if last:
                    S.dma(comb[:], COMB[b, tok0:NT, :].rearrange("(t p) e -> p t e", p=128), R=["COMB"], W=["comb"])
                S.memset(yacc[:], 0.0, W=["yacc"])
                groups = []
                for e_ in range(nexp):
                    for f0 in range(0, dff, 512):
                        groups.append((e_, f0, min(512, dff - f0)))
                tgs = [(s0, min(512, ntok - s0)) for s0 in range(0, ntok, 512)]
                cnt = {"gu": 0, "pd": 0, "sg": 0, "act": 0}

                def rec_gu(u):
                    gi, e_, f0, fw, s0, N, ai = u
                    wi = gi % 2
                    W_ = "%d" % wi
                    A_ = "%d" % ai
                    for c in range(fw // 128):
                        pi = cnt["gu"] % 2
                        cnt["gu"] += 1
                        for k in range(8):
                            S.mm(pG[pi][:, 0:N], wg[wi][:, k, c * 128:(c + 1) * 128], uT[:, k, s0:s0 + N], start=(k == 0), stop=(k == 7),
                                 R=["wg" + W_, "uT"], W=["pG%d" % pi])
                        for k in range(8):
                            S.mm(pU[pi][:, 0:N], wu[wi][:, k, c * 128:(c + 1) * 128], uT[:, k, s0:s0 + N], start=(k == 0), stop=(k == 7),
                                 R=["wu" + W_, "uT"], W=["pU%d" % pi])
                        si = cnt["sg"] % 2
                        cnt["sg"] += 1
                        S.actf(sg[si][:, 0:N], pG[pi][:, 0:N], AF.Silu, R=["pG%d" % pi], W=["sgG%d" % si])
                        S.tt(act[ai][:, c, 0:N], pU[pi][:, 0:N], sg[si][:, 0:N], ALU.mult, R=["pU%d" % pi, "sgG%d" % si], W=["act" + A_])

                def rec_down(u):
                    gi, e_, f0, fw, s0, N, ai = u
                    wi = gi % 2
                    W_ = "%d" % wi
                    A_ = "%d" % ai
                    nch = fw // 128
                    for s_ in range(N // 128):
                        tl = s0 // 128 + s_
                        for nb in range(2):
                            di = cnt["pd"] % 4
                            cnt["pd"] += 1
                            for c in range(nch):
                                S.mm(pD[di][:], act[ai][:, c, s_ * 128:(s_ + 1) * 128], wd[wi][:, c, nb * 512:(nb + 1) * 512],
                                     start=(c == 0), stop=(c == nch - 1), R=["act" + A_, "wd" + W_], W=["pD%d" % di])
                            ya = yacc[:, tl, nb * 512:(nb + 1) * 512]
                            yk = "yacc%d_%d" % (tl, nb)
                            sc = comb[:, tl, e_:e_ + 1] if last else 1.0
                            S.stt(ya, pD[di][:], sc, ya, ALU.mult, ALU.add, R=["pD%d" % di, "comb", "yacc", yk], W=[yk])

                def rec_wload(gi, e_, f0, fw):
                    wi = gi % 2
                    W_ = "%d" % wi
                    nch = fw // 128
                    gsrc = (moe_wg[e_] if last else ffn_wg[0])
                    usrc = (moe_wu[e_] if last else ffn_wu[0])
                    dsrc = (moe_wd[e_] if last else ffn_wd[0])
                    S.dma(wg[wi][:, :, 0:fw], gsrc[:, f0:f0 + fw].rearrange("(k p) n -> p k n", p=128), W=["wg" + W_], q="gpsimd")
                    S.dma(wu[wi][:, :, 0:fw], usrc[:, f0:f0 + fw].rearrange("(k p) n -> p k n", p=128), W=["wu" + W_], q="gpsimd")
                    S.dma(wd[wi][:, 0:nch, :], dsrc[f0:f0 + fw, :].rearrange("(c p) n -> p c n", p=128), W=["wd" + W_], q="gpsimd")

                units = []
                for gi, (e_, f0, fw) in enumerate(groups):
                    for (s0, N) in tgs:
                        units.append((gi, e_, f0, fw, s0, N, cnt["act"] % 2))
                        cnt["act"] += 1
                loaded = set()

                def ensure(u):
                    if u[0] not in loaded:
                        loaded.add(u[0])
                        rec_wload(u[0], u[1], u[2], u[3])

                ensure(units[0])
                rec_gu(units[0])
                for n, u in enumerate(units):
                    if n + 1 < len(units):
                        ensure(units[n + 1])
                        rec_gu(units[n + 1])
                    rec_down(u)
                yield
                g2 = sb(stg, "g2", [128, 2, D], F32)
                S.dma(g2[:, 1, :], modbc(None, l, b, 5), R=["MODS"], W=["g2"])
                if not last:
                    S.dma(g2[:, 0, :], modbc(None, l, 2, 5), R=["MODS"], W=["g2"])
                else:
                    S.dma(g2[:, 0, :], final_w.partition_broadcast(128), W=["g2"])
                hh = [sb(stg, "hh%d" % i, [128, D], F32) for i in range(2)]
                ssb = sb(stg, "ssG", [128, 2], F32)
                rsb = sb(stg, "rsG", [128, 2], F32)
                junk = sb(stg, "junkG", [128, D], BF16)
                for tl in range(ntile):
                    t = t0 + tl
                    i = tl % 2
                    I_ = "%d" % i
                    tsl = slice(t * 128, (t + 1) * 128)
                    ykeys = ["yacc", "yacc%d_0" % tl, "yacc%d_1" % tl]
                    S.dma(hh[i][:], HA[b, tsl, :], R=["HA"], W=["hh" + I_])
                    gsel = 1 if (t >= 2) else 0
                    S.tt(yacc[:, tl, :], yacc[:, tl, :], g2[:, gsel if not last else 1, :], ALU.mult, R=ykeys + ["g2"], W=ykeys[1:])
                    S.tt(hh[i][:], hh[i][:], yacc[:, tl, :], ALU.add, R=ykeys + ["hh" + I_], W=["hh" + I_], eng="gpsimd")
                    if not last:
                        S.dma(HB[b, tsl, :], hh[i][:], R=["hh" + I_], W=["HB"], q="gpsimd")
                    else:
                        S.actf(junk[:], hh[i][:], AF.Square, R=["hh" + I_], W=["junkG", "ssG" + I_], accum_out=ssb[:, i:i + 1])
                        S.actf(rsb[:, i:i + 1], ssb[:, i:i + 1], AF.Sqrt, R=["ssG" + I_, "epsb"], W=["rsG" + I_], scale=1.0 / D, bias=epsb[:, 0:1])
                        S.recip(rsb[:, i:i + 1], rsb[:, i:i + 1], R=["rsG" + I_], W=["rsG" + I_])
                        S.stt(hh[i][:], hh[i][:], rsb[:, i:i + 1], g2[:, 0, :], ALU.mult, ALU.mult, R=["hh" + I_, "rsG" + I_, "g2"], W=["hh" + I_])
                        S.dma(out[b, (t - 2) * 128:(t - 1) * 128, :], hh[i][:], R=["hh" + I_], W=["out"], q="gpsimd")
            return f

        prog = []
        for l in range(2):
            if P1:
                prog.append(stage_b(l, (0, 1)))
                for b in range(2):
                    prog.append(stage_c(l, b))
                    prog.append(stage_d(l, b))
                    prog.append(stage_e(l, b))
                for b in range(2):
                    prog.append(stage_f(l, b))
            for b in range(2):
                if (l == 0 and P1) or (l == 1 and P2 and b in bs):
                    prog.append(stage_g(l, b))
        for i, fn in enumerate(prog):
            if i >= upto:
                break
            stage(fn)
    return nc


def host_consts(na_rpb):
    ident = np.eye(128, dtype=np.float32)
    t = np.arange(T)
    row = (t // GRID_W).astype(np.float32)
    col = (t % GRID_W).astype(np.float32)
    inv = (np.float32(10000.0) ** (-np.arange(0, 16, 2, dtype=np.float32) / np.float32(16))).astype(np.float32)
    ang = np.concatenate([row[:, None] * inv, col[:, None] * inv], axis=-1).astype(np.float32)
    cos, sin = np.cos(ang).astype(np.float32), np.sin(ang).astype(np.float32)
    cos2 = np.ones((32, NT), np.float32)
    sinS = np.zeros((32, NT), np.float32)
    cos2[0:16, LC:] = cos.T
    cos2[16:32, LC:] = cos.T
    sinS[0:16, LC:] = -sin.T
    sinS[16:32, LC:] = sin.T
    ks = np.float32(32 ** -0.5)
    rope = np.stack([np.tile(cos2, (4, 1)), np.tile(sinS, (4, 1)), np.tile(cos2, (4, 1)) * ks, np.tile(sinS, (4, 1)) * ks]).astype(np.float32)
    j = np.arange(128)[:, None].astype(np.float32)
    i = np.arange(128)[None, :].astype(np.float32)
    retc = np.zeros((128, 772), np.float32)
    retc[:, 0:128] = np.maximum(i - j, 0)
    retc[:, 128:256] = (i >= j)
    retc[:, 256:384] = np.maximum(j - i, 0)
    retc[:, 384:512] = (j >= i)
    retc[:, 512:640] = i + 1.0
    retc[:, 640:768] = 128.0 - i
    retc[:, 768] = 127.0 - j[:, 0]
    retc[:, 769] = j[:, 0]
    kc = np.arange(64)[:, None]
    qc = np.arange(64)[None, :]
    w0 = np.clip(qc - 8, 0, 48)
    ok = (kc >= w0) & (kc < w0 + 16)
    dc = np.clip(kc - qc + 15, 0, 30)
    g = na_rpb[:, :, :, dc]
    g = np.where(ok[None, None, None], g, np.float32(-30000.0)).astype(np.float32)
    napb = np.ascontiguousarray(g.transpose(0, 3, 1, 2, 4)).reshape(2, 64, NH * 15 * 64)
    return ident, rope, retc, napb


_CACHE = {}
LAUNCH2 = ((0,), (1,))
SINGLE = True


def kernel(x, c, ctx, c_ctx, w_mod, b_mod, norm1_w, norm2_w, w_in, w_out, na_rpb, conv_w, conv_b,
           conv_ln_w, conv_ln_b, ret_decay, ret_gn_w, ffn_w_gate, ffn_w_up, ffn_w_down,
           moe_router, moe_router_b, moe_w_gate, moe_w_up, moe_w_down, final_norm_w):
    f = lambda a: np.ascontiguousarray(np.asarray(a, dtype=np.float32))
    ident, rope, retc, napb = host_consts(f(na_rpb))
    shared = {
        "w_mod": f(w_mod), "b_mod": f(b_mod), "norm1_w": f(norm1_w), "norm2_w": f(norm2_w), "w_in": f(w_in), "w_out": f(w_out),
        "convwT": np.ascontiguousarray(f(conv_w).reshape(2, 31, 2, 128).transpose(0, 3, 2, 1)),
        "cpard": np.ascontiguousarray(np.stack([f(conv_b), f(conv_ln_w), f(conv_ln_b)], axis=1).reshape(2, 3, 2, 128).transpose(0, 3, 1, 2)),
        "ret_decay": f(ret_decay).reshape(2, 8), "ret_gn_w": f(ret_gn_w),
        "ffn_w_gate": f(ffn_w_gate), "ffn_w_up": f(ffn_w_up), "ffn_w_down": f(ffn_w_down),
        "moe_routerT": np.ascontiguousarray(f(moe_router)[0].T), "moe_router_b": f(moe_router_b).reshape(1, NE),
        "moe_w_gate": f(moe_w_gate)[0], "moe_w_up": f(moe_w_up)[0], "moe_w_down": f(moe_w_down)[0],
        "final_norm_w": f(final_norm_w).reshape(1, D),
        "ident": ident, "rope": rope, "retc": retc, "napb": napb,
    }
    x = f(x); c = f(c); ctx = f(ctx); c_ctx = f(c_ctx)
    p2names = ("moe_w_gate", "moe_w_up", "moe_w_down", "final_norm_w")
    in1 = []
    for i in range(8):
        m = {k: v for k, v in shared.items() if k not in p2names}
        m["x2"] = x[2 * i:2 * i + 2]
        m["ctx2"] = ctx[2 * i:2 * i + 2]
        cv = np.concatenate([c[2 * i:2 * i + 2], c_ctx[None, :]], axis=0)
        m["cvecT"] = np.ascontiguousarray(cv.reshape(3, 8, 128).transpose(2, 1, 0))
        in1.append(m)
    if SINGLE:
        for i in range(8):
            for k in p2names:
                in1[i][k] = shared[k]
        if "nc" not in _CACHE:
            _CACHE["nc"] = build_program(part=0)
        r0 = run_bass_kernel_spmd(_CACHE["nc"], in1, core_ids=list(range(8))).results
        return np.concatenate([r["out"] for r in r0], axis=0)
    if "nc1" not in _CACHE:
        _CACHE["nc1"] = build_program(part=1)
        _CACHE["nc2"] = [build_program(part=2, bs=bs_) for bs_ in LAUNCH2]
    r1 = run_bass_kernel_spmd(_CACHE["nc1"], in1, core_ids=list(range(8))).results
    outs = [np.zeros((2, T, D), np.float32) for _ in range(8)]
    for bs_, nc2 in zip(LAUNCH2, _CACHE["nc2"]):
        in2 = []
        for i in range(8):
            m = {k: shared[k] for k in p2names}
            for k in ("MODS", "HA", "UT", "COMB"):
                m[k] = r1[i][k]
            in2.append(m)
        r2 = run_bass_kernel_spmd(nc2, in2, core_ids=list(range(8))).results
        for i in range(8):
            for b in bs_:
                outs[i][b] = r2[i]["out"][b]
    return np.concatenate(outs, axis=0)
```
